# Optimizing a Trainium2 kernel written in Bass

```python
import jax
import jax.numpy as jnp
from jax import lax
import numpy as np

D_MODEL = 2048
BATCH = 4
SEQ = 4096
DEPTH = 1

CTX_LEN = 256
GRID_W = 64
EPS = 1e-6

GLA_HEADS = 4
GLA_DK = 256
GLA_DV = 512
GLA_LOWRANK = 16
GLA_TAU = 16.0
GLA_CHUNK = 64
GLA_QK_W = GLA_HEADS * GLA_DK
GLA_V_W = GLA_HEADS * GLA_DV

MLA_HEADS = 16
MLA_Q_RANK = 512
MLA_KV_RANK = 512
MLA_NOPE = 128
MLA_ROPE = 64
MLA_V = 128
MLA_QK_DIM = MLA_NOPE + MLA_ROPE
MLA_OUT_W = MLA_HEADS * MLA_V
ROPE_THETA = 10000.0
Q_BLOCK = 128

N_GROUPS = 8
EXP_PER_GROUP = 8
N_EXPERTS = N_GROUPS * EXP_PER_GROUP
TOP_K_IN_GROUP = 2
D_EXPERT = 512
DISPATCH_BLOCK = 128

IN_SPLITS = (GLA_QK_W, GLA_QK_W, GLA_V_W, GLA_V_W, GLA_LOWRANK, GLA_LOWRANK,
             MLA_Q_RANK, MLA_KV_RANK, MLA_ROPE, D_MODEL, D_MODEL)
D_IN = 2 * GLA_QK_W + 2 * GLA_V_W + 2 * GLA_LOWRANK + MLA_Q_RANK + MLA_KV_RANK + MLA_ROPE + 2 * D_MODEL

kernel_name = 'hybrid_gla_mla_hmoe_dit_block'


def rmsnorm(x, g):
    xf = x.astype(jnp.float32)
    y = xf * lax.rsqrt(jnp.mean(xf * xf, axis=-1, keepdims=True) + EPS)
    return (y * g.astype(jnp.float32)).astype(x.dtype)


def modulate(x, g, shift, scale):
    return rmsnorm(x, g) * (1 + scale) + shift


def split_heads(x, n):
    b, l, _ = x.shape
    return x.reshape(b, l, n, -1).transpose(0, 2, 1, 3)


def merge_heads(x):
    b, h, l, d = x.shape
    return x.transpose(0, 2, 1, 3).reshape(b, l, h * d)


def flip_seq(t):
    return jnp.flip(t, axis=2)


def split_in(p):
    offsets = np.cumsum(IN_SPLITS)[:-1].tolist()
    return jnp.split(p, offsets, axis=-1)


def axial_angles(length):
    rows = length // GRID_W
    row = jnp.repeat(jnp.arange(rows), GRID_W).astype(jnp.float32)
    col = jnp.tile(jnp.arange(GRID_W), rows).astype(jnp.float32)
    n_freq = MLA_ROPE // 4
    inv = 1.0 / (ROPE_THETA ** (jnp.arange(n_freq, dtype=jnp.float32) / n_freq))
    return row[:, None] * inv, col[:, None] * inv


def rope_half(x, ang):
    half = x.shape[-1] // 2
    cos = jnp.cos(ang).astype(x.dtype)
    sin = jnp.sin(ang).astype(x.dtype)
    x1, x2 = x[..., :half], x[..., half:]
    return jnp.concatenate([x1 * cos - x2 * sin, x1 * sin + x2 * cos], axis=-1)


def rope_tail_2d(x, angles):
    ang_r, ang_c = angles
    half = MLA_ROPE // 2
    x_nope = x[..., :MLA_NOPE]
    x_row = x[..., MLA_NOPE:MLA_NOPE + half]
    x_col = x[..., MLA_NOPE + half:]
    return jnp.concatenate([x_nope, rope_half(x_row, ang_r), rope_half(x_col, ang_c)], axis=-1)


def to_heads_f32(t, n):
    return split_heads(t, n).astype(jnp.float32)


def gla_log_decay(a_low, w2, b2):
    z = (a_low @ w2 + b2).astype(jnp.float32)
    return split_heads(jax.nn.log_sigmoid(z) / GLA_TAU, GLA_HEADS)


def gla_chunked(q, k, v, g, s0):
    b_, h_, l_, dk = q.shape
    dv = v.shape[-1]
    n = l_ // GLA_CHUNK
    q, k, g = (a.reshape(b_, h_, n, GLA_CHUNK, dk) for a in (q, k, g))
    v = v.reshape(b_, h_, n, GLA_CHUNK, dv)
    bc = jnp.cumsum(g, axis=3)
    b_end = bc[:, :, :, -1:, :]
    q_dec = q * jnp.exp(bc)
    k_inv = k * jnp.exp(-bc)
    k_to_end = k * jnp.exp(b_end - bc)
    mask = jnp.tril(jnp.ones((GLA_CHUNK, GLA_CHUNK), dtype=bool))
    att = jnp.where(mask, jnp.einsum('bhnid,bhnjd->bhnij', q_dec, k_inv), 0.0)
    o_intra = jnp.einsum('bhnij,bhnje->bhnie', att, v)

    def step(state, xs):
        qd, ke, vc, de = xs
        o = jnp.einsum('bhid,bhde->bhie', qd, state)
        state = state * de[..., None] + jnp.einsum('bhjd,bhje->bhde', ke, vc)
        return state, o

    xs = tuple(jnp.moveaxis(t, 2, 0) for t in (q_dec, k_to_end, v, jnp.exp(b_end[:, :, :, 0, :])))
    s_end, o_inter = lax.scan(step, s0, xs)
    o = o_intra + jnp.moveaxis(o_inter, 0, 2)
    return o.reshape(b_, h_, l_, dv), s_end


def gla_final_state(k, v, g):
    bc = jnp.cumsum(g, axis=2)
    return jnp.einsum('bhld,bhle->bhde', k * jnp.exp(bc[:, :, -1:, :] - bc), v)


def gla_readout(o, norm_g, r):
    return merge_heads(rmsnorm(o, norm_g)).astype(r.dtype) * jax.nn.silu(r)


def mla_queries(cq, qa_g, w_uq, qn_g, angles):
    q = split_heads(rmsnorm(cq, qa_g) @ w_uq, MLA_HEADS)
    q = rmsnorm(q, qn_g)
    return q if angles is None else rope_tail_2d(q, angles)


def mla_keys_values(ckv, kr, kva_g, w_ukv, kn_g, angles):
    kv = split_heads(rmsnorm(ckv, kva_g) @ w_ukv, MLA_HEADS)
    k_nope, v = kv[..., :MLA_NOPE], kv[..., MLA_NOPE:]
    b_, h_, l_, _ = k_nope.shape
    k_rope = jnp.broadcast_to(kr[:, None, :, :], (b_, h_, l_, MLA_ROPE))
    k = rmsnorm(jnp.concatenate([k_nope, k_rope], axis=-1), kn_g)
    return (k if angles is None else rope_tail_2d(k, angles)), v


def softmax_attention(q, k, v):
    s = jnp.einsum('bhqd,bhkd->bhqk', q, k).astype(jnp.float32) * (MLA_QK_DIM ** -0.5)
    p = jax.nn.softmax(s, axis=-1).astype(v.dtype)
    return jnp.einsum('bhqk,bhkd->bhqd', p, v)


def blockwise_attention(q, k, v):
    b_, h_, l_, d_ = q.shape
    nb = l_ // Q_BLOCK
    qb = q.reshape(b_, h_, nb, Q_BLOCK, d_).transpose(2, 0, 1, 3, 4)
    ob = lax.map(lambda qi: softmax_attention(qi, k, v), qb)
    return ob.transpose(1, 2, 0, 3, 4).reshape(b_, h_, l_, v.shape[-1])


def branch_merge(y_gla, y_mla, gate_gla, gate_mla, w_o_gla, w_o_mla, w_out):
    y = jax.nn.sigmoid(gate_gla) * (y_gla @ w_o_gla) + jax.nn.sigmoid(gate_mla) * (y_mla @ w_o_mla)
    return y @ w_out


def hierarchical_moe(h, w_rg, b_rg, w_re, b_re, w_gate, w_up, w_down):
    b_, l_, d_ = h.shape
    t = b_ * l_
    hf = h.reshape(t, d_)
    p_grp = jax.nn.softmax((hf @ w_rg + b_rg).astype(jnp.float32), axis=-1)
    p_g, g_idx = lax.top_k(p_grp, 1)
    logit_e = (hf @ w_re + b_re).astype(jnp.float32).reshape(t, N_GROUPS, EXP_PER_GROUP)
    logit_e = jnp.take_along_axis(logit_e, g_idx[:, :, None], axis=1)[:, 0, :]
    p_e, e_idx = lax.top_k(jax.nn.softmax(logit_e, axis=-1), TOP_K_IN_GROUP)
    wts = (p_g * p_e / jnp.sum(p_e, axis=-1, keepdims=True)).reshape(-1)
    eid = (g_idx * EXP_PER_GROUP + e_idx).reshape(-1)
    n_assign = t * TOP_K_IN_GROUP
    tok = jnp.repeat(jnp.arange(t, dtype=jnp.int32), TOP_K_IN_GROUP)
    order = jnp.argsort(eid)
    e_s, tok_s, w_s = eid[order], tok[order], wts[order]
    counts = jnp.zeros((N_EXPERTS,), jnp.int32).at[eid].add(1)
    start = jnp.cumsum(counts) - counts
    padded = (counts + DISPATCH_BLOCK - 1) // DISPATCH_BLOCK * DISPATCH_BLOCK
    pend = jnp.cumsum(padded)
    pstart = pend - padded
    dest = pstart[e_s] + jnp.arange(n_assign, dtype=jnp.int32) - start[e_s]
    n_blocks = (n_assign + DISPATCH_BLOCK - 1) // DISPATCH_BLOCK + N_EXPERTS
    slot_tok = jnp.full((n_blocks * DISPATCH_BLOCK,), t, jnp.int32).at[dest].set(tok_s)
    slot_w = jnp.zeros((n_blocks * DISPATCH_BLOCK,), h.dtype).at[dest].set(w_s.astype(h.dtype))
    block_e = jnp.minimum(
        jnp.searchsorted(pend, jnp.arange(n_blocks, dtype=jnp.int32) * DISPATCH_BLOCK, side='right'),
        N_EXPERTS - 1)
    h_pad = jnp.concatenate([hf, jnp.zeros((1, d_), h.dtype)], axis=0)

    def expert_block(args):
        idx, w, e = args
        xb = h_pad[idx]
        y = (jax.nn.silu(xb @ w_gate[e]) * (xb @ w_up[e])) @ w_down[e]
        return y * w[:, None]

    y = lax.map(expert_block, (slot_tok.reshape(n_blocks, DISPATCH_BLOCK),
                               slot_w.reshape(n_blocks, DISPATCH_BLOCK), block_e))
    out = jnp.zeros((t + 1, d_), h.dtype).at[slot_tok].add(y.reshape(-1, d_))
    return out[:t].reshape(b_, l_, d_)


def setup_inputs(seed: int = 0) -> dict:
    key = jax.random.key(seed)
    ks = jax.random.split(key, 32)
    d = D_MODEL

    def nrm(k, shape, scale):
        return jax.random.normal(k, shape, jnp.float32) * scale

    def gain(k, n):
        return 1.0 + nrm(k, (DEPTH, n), 0.02)

    return {
        'x': nrm(ks[0], (BATCH, SEQ, d), 1.0),
        'c': nrm(ks[1], (BATCH, d), 1.0),
        'ctx': nrm(ks[2], (BATCH, CTX_LEN, d), 1.0),
        'c_ctx': nrm(ks[3], (d,), 1.0),
        'w_mod': nrm(ks[4], (DEPTH, d, 6 * d), 0.5 * d ** -0.5),
        'b_mod': nrm(ks[5], (DEPTH, 6 * d), 0.02),
        'norm1_g': gain(ks[6], d),
        'norm2_g': gain(ks[7], d),
        'w_in': nrm(ks[8], (DEPTH, d, D_IN), d ** -0.5),
        'w_decay_f': nrm(ks[9], (DEPTH, GLA_LOWRANK, GLA_QK_W), GLA_LOWRANK ** -0.5),
        'b_decay_f': nrm(ks[10], (DEPTH, GLA_QK_W), 0.1),
        'w_decay_b': nrm(ks[11], (DEPTH, GLA_LOWRANK, GLA_QK_W), GLA_LOWRANK ** -0.5),
        'b_decay_b': nrm(ks[12], (DEPTH, GLA_QK_W), 0.1),
        'gla_norm_g': gain(ks[13], GLA_DV),
        'q_a_norm_g': gain(ks[14], MLA_Q_RANK),
        'w_uq': nrm(ks[15], (DEPTH, MLA_Q_RANK, MLA_HEADS * MLA_QK_DIM), MLA_Q_RANK ** -0.5),
        'kv_a_norm_g': gain(ks[16], MLA_KV_RANK),
        'w_ukv': nrm(ks[17], (DEPTH, MLA_KV_RANK, MLA_HEADS * (MLA_NOPE + MLA_V)), MLA_KV_RANK ** -0.5),
        'q_norm_g': gain(ks[18], MLA_QK_DIM),
        'k_norm_g': gain(ks[19], MLA_QK_DIM),
        'w_o_gla': nrm(ks[20], (DEPTH, GLA_V_W, d), GLA_V_W ** -0.5),
        'w_o_mla': nrm(ks[21], (DEPTH, MLA_OUT_W, d), MLA_OUT_W ** -0.5),
        'w_out': nrm(ks[22], (DEPTH, d, d), d ** -0.5),
        'w_router_group': nrm(ks[23], (DEPTH, d, N_GROUPS), d ** -0.5),
        'b_router_group': nrm(ks[24], (DEPTH, N_GROUPS), 0.01),
        'w_router_expert': nrm(ks[25], (DEPTH, d, N_EXPERTS), d ** -0.5),
        'b_router_expert': nrm(ks[26], (DEPTH, N_EXPERTS), 0.01),
        'w_exp_gate': nrm(ks[27], (DEPTH, N_EXPERTS, d, D_EXPERT), d ** -0.5),
        'w_exp_up': nrm(ks[28], (DEPTH, N_EXPERTS, d, D_EXPERT), d ** -0.5),
        'w_exp_down': nrm(ks[29], (DEPTH, N_EXPERTS, D_EXPERT, d), D_EXPERT ** -0.5),
    }


def reference(x, c, ctx, c_ctx, w_mod, b_mod, norm1_g, norm2_g, w_in,
              w_decay_f, b_decay_f, w_decay_b, b_decay_b, gla_norm_g,
              q_a_norm_g, w_uq, kv_a_norm_g, w_ukv, q_norm_g, k_norm_g,
              w_o_gla, w_o_mla, w_out,
              w_router_group, b_router_group, w_router_expert, b_router_expert,
              w_exp_gate, w_exp_up, w_exp_down):
    angles = axial_angles(x.shape[1])
    for l in range(DEPTH):
        update_ctx = l < DEPTH - 1
        sh1, sc1, gt1, sh2, sc2, gt2 = jnp.split(
            (jax.nn.silu(c) @ w_mod[l] + b_mod[l])[:, None, :], 6, axis=-1)
        sh1c, sc1c, gt1c, sh2c, sc2c, gt2c = jnp.split(
            (jax.nn.silu(c_ctx) @ w_mod[l] + b_mod[l])[None, None, :], 6, axis=-1)

        h = modulate(x, norm1_g[l], sh1, sc1)
        hc = modulate(ctx, norm1_g[l], sh1c, sc1c)
        qg, kg, vg, rg, af, ab, cq, ckv, kr, ga, gb = split_in(h @ w_in[l])
        qgc, kgc, vgc, rgc, afc, abc, cqc, ckvc, krc, gac, gbc = split_in(hc @ w_in[l])

        q = to_heads_f32(qg, GLA_HEADS) * GLA_DK ** -0.5
        k = to_heads_f32(kg, GLA_HEADS)
        v = to_heads_f32(vg, GLA_HEADS)
        g_f = gla_log_decay(af, w_decay_f[l], b_decay_f[l])
        g_b = gla_log_decay(ab, w_decay_b[l], b_decay_b[l])
        kc = to_heads_f32(kgc, GLA_HEADS)
        vc = to_heads_f32(vgc, GLA_HEADS)
        gc_f = gla_log_decay(afc, w_decay_f[l], b_decay_f[l])
        gc_b = gla_log_decay(abc, w_decay_b[l], b_decay_b[l])
        if update_ctx:
            qc = to_heads_f32(qgc, GLA_HEADS) * GLA_DK ** -0.5
            zero = jnp.zeros(kc.shape[:2] + (GLA_DK, GLA_DV), jnp.float32)
            oc_f, s_f = gla_chunked(qc, kc, vc, gc_f, zero)
            oc_b, s_b = gla_chunked(flip_seq(qc), flip_seq(kc), flip_seq(vc), flip_seq(gc_b), zero)
            oc_gla = oc_f + flip_seq(oc_b)
        else:
            s_f = gla_final_state(kc, vc, gc_f)
            s_b = gla_final_state(flip_seq(kc), flip_seq(vc), flip_seq(gc_b))
        o_f, _ = gla_chunked(q, k, v, g_f, s_f)
        o_b, _ = gla_chunked(flip_seq(q), flip_seq(k), flip_seq(v), flip_seq(g_b), s_b)
        y_gla = gla_readout(o_f + flip_seq(o_b), gla_norm_g[l], rg)

        q_m = mla_queries(cq, q_a_norm_g[l], w_uq[l], q_norm_g[l], angles)
        k_m, v_m = mla_keys_values(ckv, kr, kv_a_norm_g[l], w_ukv[l], k_norm_g[l], angles)
        k_mc, v_mc = mla_keys_values(ckvc, krc, kv_a_norm_g[l], w_ukv[l], k_norm_g[l], None)
        y_mla = merge_heads(blockwise_attention(
            q_m, jnp.concatenate([k_m, k_mc], axis=2), jnp.concatenate([v_m, v_mc], axis=2)))

        if update_ctx:
            q_mc = mla_queries(cqc, q_a_norm_g[l], w_uq[l], q_norm_g[l], None)
            yc_mla = merge_heads(softmax_attention(q_mc, k_mc, v_mc))
            yc_gla = gla_readout(oc_gla, gla_norm_g[l], rgc)
            ctx = ctx + gt1c * branch_merge(yc_gla, yc_mla, gac, gbc, w_o_gla[l], w_o_mla[l], w_out[l])
            ctx = ctx + gt2c * hierarchical_moe(
                modulate(ctx, norm2_g[l], sh2c, sc2c), w_router_group[l], b_router_group[l],
                w_router_expert[l], b_router_expert[l], w_exp_gate[l], w_exp_up[l], w_exp_down[l])

        x = x + gt1 * branch_merge(y_gla, y_mla, ga, gb, w_o_gla[l], w_o_mla[l], w_out[l])
        x = x + gt2 * hierarchical_moe(
            modulate(x, norm2_g[l], sh2, sc2), w_router_group[l], b_router_group[l],
            w_router_expert[l], b_router_expert[l], w_exp_gate[l], w_exp_up[l], w_exp_down[l])
    return x
```

```python
import contextlib
import numpy as np
import concourse.bass as bass
import concourse.mybir as mybir
from concourse.bass_utils import run_bass_kernel_spmd

F32 = mybir.dt.float32
BF16 = mybir.dt.bfloat16
I32 = mybir.dt.int32
AF = mybir.ActivationFunctionType
ALU = mybir.AluOpType
AX = mybir.AxisListType

D = 2048
NLOC = 2048
NA = 2304
NB = 256
NKEY = NA + NLOC
EPS = 1e-6
NEXP = 64
CAP = 384
D_IN = 11360

ENGS = ("pe", "act", "dve", "pool", "sp")
NDSEM = 6
import os
POOL_ENG = os.environ.get("POOL_ENG", "pool")


class Buf:
    __slots__ = ("name", "writers", "readers", "excl")

    def __init__(self, name=""):
        self.name = name
        self.writers = {}
        self.readers = {}
        self.excl = False


class Op:
    __slots__ = ("eng", "fn", "deps", "is_dma", "dma_sem", "dma_val", "inc", "cnt")

    def __init__(self, eng, fn, is_dma):
        self.eng = eng
        self.fn = fn
        self.deps = []
        self.is_dma = is_dma
        self.dma_sem = None
        self.dma_val = None
        self.inc = False
        self.cnt = None


class Prog:
    def __init__(self, nc):
        self.nc = nc
        self.ops = {e: [] for e in ENGS}
        self.ndma = {e: 0 for e in ENGS}
        self.last_dma = {}
        self.pending = {e: None for e in ENGS}

    def _add(self, eng, fn, reads, writes, is_dma):
        op = Op(eng, fn, is_dma)
        deps = {}
        reads = [getattr(b, "buf", b) for b in reads]
        writes = [getattr(b, "buf", b) for b in writes]
        writes = writes + [b for b in reads if b.excl and b not in writes]
        reads = [b for b in reads if not b.excl]
        if self.pending[eng] is not None:
            for t in self.pending[eng]:
                deps[id(t)] = t
            self.pending[eng] = None
        for b in reads:
            b = getattr(b, "buf", b)
            for t in b.writers.values():
                deps[id(t)] = t
        for b in writes:
            b = getattr(b, "buf", b)
            for t in b.writers.values():
                deps[id(t)] = t
            for t in b.readers.values():
                deps[id(t)] = t
        if is_dma:
            k = self.ndma[eng]
            self.ndma[eng] += 1
            op.dma_sem = (eng, k % NDSEM)
            op.dma_val = 16 * (k // NDSEM + 1)
            self.last_dma[op.dma_sem] = op
            key = ("dma", id(op))
        else:
            key = eng
        op.deps = list(deps.values())
        for b in reads:
            b = getattr(b, "buf", b)
            b.readers[key] = op
        for b in writes:
            b = getattr(b, "buf", b)
            b.writers = {key: op}
            b.readers = {}
        self.ops[eng].append(op)
        return op

    def op(self, eng, fn, reads=(), writes=()):
        return self._add(eng, fn, reads, writes, False)

    def dma(self, eng, fn, reads=(), writes=()):
        return self._add(eng, fn, reads, writes, True)

    def barrier(self):
        toks = []
        for e in ENGS:
            for op in reversed(self.ops[e]):
                if not op.is_dma:
                    toks.append(op)
                    break
        toks += list(self.last_dma.values())
        for e in ENGS:
            self.pending[e] = list(toks) + (self.pending[e] or [])

    def emit(self):
        nc = self.nc
        for e in ENGS:
            for op in self.ops[e]:
                for d in op.deps:
                    if not d.is_dma and not (d.eng == e and e == "pe"):
                        d.inc = True
        for e in ENGS:
            c = 0
            for op in self.ops[e]:
                if not op.is_dma and op.inc:
                    c += 1
                    op.cnt = c
        with contextlib.ExitStack() as st:
            esem = {e: st.enter_context(nc.semaphore("s_" + e)) for e in ENGS if e != "sp"}
            dsem = {}
            for e in ENGS:
                if self.ndma[e] > 0:
                    for i in range(NDSEM):
                        dsem[(e, i)] = st.enter_context(nc.semaphore(f"d_{e}{i}"))
            block = st.enter_context(nc.Block())

            def run(e, eng):
                waited = {}

                def wait(sem, val, key):
                    if waited.get(key, 0) >= val:
                        return
                    waited[key] = val
                    eng.wait_ge(sem, val)

                for op in self.ops[e]:
                    for d in op.deps:
                        if d.is_dma:
                            wait(dsem[d.dma_sem], d.dma_val, d.dma_sem)
                        elif not (d.eng == e and e == "pe"):
                            wait(esem[d.eng], d.cnt, d.eng)
                    if op.is_dma:
                        if op.dma_val > 16:
                            wait(dsem[op.dma_sem], op.dma_val - 16, op.dma_sem)
                        op.fn(eng).then_inc(dsem[op.dma_sem], 16)
                    else:
                        ins = op.fn(eng)
                        if op.inc:
                            ins.then_inc(esem[e], 1)
                last = {}
                for op in self.ops[e]:
                    if op.is_dma:
                        last[op.dma_sem] = op.dma_val
                for k, v in last.items():
                    wait(dsem[k], v, k)

            if self.ops["sp"]:
                @block.sync
                def _(eng):
                    run("sp", eng)
            if self.ops["act"]:
                @block.scalar
                def _(eng):
                    run("act", eng)
            if self.ops["dve"]:
                @block.vector
                def _(eng):
                    run("dve", eng)
            if self.ops["pool"]:
                @block.gpsimd
                def _(eng):
                    run("pool", eng)
            if self.ops["pe"]:
                @block.tensor
                def _(eng):
                    run("pe", eng)


class Tl:
    def __init__(self, ap, name=""):
        self.ap = ap
        self.buf = Buf(name)

    def __getitem__(self, k):
        return self.ap[k]


class Ring:
    def __init__(self, tiles):
        self.t = tiles
        self.i = 0

    def next(self):
        t = self.t[self.i % len(self.t)]
        self.i += 1
        return t


_DSZ = {F32: 4, BF16: 2, I32: 4}


class Arena:
    def __init__(self, nc, st, nbytes):
        self.t = st.enter_context(nc.sbuf_tensor("arena", [128, nbytes // 4], F32))
        self.cap = nbytes
        self.off = 0

    def reset(self):
        self.off = 0

    def alloc(self, free, dt, name=""):
        free = tuple(free)
        n = int(np.prod(free))
        nb = (n * _DSZ[dt] + 31) // 32 * 32
        assert self.off + nb <= self.cap, (name, self.off, nb, self.cap)
        ap = self.t[:, self.off // 4:(self.off + nb) // 4]
        self.off += nb
        if dt != F32:
            ap = ap.bitcast(dt)
        ap = ap[:, 0:n]
        if len(free) == 2:
            ap = ap.rearrange("p (a b) -> p a b", a=free[0])
        elif len(free) == 3:
            ap = ap.rearrange("p (a b c) -> p a b c", a=free[0], b=free[1])
        return Tl(ap, name)

    def ring(self, k, free, dt, name=""):
        return Ring([self.alloc(free, dt, f"{name}{i}") for i in range(k)])


def build(dbg=(), stop_after=99, nexp=NEXP):
    nc = bass.Bass("TRN2", target_bir_lowering=False)
    dbg = set(dbg)

    def din(name, shape, dt=F32):
        return nc.dram_tensor(name, list(shape), dt, kind="ExternalInput").ap()

    def dscr(name, shape, dt):
        kind = "ExternalOutput" if name in dbg else "Internal"
        return Tl(nc.dram_tensor(name, list(shape), dt, kind=kind).ap(), name)

    x_loc = din("x_loc", [NLOC, D])
    x_A = din("x_A", [NA, D])
    x_B = din("x_B", [NB, D])
    cT = din("cT", [128, 16, 2])
    w_mod = din("w_mod", [D, 6 * D])
    bmod2 = din("bmod2", [2, 6 * D])
    norm1_g = din("norm1_g", [1, D])
    norm2_g = din("norm2_g", [1, D])
    w_in = din("w_in", [D, D_IN])
    w_a12 = din("w_a12", [D, 32])
    wd1 = din("wd1", [17, 1024])
    wd2 = din("wd2", [17, 1024])
    gla_g = din("gla_g", [1, 512])
    qa_g = din("qa_g", [128, 4])
    kva_g = din("kva_g", [128, 4])
    w_uq_n = din("w_uq_n", [512, 2048])
    w_uq_r = din("w_uq_r", [512, 1024])
    w_ukv_k = din("w_ukv_k", [512, 2048])
    w_ukv_v = din("w_ukv_v", [512, 2048])
    qn_g = din("qn_g", [192, 1])
    kn_g = din("kn_g", [192, 1])
    w_o_gla = din("w_o_gla", [D, D])
    w_o_mla = din("w_o_mla", [D, D])
    w_out = din("w_out", [D, D])
    w_rt = din("w_rt", [D, 72])
    b_rt = din("b_rt", [1, 72])
    w_eg = din("w_eg", [nexp, D, 512])
    w_eu = din("w_eu", [nexp, D, 512])
    w_ed = din("w_ed", [nexp, 512, D])
    cosq = din("cosq", [64, NLOC])
    sinq = din("sinq", [64, NLOC])
    cosk = din("cosk", [64, NKEY])
    sink = din("sink", [64, NKEY])
    consts = din("consts", [8, 128, 128])
    rt_c = din("rt_c", [64, 64])
    iota_c = din("iota_c", [128, 64])
    out_d = nc.dram_tensor("out", [NLOC, D], F32, kind="ExternalOutput").ap()

    mod_s = dscr("mod_s", [2, 6 * D], F32)
    qT_s = dscr("qT_s", [1024, NLOC], BF16)
    kT_s = dscr("kT_s", [1024, NLOC], BF16)
    k_s = dscr("k_s", [NLOC, 1024], BF16)
    v_s = dscr("v_s", [NLOC, 2048], BF16)
    r_s = dscr("r_s", [NLOC, 2048], BF16)
    aT_s = dscr("aT_s", [32, NLOC], F32)
    cqnT_s = dscr("cqnT_s", [512, NLOC], BF16)
    ckvnT_s = dscr("ckvnT_s", [512, NKEY], BF16)
    krT_s = dscr("krT_s", [64, NKEY], F32)
    gaT_s = dscr("gaT_s", [D, NLOC], F32)
    gbT_s = dscr("gbT_s", [D, NLOC], F32)
    kA_s = dscr("kA_s", [NA, 1024], BF16)
    vA_s = dscr("vA_s", [NA, 2048], BF16)
    aA_s = dscr("aA_s", [16, NA], F32)
    kB_s = dscr("kB_s", [NB, 1024], BF16)
    vB_s = dscr("vB_s", [NB, 2048], BF16)
    aB_s = dscr("aB_s", [16, NB], F32)
    of_s = dscr("of_s", [NLOC, 2048], F32)
    yglaT_s = dscr("yglaT_s", [D, NLOC], BF16)
    ymlaT_s = dscr("ymlaT_s", [D, NLOC], BF16)
    x1_s = dscr("x1_s", [NLOC, D], F32)
    h2_s = dscr("h2_s", [NLOC, D], BF16)
    xdisp = dscr("xdisp", [NEXP * CAP + 128, D], BF16)
    cnt_s = dscr("cnt_s", [1, 64], F32)
    ydisp = dscr("ydisp", [NEXP * CAP + 128, D], F32)

    P = Prog(nc)
    with contextlib.ExitStack() as st:
        A = Arena(nc, st, 200 * 1024)
        def sbt(name, shape, dt):
            return Tl(st.enter_context(nc.sbuf_tensor(name, list(shape), dt)), name)
        cst_f = sbt("cst_f", [128, 8, 128], F32)
        cst_b = sbt("cst_b", [128, 8, 128], BF16)
        psb = [Tl(st.enter_context(nc.psum_tensor(f"ps{i}", [128, 512], F32)), f"ps{i}") for i in range(5)]
        psT = [Tl(st.enter_context(nc.psum_tensor(f"psT{i}", [128, 8, 128], BF16)), f"psT{i}") for i in range(2)]
        psS = Tl(st.enter_context(nc.psum_tensor("psS", [128, 512], F32)), "psS")
        for _t in psb + psT + [psS]:
            _t.buf.excl = True
        psr = Ring(psb)
        psTr = Ring(psT[:int(os.environ.get('NPST', '2'))])

        IDENT, SU, UI, MASKF, SL, LI, MASKB, ONES = range(8)

        P.dma("sp", lambda e: e.dma_start(out=cst_f[:], in_=consts.rearrange("c p n -> p c n")), writes=[cst_f])
        P.op("dve", lambda e: e.tensor_copy(out=cst_b[:], in_=cst_f[:]), reads=[cst_f], writes=[cst_b])

        def ACT(out_ap, in_ap, func, reads, writes, **kw):
            P.op("act", lambda e: e.activation(out=out_ap, in_=in_ap, func=func, **kw), reads=reads, writes=writes)

        def CP(eng, out_ap, in_ap, reads, writes):
            if eng == "act":
                P.op("act", lambda e: e.copy(out=out_ap, in_=in_ap), reads=reads, writes=writes)
            else:
                P.op(eng, lambda e: e.tensor_copy(out=out_ap, in_=in_ap), reads=reads, writes=writes)

        def TT(eng, out_ap, in0, in1, op, reads, writes):
            P.op(eng, lambda e: e.tensor_tensor(out=out_ap, in0=in0, in1=in1, op=op), reads=reads, writes=writes)

        def TS(eng, out_ap, in0, s1, s2, op0, op1, reads, writes):
            if s2 is None:
                P.op(eng, lambda e: e.tensor_scalar(out=out_ap, in0=in0, scalar1=s1, scalar2=None, op0=op0),
                     reads=reads, writes=writes)
            else:
                P.op(eng, lambda e: e.tensor_scalar(out=out_ap, in0=in0, scalar1=s1, scalar2=s2, op0=op0, op1=op1),
                     reads=reads, writes=writes)

        def STT(out_ap, in0, scalar, in1, op0, op1, reads, writes):
            P.op("dve", lambda e: e.scalar_tensor_tensor(out=out_ap, in0=in0, scalar=scalar, in1=in1, op0=op0, op1=op1),
                 reads=reads, writes=writes)

        def MM(ps_ap, lhsT, rhs, start, stop, reads, writes):
            P.op("pe", lambda e: e.matmul(ps_ap, lhsT=lhsT, rhs=rhs, start=start, stop=stop), reads=reads, writes=writes)

        def TR(ps_ap, in_ap, ident, reads, writes):
            P.op("pe", lambda e: e.transpose(out=ps_ap, in_=in_ap, identity=ident), reads=reads, writes=writes)

        def DMA(eng, out_ap, in_ap, reads, writes):
            P.dma(eng, lambda e: e.dma_start(out=out_ap, in_=in_ap), reads=reads, writes=writes)

        def RED(eng, out_ap, in_ap, op, reads, writes):
            P.op(eng, lambda e: e.tensor_reduce(out=out_ap, in_=in_ap, axis=AX.X, op=op), reads=reads, writes=writes)

        flip = [0]

        EVAC = os.environ.get("EVAC", "both")

        def evac_eng():
            flip[0] ^= 1
            if EVAC != "both":
                return EVAC
            return "act" if flip[0] else "dve"

        def rstd_ops(sst, ssa, rst, rsa, n, extra=1.0):
            ACT(rsa, ssa, AF.Sqrt, [sst], [rst], scale=1.0 / n, bias=EPS)
            P.op("dve", lambda e: e.reciprocal(out=rsa, in_=rsa), reads=[rst], writes=[rst])
            if extra != 1.0:
                TS("dve", rsa, rsa, extra, None, ALU.mult, None, [rst], [rst])

        IDENT, SU, UI, MASKF, SL, LI, MASKB, ONES = range(8)
        DMA("sp", cst_f[:], consts.rearrange("c p n -> p c n"), [], [cst_f])
        CP("dve", cst_b[:], cst_f[:], [cst_f], [cst_b])
        identb_t = sbt("identb", [128, 128], BF16)
        CP("dve", identb_t[:], cst_f[:, IDENT, :], [cst_f], [identb_t])
        ident_b = identb_t[:] if os.environ.get("IDSEP", "1") == "1" else cst_b[:, IDENT, :]
        ident_f = cst_f[:, IDENT, :]

        A.reset()
        cTf = A.alloc([16, 2], F32, "cTf")
        cTe = A.alloc([16, 2], F32, "cTe")
        cTs = A.alloc([16, 2], BF16, "cTs")
        bm = A.alloc([6 * D], F32, "bm")
        mrow = A.alloc([6 * D], F32, "mrow")
        wtm = A.ring(3, [16, 512], BF16, "wtm")
        DMA("sp", cTf[:], cT, [], [cTf])
        DMA("sp", bm[0:2, :], bmod2, [], [bm])
        ACT(cTe[:], cTf[:], AF.Sigmoid, [cTf], [cTe])
        TT("dve", cTs[:], cTf[:], cTe[:], ALU.mult, [cTf, cTe], [cTs])
        for nb in range(24):
            wt = wtm.next()
            DMA("pool", wt[:], w_mod[:, nb * 512:(nb + 1) * 512].rearrange("(kc p) n -> p kc n", p=128), [], [wt])
            ps = psr.next()
            for kc in range(16):
                MM(ps[0:2, :], cTs[:, kc, :], wt[:, kc, :], kc == 0, kc == 15, [cTs, wt], [ps])
            TT("dve", mrow[0:2, nb * 512:(nb + 1) * 512], ps[0:2, :], bm[0:2, nb * 512:(nb + 1) * 512], ALU.add,
               [ps, bm], [mrow])
        DMA("sp", mod_s[:], mrow[0:2, :], [mrow], [mod_s])
        P.barrier()
        if stop_after == 0:
            P.emit()
            return nc

        def mod_tile(dst, tmp1, tmp2, row, chunk, gain_ap):
            DMA("sp", tmp1[:], mod_s[row, chunk * D:(chunk + 1) * D].partition_broadcast(128), [mod_s], [tmp1])
            DMA("sp", tmp2[:], gain_ap.partition_broadcast(128), [], [tmp2])
            STT(dst[:], tmp1[:], 1.0, tmp2[:], ALU.add, ALU.mult, [tmp1, tmp2], [dst])

        def shift_tile(dst, row, chunk):
            DMA("sp", dst[:], mod_s[row, chunk * D:(chunk + 1) * D].partition_broadcast(128), [mod_s], [dst])

        STEPS = int(os.environ.get("P1A_STEPS", "9"))

        def norm_mod_tile(xt, Bsc_, Bsh_, hb, junk, ss, rs, tmp, n=D):
            if STEPS >= 2:
                ACT(junk[:], xt[:], AF.Square, [xt], [junk, ss], accum_out=ss[:])
            if STEPS >= 3:
                rstd_ops(ss, ss[:], rs, rs[:], n)
            if STEPS >= 4:
                STT(tmp[:], xt[:], rs[:, 0:1], Bsc_[:], ALU.mult, ALU.mult, [xt, rs, Bsc_], [tmp])
            if STEPS >= 5:
                TT(POOL_ENG, hb[:], tmp[:], Bsh_[:], ALU.add, [tmp, Bsh_], [hb])

        dbg_t = sbt('dbg_t', [128, 128], BF16)

        def transpose_tile(src, dstT, col0, nch=16):
            for g in range(0, nch, 8):
                pt = psTr.next()
                for j in range(8):
                    c = g + j
                    TR(pt[:, j, :], src[:, c * 128:(c + 1) * 128], ident_b, [src, cst_b, identb_t], [pt])
                CP(evac_eng(), dstT[:, g:g + 8, col0:col0 + 128], pt[:], [pt], [dstT])

        A.reset()
        hT = A.alloc([16, NA], BF16, "hT")
        Bsc = A.alloc([D], F32, "Bsc")
        Bsh = A.alloc([D], F32, "Bsh")
        Bscc = A.alloc([D], F32, "Bscc")
        Bshc = A.alloc([D], F32, "Bshc")
        mark1 = A.off
        t1 = A.alloc([D], F32, "t1")
        t2 = A.alloc([D], F32, "t2")
        mod_tile(Bsc, t1, t2, 0, 1, norm1_g[0, :])
        shift_tile(Bsh, 0, 0)
        t1b = A.alloc([D], F32, "t1b")
        t2b = A.alloc([D], F32, "t2b")
        mod_tile(Bscc, t1b, t2b, 1, 1, norm1_g[0, :])
        shift_tile(Bshc, 1, 0)
        P.barrier()
        if stop_after == 10:
            P.emit()
            return nc
        A.off = mark1
        mark_u = A.off
        xtr = A.ring(2, [D], F32, "xt")
        tmpr = A.ring(1, [D], F32, "tmp")
        hbr = A.ring(2, [D], BF16, "hb")
        junk = A.alloc([D], BF16, "junk")
        ssr = A.ring(2, [1], F32, "ss")
        rsr = A.ring(2, [1], F32, "rs")
        A.off = mark_u
        wtr = A.ring(2, [16, 512], BF16, "wt")
        stT = A.ring(2, [NA], F32, "stT")
        stN = A.ring(2, [9, 512], BF16, "stN")
        raw4 = A.alloc([4, 512], F32, "raw4")
        sq4 = A.ring(2, [512], BF16, "sq")
        sgr = A.ring(2, [512], F32, "sg")
        rbc = A.alloc([512], F32, "rbc")
        gcol_q = A.alloc([4], F32, "gcq")
        gcol_kv = A.alloc([4], F32, "gckv")

        def p1a(x_d, ntok, nctx_tiles):
            for t in range(min(ntok // 128, int(os.environ.get('P1A_TILES', '99')))):
                xt = xtr.next()
                DMA("sp", xt[:], x_d[t * 128:(t + 1) * 128, :], [], [xt])
                hb = hbr.next()
                isc = t < nctx_tiles
                norm_mod_tile(xt, Bscc if isc else Bsc, Bshc if isc else Bsh, hb, junk, ssr.next(), rsr.next(), tmpr.next())
                if STEPS >= 6:
                    transpose_tile(hb, hT, t * 128)

        def load_w(src2d, ncols):
            wt = wtr.next()
            DMA("pool", wt[:, :, 0:ncols], src2d.rearrange("(kc p) n -> p kc n", p=128), [], [wt])
            return wt

        def mm_T(wt, m0, mc, g0, gn):
            ps = psr.next()
            for kc in range(16):
                MM(ps[0:mc, 0:gn], wt[:, kc, m0:m0 + mc], hT[:, kc, g0:g0 + gn], kc == 0, kc == 15, [wt, hT], [ps])
            return ps

        def mm_N(wt, ncols, t):
            ps = psr.next()
            for kc in range(16):
                MM(ps[:, 0:ncols], hT[:, kc, t * 128:(t + 1) * 128], wt[:, kc, 0:ncols], kc == 0, kc == 15, [wt, hT], [ps])
            return ps

        def groups(ntok):
            return [(g0, min(512, ntok - g0)) for g0 in range(0, ntok, 512)]

        def job_T(wsrc, ncols, ntok, dst, dst_row0, dst_col0, dt, func=None, scale=1.0):
            wt = load_w(wsrc, ncols)
            for m0 in range(0, ncols, 128):
                mc = min(128, ncols - m0)
                stg = stT.next()
                sap = stg[:] if dt == F32 else stg[:].bitcast(BF16)
                for (g0, gn) in groups(ntok):
                    ps = mm_T(wt, m0, mc, g0, gn)
                    if func is not None:
                        ACT(sap[0:mc, g0:g0 + gn], ps[0:mc, 0:gn], func, [ps], [stg], scale=scale)
                    elif scale != 1.0:
                        P_mul(sap[0:mc, g0:g0 + gn], ps[0:mc, 0:gn], scale, [ps], [stg])
                    else:
                        CP(evac_eng(), sap[0:mc, g0:g0 + gn], ps[0:mc, 0:gn], [ps], [stg])
                DMA("sp", dst[dst_row0 + m0:dst_row0 + m0 + mc, dst_col0:dst_col0 + ntok], sap[0:mc, 0:ntok], [stg], [dst])
            return wt

        def P_mul(out_ap, in_ap, scale, reads, writes):
            P.op("act", lambda e: e.mul(out=out_ap, in_=in_ap, mul=scale), reads=reads, writes=writes)

        def job_N(wsrc, ncols, ntok, dst, dst_col0, silu=False, wt=None):
            if wt is None:
                wt = load_w(wsrc, ncols)
            nt = ntok // 128
            for t0 in range(0, nt, 9):
                tn = min(9, nt - t0)
                stg = stN.next()
                for tt in range(tn):
                    t = t0 + tt
                    ps = mm_N(wt, ncols, t)
                    if silu:
                        sg = sgr.next()
                        ACT(sg[:, 0:ncols], ps[:, 0:ncols], AF.Sigmoid, [ps], [sg])
                        TT("dve", stg[:, tt, 0:ncols], ps[:, 0:ncols], sg[:, 0:ncols], ALU.mult, [ps, sg], [stg])
                    else:
                        CP(evac_eng(), stg[:, tt, 0:ncols], ps[:, 0:ncols], [ps], [stg])
                DMA("sp", dst[t0 * 128:(t0 + tn) * 128, dst_col0:dst_col0 + ncols].rearrange("(t p) c -> p t c", p=128),
                    stg[:, 0:tn, 0:ncols], [stg], [dst])

        def job_Tnorm(wsrc, ntok, gcol, dst, dst_col0):
            wt = load_w(wsrc, 512)
            stgs = [stT.next(), stT.next()]
            for (g0, gn) in groups(ntok):
                for m in range(4):
                    ps = mm_T(wt, m * 128, 128, g0, gn)
                    sq = sq4.next()
                    CP("dve", raw4[:, m, 0:gn], ps[:, 0:gn], [ps], [raw4])
                    ACT(sq[:, 0:gn], raw4[:, m, 0:gn], AF.Square, [raw4], [sq])
                    MM(psS[:, 0:gn], cst_b[:, ONES, :], sq[:, 0:gn], m == 0, m == 3, [sq, cst_b], [psS])
                rstd_ops(psS, psS[:, 0:gn], rbc, rbc[:, 0:gn], 512)
                for m in range(4):
                    stg = stgs[m // 2]
                    sap = stg[:].bitcast(BF16)
                    off = (m % 2) * NA
                    STT(sap[:, off + g0:off + g0 + gn], raw4[:, m, 0:gn], gcol[:, m:m + 1], rbc[:, 0:gn],
                        ALU.mult, ALU.mult, [raw4, gcol, rbc], [stg])
            for m in range(4):
                stg = stgs[m // 2]
                sap = stg[:].bitcast(BF16)
                off = (m % 2) * NA
                DMA("sp", dst[m * 128:(m + 1) * 128, dst_col0:dst_col0 + ntok], sap[:, off:off + ntok], [stg], [dst])

        O_Q, O_K, O_V, O_R, O_CQ, O_CKV, O_KR, O_GA, O_GB = 0, 1024, 2048, 4096, 6176, 6688, 7200, 7264, 9312

        def load_gcols():
            DMA("sp", gcol_q[:], qa_g, [], [gcol_q])
            DMA("sp", gcol_kv[:], kva_g, [], [gcol_kv])

        p1a(x_A, NA, 2)
        P.barrier()
        if stop_after == 11:
            P.emit()
            return nc
        load_gcols()
        for b in range(2):
            job_N(w_in[:, O_K + b * 512:O_K + (b + 1) * 512], 512, NA, kA_s, b * 512)
        for b in range(4):
            job_N(w_in[:, O_V + b * 512:O_V + (b + 1) * 512], 512, NA, vA_s, b * 512)
        if stop_after == 12:
            P.emit()
            return nc
        job_T(w_a12[:, 0:16], 16, NA, aA_s, 0, 0, F32)
        if stop_after == 13:
            P.emit()
            return nc
        job_Tnorm(w_in[:, O_CKV:O_CKV + 512], NA, gcol_kv, ckvnT_s, 0)
        if stop_after == 14:
            P.emit()
            return nc
        job_T(w_in[:, O_KR:O_KR + 64], 64, NA, krT_s, 0, 0, F32)
        if stop_after == 15:
            P.emit()
            return nc
        P.barrier()
        p1a(x_B, NB, 2)
        P.barrier()
        load_gcols()
        for b in range(2):
            job_N(w_in[:, O_K + b * 512:O_K + (b + 1) * 512], 512, NB, kB_s, b * 512)
        for b in range(4):
            job_N(w_in[:, O_V + b * 512:O_V + (b + 1) * 512], 512, NB, vB_s, b * 512)
        job_T(w_a12[:, 16:32], 16, NB, aB_s, 0, 0, F32)
        P.barrier()
        p1a(x_loc, NLOC, 0)
        P.barrier()
        load_gcols()
        for b in range(2):
            job_T(w_in[:, O_Q + b * 512:O_Q + (b + 1) * 512], 512, NLOC, qT_s, b * 512, 0, BF16, scale=1.0 / 16.0)
        for b in range(2):
            wtk = job_T(w_in[:, O_K + b * 512:O_K + (b + 1) * 512], 512, NLOC, kT_s, b * 512, 0, BF16)
            job_N(None, 512, NLOC, k_s, b * 512, wt=wtk)
        for b in range(4):
            job_N(w_in[:, O_V + b * 512:O_V + (b + 1) * 512], 512, NLOC, v_s, b * 512)
        for b in range(4):
            job_N(w_in[:, O_R + b * 512:O_R + (b + 1) * 512], 512, NLOC, r_s, b * 512, silu=True)
        job_T(w_a12, 32, NLOC, aT_s, 0, 0, F32)
        job_Tnorm(w_in[:, O_CQ:O_CQ + 512], NLOC, gcol_q, cqnT_s, 0)
        job_Tnorm(w_in[:, O_CKV:O_CKV + 512], NLOC, gcol_kv, ckvnT_s, NA)
        job_T(w_in[:, O_KR:O_KR + 64], 64, NLOC, krT_s, 0, NA, F32)
        for b in range(4):
            job_T(w_in[:, O_GA + b * 512:O_GA + (b + 1) * 512], 512, NLOC, gaT_s, b * 512, 0, F32, func=AF.Sigmoid)
        for b in range(4):
            job_T(w_in[:, O_GB + b * 512:O_GB + (b + 1) * 512], 512, NLOC, gbT_s, b * 512, 0, F32, func=AF.Sigmoid)
        P.barrier()
        if stop_after == 1:
            P.emit()
            return nc
        A.reset()
        NKT = NKEY // 128
        ckvnT = A.alloc([4, NKEY], BF16, "ckvnT")
        cqnT = A.alloc([4, NLOC], BF16, "cqnT")
        KrT = A.alloc([NKEY], BF16, "KrT")
        krss = A.alloc([NKT], F32, "krss")
        gcols = A.alloc([4], F32, "gcols")
        RTf = A.alloc([64], F32, "RTf")
        RTb = A.alloc([64], BF16, "RTb")
        wk = A.alloc([4, 256], BF16, "wk")
        wv = A.alloc([4, 256], BF16, "wv")
        wqn = A.alloc([4, 256], BF16, "wqn")
        wqr = A.alloc([4, 128], BF16, "wqr")
        KT = A.alloc([2, NKEY], BF16, "KT")
        Vt = A.alloc([NKT, 256], BF16, "V")
        kscale = A.alloc([2, NKT], F32, "kscale")
        QTn = A.alloc([2, NLOC], BF16, "QTn")
        QTr = A.alloc([2, NLOC], BF16, "QTr")
        rawn = A.alloc([512], F32, "rawn")
        rawr = A.alloc([512], F32, "rawr")
        sqn = A.alloc([512], BF16, "sqn")
        sqr = A.alloc([512], BF16, "sqr")
        rbc2 = A.alloc([512], F32, "rbc2")
        qrg = A.alloc([512], BF16, "qrg")
        tA = A.alloc([512], F32, "tA")
        tB = A.alloc([512], F32, "tB")
        cosc = A.alloc([512], F32, "cosc")
        sinc = A.alloc([512], F32, "sinc")
        krf = A.alloc([512], F32, "krf")
        tmpk = A.alloc([8], F32, "tmpk")
        pTr = A.ring(3, [512], BF16, "pT")
        recip = A.alloc([512], F32, "recip")
        racc = A.alloc([512], F32, "racc")
        rhi = A.alloc([512], BF16, "rhi")
        rlo = A.alloc([512], BF16, "rlo")
        ystg = A.ring(2, [NLOC], BF16, "ystg")
        pss4 = Ring(psb[0:4])
        po = psb[4]
        ones_b = cst_b[:, ONES, :]

        DMA("sp", ckvnT[:], ckvnT_s[:].rearrange("(rc p) n -> p rc n", p=128), [ckvnT_s], [ckvnT])
        DMA("sp", cqnT[:], cqnT_s[:].rearrange("(rc p) n -> p rc n", p=128), [cqnT_s], [cqnT])
        DMA("sp", gcols[:, 0:1], kn_g[0:128, :], [], [gcols])
        DMA("sp", gcols[0:64, 1:2], kn_g[128:192, :], [], [gcols])
        DMA("sp", gcols[:, 2:3], qn_g[0:128, :], [], [gcols])
        DMA("sp", gcols[0:64, 3:4], qn_g[128:192, :], [], [gcols])
        DMA("sp", RTf[0:64, :], rt_c, [], [RTf])
        CP("dve", RTb[0:64, :], RTf[0:64, :], [RTf], [RTb])

        def kchunks():
            return [(c0, min(512, NKEY - c0)) for c0 in range(0, NKEY, 512)]

        def rope_apply(dst_ap, dst_t, xg_bf, xg_t, cos_ap, sin_ap, tabs, n):
            ps2 = pss4.next()
            MM(ps2[0:64, 0:n], RTb[0:64, :], xg_bf, True, True, [RTb, xg_t], [ps2])
            TT("dve", tA[0:64, 0:n], xg_bf, cos_ap, ALU.mult, [xg_t] + tabs, [tA])
            TT("dve", tB[0:64, 0:n], ps2[0:64, 0:n], sin_ap, ALU.mult, [ps2] + tabs, [tB])
            TT("pool", dst_ap, tA[0:64, 0:n], tB[0:64, 0:n], ALU.add, [tA, tB], [dst_t])

        for (c0, cn) in kchunks():
            kt0, nt = c0 // 128, cn // 128
            DMA("sp", krf[0:64, 0:cn], krT_s[:, c0:c0 + cn], [krT_s], [krf])
            DMA("sp", cosc[0:64, 0:cn], cosk[:, c0:c0 + cn], [], [cosc])
            DMA("sp", sinc[0:64, 0:cn], sink[:, c0:c0 + cn], [], [sinc])
            ACT(sqr[0:64, 0:cn], krf[0:64, 0:cn], AF.Square, [krf], [sqr])
            for j in range(nt):
                MM(psS[:, j:j + 1], sqr[0:64, j * 128:(j + 1) * 128], ones_b[0:64, 0:1], True, True, [sqr, cst_b], [psS])
            CP("dve", krss[:, kt0:kt0 + nt], psS[:, 0:nt], [psS], [krss])
            TS("dve", qrg[0:64, 0:cn], krf[0:64, 0:cn], gcols[0:64, 1:2], None, ALU.mult, None, [krf, gcols], [qrg])
            rope_apply(KrT[0:64, c0:c0 + cn], KrT, qrg[0:64, 0:cn], qrg, cosc[0:64, 0:cn], sinc[0:64, 0:cn], [cosc, sinc], cn)

        zrow2 = A.alloc([D], BF16, "zrow2")
        zrowf = A.alloc([D], F32, "zrowf")
        P.op("pool", lambda e: e.memset(zrow2[:], 0.0), reads=[], writes=[zrow2])
        P.op("pool", lambda e: e.memset(zrowf[:], 0.0), reads=[], writes=[zrowf])
        SCL = 192.0 ** -0.5
        NGRP = int(os.environ.get("MLA_GROUPS", "8"))
        for g in range(NGRP):
            DMA("pool", wk[:], w_ukv_k[:, g * 256:(g + 1) * 256].rearrange("(rc p) n -> p rc n", p=128), [], [wk])
            DMA("pool", wv[:], w_ukv_v[:, g * 256:(g + 1) * 256].rearrange("(rc p) n -> p rc n", p=128), [], [wv])
            DMA("pool", wqn[:], w_uq_n[:, g * 256:(g + 1) * 256].rearrange("(rc p) n -> p rc n", p=128), [], [wqn])
            DMA("pool", wqr[:], w_uq_r[:, g * 128:(g + 1) * 128].rearrange("(rc p) n -> p rc n", p=128), [], [wqr])
            if g == 0:
                for e_ in range((NEXP * CAP) // 128 + 1):
                    DMA("pool", xdisp[e_ * 128:(e_ + 1) * 128, :], zrow2[:], [zrow2], [])
                DMA("pool", ydisp[NEXP * CAP:NEXP * CAP + 128, :], zrowf[:], [zrowf], [])
            for (c0, cn) in kchunks():
                kt0, nt = c0 // 128, cn // 128
                for hl in range(2):
                    ps = pss4.next()
                    for rc in range(4):
                        MM(ps[:, 0:cn], wk[:, rc, hl * 128:(hl + 1) * 128], ckvnT[:, rc, c0:c0 + cn], rc == 0, rc == 3,
                           [wk, ckvnT], [ps])
                    TS("dve", KT[:, hl, c0:c0 + cn], ps[:, 0:cn], gcols[:, 0:1], None, ALU.mult, None, [ps, gcols], [KT])
                    ACT(sqn[:, 0:cn], ps[:, 0:cn], AF.Square, [ps], [sqn])
                    for j in range(nt):
                        MM(psS[:, hl * 4 + j:hl * 4 + j + 1], sqn[:, j * 128:(j + 1) * 128], ones_b[:, 0:1], True, True,
                           [sqn, cst_b], [psS])
                for hl in range(2):
                    TT("dve", tmpk[:, 0:nt], psS[:, hl * 4:hl * 4 + nt], krss[:, kt0:kt0 + nt], ALU.add, [psS, krss], [tmpk])
                    rstd_ops(tmpk, tmpk[:, 0:nt], kscale, kscale[:, hl, kt0:kt0 + nt], 192, extra=SCL)
            for kt in range(NKT):
                ps = pss4.next()
                for rc in range(4):
                    MM(ps[:, 0:256], ckvnT[:, rc, kt * 128:(kt + 1) * 128], wv[:, rc, :], rc == 0, rc == 3, [wv, ckvnT], [ps])
                CP(evac_eng(), Vt[:, kt, :], ps[:, 0:256], [ps], [Vt])
            for qc in range(4):
                q0 = qc * 512
                DMA("sp", cosc[0:64, :], cosq[:, q0:q0 + 512], [], [cosc])
                DMA("sp", sinc[0:64, :], sinq[:, q0:q0 + 512], [], [sinc])
                for hl in range(2):
                    psn = pss4.next()
                    for rc in range(4):
                        MM(psn[:, :], wqn[:, rc, hl * 128:(hl + 1) * 128], cqnT[:, rc, q0:q0 + 512], rc == 0, rc == 3,
                           [wqn, cqnT], [psn])
                    psq = pss4.next()
                    for rc in range(4):
                        MM(psq[0:64, :], wqr[:, rc, hl * 64:(hl + 1) * 64], cqnT[:, rc, q0:q0 + 512], rc == 0, rc == 3,
                           [wqr, cqnT], [psq])
                    CP("dve", rawn[:], psn[:, :], [psn], [rawn])
                    CP("act", rawr[0:64, :], psq[0:64, :], [psq], [rawr])
                    ACT(sqn[:], rawn[:], AF.Square, [rawn], [sqn])
                    ACT(sqr[0:64, :], rawr[0:64, :], AF.Square, [rawr], [sqr])
                    MM(psS[:, :], ones_b, sqn[:], True, False, [sqn, cst_b], [psS])
                    MM(psS[:, :], cst_b[0:64, ONES, :], sqr[0:64, :], False, True, [sqr, cst_b], [psS])
                    rstd_ops(psS, psS[:, :], rbc2, rbc2[:], 192)
                    STT(QTn[:, hl, q0:q0 + 512], rawn[:], gcols[:, 2:3], rbc2[:], ALU.mult, ALU.mult, [rawn, gcols, rbc2], [QTn])
                    STT(qrg[0:64, :], rawr[0:64, :], gcols[0:64, 3:4], rbc2[0:64, :], ALU.mult, ALU.mult,
                        [rawr, gcols, rbc2], [qrg])
                    rope_apply(QTr[0:64, hl, q0:q0 + 512], QTr, qrg[0:64, :], qrg, cosc[0:64, :], sinc[0:64, :], [cosc, sinc], 512)
            for hl in range(0 if os.environ.get('MLA_NOATT') else 2):
                h = 2 * g + hl
                stg = ystg.next()
                for qc in range(4):
                    q0 = qc * 512
                    def qk(kt, hl=hl, q0=q0):
                        pss = pss4.next()
                        MM(pss[:, :], KT[:, hl, kt * 128:(kt + 1) * 128], QTn[:, hl, q0:q0 + 512], True, False, [KT, QTn], [pss])
                        MM(pss[:, :], KrT[0:64, kt * 128:(kt + 1) * 128], QTr[0:64, hl, q0:q0 + 512], False, True,
                           [KrT, QTr], [pss])
                        return pss
                    LA = 2
                    pend = [qk(i) for i in range(LA)]
                    for kt in range(NKT):
                        pss = pend.pop(0)
                        if kt + LA < NKT:
                            pend.append(qk(kt + LA))
                        pT = pTr.next()
                        ACT(pT[:], pss[:, :], AF.Exp, [pss, kscale], [pT], scale=kscale[:, hl, kt:kt + 1])
                        MM(po[:, :], Vt[:, kt, hl * 128:(hl + 1) * 128], pT[:], kt == 0, kt == NKT - 1, [Vt, pT], [po])
                        MM(psS[:, :], ones_b, pT[:], kt == 0, kt == NKT - 1, [pT, cst_b], [psS])
                    P.op("dve", lambda e: e.reciprocal(out=recip[:], in_=psS[:, :]), reads=[psS], writes=[recip])
                    TT("dve", stg[:, q0:q0 + 512], po[:, :], recip[:], ALU.mult, [po, recip], [stg])
                DMA("sp", ymlaT_s[h * 128:(h + 1) * 128, :], stg[:], [stg], [ymlaT_s])
        P.barrier()
        if stop_after == 2:
            P.emit()
            return nc
        A.reset()
        S = A.alloc([4, 2, 512], F32, "S")
        Sb = A.alloc([4, 2, 512], BF16, "Sb")
        wdf = A.alloc([1024], F32, "wdf")
        wdh = A.alloc([1024], BF16, "wdh")
        wdl = A.alloc([1024], BF16, "wdl")
        wdt = A.alloc([1024], F32, "wdt")
        glg = A.alloc([512], F32, "glg")
        aTr = A.ring(2, [128], F32, "aT")
        ahr = A.ring(2, [128], BF16, "ah")
        alr = A.ring(2, [128], BF16, "al")
        att_ = A.alloc([128], F32, "att_")
        gex = A.alloc([1024], F32, "gex")
        gtm = A.alloc([1024], F32, "gtm")
        ghi = A.alloc([1024], BF16, "ghi")
        glo = A.alloc([1024], BF16, "glo")
        gt2 = A.alloc([1024], F32, "gt2")
        qTr = A.ring(2, [8, 128], BF16, "qTt")
        kTr = A.ring(2, [8, 128], BF16, "kTt")
        ktr = A.ring(2, [1024], BF16, "kt")
        vtr = A.ring(2, [2048], BF16, "vt")
        ekt = A.alloc([256], F32, "ekt")
        kend = A.ring(2, [256], BF16, "kend")
        eb = A.ring(2, [256], F32, "eb")
        enb = A.ring(2, [256], F32, "enb")
        qdec = A.ring(2, [2, 128], BF16, "qdec")
        kinv = A.ring(2, [2, 128], BF16, "kinv")
        attm = A.ring(2, [128], BF16, "attm")
        de2 = A.ring(2, [2], F32, "de2")
        ofst = A.ring(2, [2048], F32, "ofst")
        rtl = A.ring(2, [2048], BF16, "rt")
        ssg = A.ring(2, [4], F32, "ssg")
        rsg = A.ring(2, [4], F32, "rsg")
        junkg = A.alloc([512], BF16, "junkg")
        ytmp = A.alloc([512], F32, "ytmp")
        ybf = A.ring(2, [2048], BF16, "ybf")
        yTs = A.ring(2, [16, 128], BF16, "yTs")

        DMA("sp", glg[:], gla_g[0, :].partition_broadcast(128), [], [glg])

        def MEMSET(eng, ap, val, writes):
            P.op(eng, lambda e: e.memset(ap, val), reads=[], writes=writes)

        def hilo(eng, src_ap, hi_ap, lo_ap, tmp_ap, src_t, hi_t, lo_t, tmp_t):
            CP(eng, hi_ap, src_ap, [src_t], [hi_t])
            TT(eng, tmp_ap, src_ap, hi_ap, ALU.subtract, [src_t, hi_t], [tmp_t])
            CP(eng, lo_ap, tmp_ap, [tmp_t], [lo_t])

        def load_wd(wd_d):
            DMA("sp", wdf[0:17, :], wd_d, [], [wdf])
            hilo("dve", wdf[0:17, :], wdh[0:17, :], wdl[0:17, :], wdt[0:17, :], wdf, wdh, wdl, wdt)

        def load_a(a_src_ap, a_src_t):
            aT = aTr.next()
            MEMSET("pool", aT[0:32, :], 1.0, [aT])
            DMA("sp", aT[0:16, :], a_src_ap, [a_src_t], [aT])
            return aT

        def gates(aT):
            ah = ahr.next()
            al = alr.next()
            hilo("pool", aT[0:32, :], ah[0:32, :], al[0:32, :], att_[0:32, :], aT, ah, al, att_)
            for half in range(2):
                ps = pss4.next()
                cs = slice(half * 512, (half + 1) * 512)
                MM(ps[:, :], ah[0:17, :], wdh[0:17, cs], True, False, [ah, wdh], [ps])
                MM(ps[:, :], ah[0:17, :], wdl[0:17, cs], False, False, [ah, wdl], [ps])
                MM(ps[:, :], al[0:17, :], wdh[0:17, cs], False, True, [al, wdh], [ps])
                ACT(gex[:, cs], ps[:, :], AF.Exp, [ps], [gex], scale=-1.0)
            ACT(gtm[:], gex[:], AF.Ln, [gex], [gtm], bias=1.0)
            TS("dve", gtm[:], gtm[:], -1.0 / 16.0, None, ALU.mult, None, [gtm], [gtm])
            hilo("dve", gtm[:], ghi[:], glo[:], gt2[:], gtm, ghi, glo, gt2)

        def mm_hl(ps_ap, ps_t, lhs_fn, rhs_fn):
            for i, gx in enumerate((ghi, glo)):
                MM(ps_ap, lhs_fn(gx), rhs_fn(gx), i == 0, i == 1, [gx, cst_b], [ps_t])

        def kend_for(h, kt_tile, EM):
            ps = pss4.next()
            mm_hl(ps[:, 0:256], ps, lambda gx: cst_b[:, EM, :], lambda gx: gx[:, h * 256:(h + 1) * 256])
            ACT(ekt[:], ps[:, 0:256], AF.Exp, [ps], [ekt])
            ke = kend.next()
            TT("dve", ke[:], kt_tile[:, h * 256:(h + 1) * 256], ekt[:], ALU.mult, [kt_tile, ekt], [ke])
            return ke

        def state_update(h, ke, vt, de_ap, de_t):
            for dkc in range(2):
                ps = pss4.next()
                MM(ps[:, :], ke[:, dkc * 128:(dkc + 1) * 128], vt[:, h * 512:(h + 1) * 512], True, True, [ke, vt], [ps])
                STT(S[:, h, dkc, :], S[:, h, dkc, :], de_ap(dkc), ps[:, :], ALU.mult, ALU.add, [S, de_t, ps], [S])
            CP("act", Sb[:, h, :, :], S[:, h, :, :], [S], [Sb])

        def state_pass(k_d, v_d, a_d, ntok):
            def loads(t):
                kt_tile = ktr.next()
                vt = vtr.next()
                DMA("sp", kt_tile[:], k_d[t * 128:(t + 1) * 128, :], [k_d], [kt_tile])
                DMA("sp", vt[:], v_d[t * 128:(t + 1) * 128, :], [v_d], [vt])
                aT = load_a(a_d[0:16, t * 128:(t + 1) * 128], a_d)
                return kt_tile, vt, aT
            nt_ = ntok // 128
            nxt = loads(0)
            for t in range(nt_):
                kt_tile, vt, aT = nxt
                if t + 1 < nt_:
                    nxt = loads(t + 1)
                gates(aT)
                for h in range(4):
                    ke = kend_for(h, kt_tile, SU)
                    d2 = de2.next()
                    ps = pss4.next()
                    for dkc in range(2):
                        c0 = h * 256 + dkc * 128
                        mm_hl(ps[:, dkc:dkc + 1], ps, lambda gx, c0=c0: gx[:, c0:c0 + 128], lambda gx: cst_b[:, ONES, 0:1])
                    ACT(d2[:], ps[:, 0:2], AF.Exp, [ps], [d2])
                    state_update(h, ke, vt, lambda dkc, d2=d2: d2[:, dkc:dkc + 1], d2)

        def local_pass(direction):
            EM, CM = (SU, UI) if direction == 0 else (SL, LI)
            decol = 127 if direction == 0 else 0
            a_row0 = 0 if direction == 0 else 16
            tiles = range(NLOC // 128) if direction == 0 else range(NLOC // 128 - 1, -1, -1)
            def loads(t):
                ts_ = slice(t * 128, (t + 1) * 128)
                qTt = qTr.next()
                kTt = kTr.next()
                kt_tile = ktr.next()
                vt = vtr.next()
                DMA("sp", qTt[:], qT_s[:, ts_].rearrange("(c p) t -> p c t", p=128), [qT_s], [qTt])
                DMA("sp", kTt[:], kT_s[:, ts_].rearrange("(c p) t -> p c t", p=128), [kT_s], [kTt])
                DMA("sp", kt_tile[:], k_s[ts_, :], [k_s], [kt_tile])
                DMA("sp", vt[:], v_s[ts_, :], [v_s], [vt])
                aT = load_a(aT_s[a_row0:a_row0 + 16, ts_], aT_s)
                of = ofst.next()
                rt = None
                if direction == 1:
                    DMA("sp", of[:], of_s[ts_, :], [of_s], [of])
                    rt = rtl.next()
                    DMA("sp", rt[:], r_s[ts_, :], [r_s], [rt])
                return qTt, kTt, kt_tile, vt, aT, of, rt
            tiles = list(tiles)
            nxt = loads(tiles[0])
            for ti, t in enumerate(tiles):
                ts_ = slice(t * 128, (t + 1) * 128)
                qTt, kTt, kt_tile, vt, aT, of, rt = nxt
                if ti + 1 < len(tiles):
                    nxt = loads(tiles[ti + 1])
                gates(aT)
                if direction == 1:
                    ss = ssg.next()
                    rs = rsg.next()
                for h in range(4):
                    ke = kend_for(h, kt_tile, EM)
                    psb_ = pss4.next()
                    for dkc in range(2):
                        c0 = h * 256 + dkc * 128
                        mm_hl(psb_[:, dkc * 128:(dkc + 1) * 128], psb_, lambda gx, c0=c0: gx[:, c0:c0 + 128],
                              lambda gx: cst_b[:, CM, :])
                    e1 = eb.next()
                    e2 = enb.next()
                    ACT(e1[:], psb_[:, 0:256], AF.Exp, [psb_], [e1])
                    ACT(e2[:], psb_[:, 0:256], AF.Exp, [psb_], [e2], scale=-1.0)
                    qd = qdec.next()
                    ki = kinv.next()
                    TT("dve", qd[:], qTt[:, 2 * h:2 * h + 2, :], e1[:].rearrange("p (a b) -> p a b", a=2), ALU.mult,
                       [qTt, e1], [qd])
                    TT("pool", ki[:], kTt[:, 2 * h:2 * h + 2, :], e2[:].rearrange("p (a b) -> p a b", a=2), ALU.mult,
                       [kTt, e2], [ki])
                    psa = pss4.next()
                    for dkc in range(2):
                        MM(psa[:, 0:128], ki[:, dkc, :], qd[:, dkc, :], dkc == 0, dkc == 1, [ki, qd], [psa])
                    am = attm.next()
                    TT("dve", am[:], psa[:, 0:128], cst_f[:, CM, :], ALU.mult, [psa, cst_f], [am])
                    MM(po[:, :], am[:], vt[:, h * 512:(h + 1) * 512], True, False, [am, vt], [po])
                    for dkc in range(2):
                        MM(po[:, :], qd[:, dkc, :], Sb[:, h, dkc, :], False, dkc == 1, [qd, Sb], [po])
                    if direction == 0:
                        CP("act", of[:, h * 512:(h + 1) * 512], po[:, :], [po], [of])
                    else:
                        TT("dve", of[:, h * 512:(h + 1) * 512], po[:, :], of[:, h * 512:(h + 1) * 512], ALU.add, [po, of], [of])
                        ACT(junkg[:], of[:, h * 512:(h + 1) * 512], AF.Square, [of], [junkg, ss], accum_out=ss[:, h:h + 1])
                    state_update(h, ke, vt, lambda dkc, e1=e1: e1[:, dkc * 128 + decol:dkc * 128 + decol + 1], e1)
                if direction == 0:
                    DMA("sp", of_s[ts_, :], of[:], [of], [of_s])
                else:
                    rstd_ops(ss, ss[:], rs, rs[:], 512)
                    yb = ybf.next()
                    for h in range(4):
                        hs = slice(h * 512, (h + 1) * 512)
                        STT(ytmp[:], of[:, hs], rs[:, h:h + 1], glg[:], ALU.mult, ALU.mult, [of, rs, glg], [ytmp])
                        TT("pool", yb[:, hs], ytmp[:], rt[:, hs], ALU.mult, [ytmp, rt], [yb])
                    yT = yTs.next()
                    transpose_tile(yb, yT, 0)
                    DMA("sp", yglaT_s[:, ts_].rearrange("(c p) t -> p c t", p=128), yT[:], [yT], [yglaT_s])

        def zero_state():
            MEMSET("dve", S[:], 0.0, [S])
            MEMSET("pool", Sb[:], 0.0, [Sb])

        load_wd(wd1)
        zero_state()
        state_pass(kA_s, vA_s, aA_s, NA)
        local_pass(0)
        load_wd(wd2)
        zero_state()
        state_pass(kB_s, vB_s, aB_s, NB)
        local_pass(1)
        P.barrier()
        if stop_after == 3:
            P.emit()
            return nc
        A.reset()
        Bgt1 = A.alloc([D], F32, "Bgt1")
        shift_tile(Bgt1, 0, 2)
        GT = 1024
        ygT = A.alloc([16, GT], BF16, "ygT")
        ymT = A.alloc([16, GT], BF16, "ymT")
        yT3 = A.alloc([16, GT], BF16, "yT3")
        w3r = A.ring(2, [16, 512], BF16, "w3")
        gar = A.ring(2, [4, 512], F32, "ga")
        gbr = A.ring(2, [4, 512], F32, "gb")
        t3a = A.ring(2, [512], F32, "t3a")
        t3b = A.ring(2, [512], F32, "t3b")
        x3r = A.ring(4, [512], F32, "x3")
        x3o = A.ring(2, [512], F32, "x3o")

        def load_w3(src2d):
            wt = w3r.next()
            DMA("pool", wt[:], src2d.rearrange("(kc p) n -> p kc n", p=128), [], [wt])
            return wt

        for g in range(NLOC // GT):
            gs = slice(g * GT, (g + 1) * GT)
            DMA("sp", ygT[:], yglaT_s[:, gs].rearrange("(c p) t -> p c t", p=128), [yglaT_s], [ygT])
            DMA("sp", ymT[:], ymlaT_s[:, gs].rearrange("(c p) t -> p c t", p=128), [ymlaT_s], [ymT])
            for mb in range(4):
                ms = slice(mb * 512, (mb + 1) * 512)
                Wg = load_w3(w_o_gla[:, ms])
                Wm = load_w3(w_o_mla[:, ms])
                for hf in range(GT // 512):
                    hs = slice(hf * 512, (hf + 1) * 512)
                    ts_ = slice(g * GT + hf * 512, g * GT + (hf + 1) * 512)
                    ga = gar.next()
                    gb = gbr.next()
                    DMA("sp", ga[:], gaT_s[ms, ts_].rearrange("(m p) t -> p m t", p=128), [gaT_s], [ga])
                    DMA("sp", gb[:], gbT_s[ms, ts_].rearrange("(m p) t -> p m t", p=128), [gbT_s], [gb])
                    for m in range(4):
                        ps1 = pss4.next()
                        for kc in range(16):
                            MM(ps1[:, :], Wg[:, kc, m * 128:(m + 1) * 128], ygT[:, kc, hs], kc == 0, kc == 15, [Wg, ygT], [ps1])
                        ps2 = pss4.next()
                        for kc in range(16):
                            MM(ps2[:, :], Wm[:, kc, m * 128:(m + 1) * 128], ymT[:, kc, hs], kc == 0, kc == 15, [Wm, ymT], [ps2])
                        ta = t3a.next()
                        tb = t3b.next()
                        TT("dve", ta[:], ps1[:, :], ga[:, m, :], ALU.mult, [ps1, ga], [ta])
                        TT("dve", tb[:], ps2[:, :], gb[:, m, :], ALU.mult, [ps2, gb], [tb])
                        TT("pool", yT3[:, mb * 4 + m, hs], ta[:], tb[:], ALU.add, [ta, tb], [yT3])
            for nb in range(4):
                ns = slice(nb * 512, (nb + 1) * 512)
                Wo = load_w3(w_out[:, ns])
                for t4 in range(0, GT // 128, 4):
                    xts = []
                    for tt in range(t4, t4 + 4):
                        rows = slice(g * GT + tt * 128, g * GT + (tt + 1) * 128)
                        xt = x3r.next()
                        DMA("sp", xt[:], x_loc[rows, ns], [], [xt])
                        xts.append(xt)
                    for tt in range(t4, t4 + 4):
                        rows = slice(g * GT + tt * 128, g * GT + (tt + 1) * 128)
                        xt = xts[tt - t4]
                        ps = pss4.next()
                        for kc in range(16):
                            MM(ps[:, :], yT3[:, kc, tt * 128:(tt + 1) * 128], Wo[:, kc, :], kc == 0, kc == 15, [yT3, Wo], [ps])
                        ta = t3a.next()
                        TT("dve", ta[:], ps[:, :], Bgt1[:, ns], ALU.mult, [ps, Bgt1], [ta])
                        xo = x3o.next()
                        TT("pool", xo[:], ta[:], xt[:], ALU.add, [ta, xt], [xo])
                        DMA("sp", x1_s[rows, ns], xo[:], [xo], [x1_s])
        P.barrier()
        if stop_after == 4:
            P.emit()
            return nc

        A.reset()
        NT = NLOC // 128
        Bgt2 = A.alloc([D], F32, "Bgt2")
        shift_tile(Bgt2, 0, 5)
        destI = A.alloc([NT, 2], I32, "destI")
        wts = A.alloc([NT, 2], F32, "wts")
        carry = A.alloc([64], F32, "carry")
        iot = A.alloc([64], F32, "iot")
        brt = A.alloc([72], F32, "brt")
        wrf = A.alloc([16, 72], F32, "wrf")
        wrh = A.alloc([16, 72], BF16, "wrh")
        wrl = A.alloc([16, 72], BF16, "wrl")
        wrt = A.alloc([16, 72], F32, "wrt")
        m4b = A.off
        Bsc2 = A.alloc([D], F32, "Bsc2")
        Bsh2 = A.alloc([D], F32, "Bsh2")
        m4 = A.off
        u1 = A.alloc([D], F32, "u1")
        u2 = A.alloc([D], F32, "u2")
        mod_tile(Bsc2, u1, u2, 0, 4, norm2_g[0, :])
        shift_tile(Bsh2, 0, 3)
        P.barrier()
        A.off = m4
        x1t = A.ring(2, [D], F32, "x1t")
        tmp4 = A.alloc([D], F32, "tmp4")
        h2f = A.alloc([D], F32, "h2f")
        h2h = A.ring(2, [D], BF16, "h2h")
        h2l = A.alloc([D], BF16, "h2l")
        h2p = A.ring(2, [D], BF16, "h2p")
        junk4 = A.alloc([D], BF16, "junk4")
        h2hT = A.alloc([16, 128], BF16, "h2hT")
        h2lT = A.alloc([16, 128], BF16, "h2lT")
        zrow = A.alloc([D], BF16, "zrow")
        sm = A.alloc([64], F32, "sm")
        lg = A.alloc([72], F32, "lg")
        ohG = A.alloc([8], F32, "ohG")
        eg8 = A.alloc([8], F32, "eg8")
        msk = A.alloc([8, 8], F32, "msk")
        le = A.alloc([8], F32, "le")
        le2 = A.alloc([8], F32, "le2")
        oh1 = A.alloc([8], F32, "oh1")
        oh2 = A.alloc([8], F32, "oh2")
        o64a = A.alloc([8, 8], F32, "o64a")
        o64b = A.alloc([8, 8], F32, "o64b")
        Cb = A.alloc([64], BF16, "Cb")
        Cf = A.alloc([64], F32, "Cf")
        pex = A.alloc([64], F32, "pex")
        t64 = A.alloc([64], F32, "t64")
        dstf = A.alloc([2], F32, "dstf")

        DMA("sp", iot[:], iota_c, [], [iot])
        DMA("sp", brt[:], b_rt[0, :].partition_broadcast(128), [], [brt])
        DMA("sp", wrf[:], w_rt.rearrange("(kc p) n -> p kc n", p=128), [], [wrf])
        hilo("dve", wrf[:], wrh[:], wrl[:], wrt[:], wrf, wrh, wrl, wrt)
        MEMSET("dve", carry[:], 0.0, [carry])

        def col(i):
            return sm[:, i:i + 1]

        def TSs(out_ap, in0, s1, op0, reads, writes, s2=None, op1=None):
            TS("dve", out_ap, in0, s1, s2, op0, op1, reads, writes)

        for t in range(NT):
            rows = slice(t * 128, (t + 1) * 128)
            xt = x1t.next()
            DMA("sp", xt[:], x1_s[rows, :], [x1_s], [xt])
            ss = ssr_4 = None
            ACT(junk4[:], xt[:], AF.Square, [xt], [junk4, sm], accum_out=col(0))
            rstd_ops(sm, col(0), sm, col(1), D)
            STT(tmp4[:], xt[:], col(1), Bsc2[:], ALU.mult, ALU.mult, [xt, sm, Bsc2], [tmp4])
            TT("pool", h2f[:], tmp4[:], Bsh2[:], ALU.add, [tmp4, Bsh2], [h2f])
            hh = h2h.next()
            CP("act", hh[:], h2f[:], [h2f], [hh])
            TT("dve", tmp4[:], h2f[:], hh[:], ALU.subtract, [h2f, hh], [tmp4])
            CP("pool", h2l[:], tmp4[:], [tmp4], [h2l])
            hp = h2p.next()
            CP("pool", hp[:].rearrange("s (kc p) -> s kc p", kc=16), hh[:].rearrange("s (p kc) -> s kc p", kc=16), [hh], [hp])
            transpose_tile(hh, h2hT, 0)
            transpose_tile(h2l, h2lT, 0)
            psl = pss4.next()
            for kc in range(16):
                MM(psl[:, 0:72], h2hT[:, kc, :], wrh[:, kc, :], kc == 0, False, [h2hT, wrh], [psl])
                MM(psl[:, 0:72], h2hT[:, kc, :], wrl[:, kc, :], False, False, [h2hT, wrl], [psl])
                MM(psl[:, 0:72], h2lT[:, kc, :], wrh[:, kc, :], False, kc == 15, [h2lT, wrh], [psl])
            TT("dve", lg[:], psl[:, 0:72], brt[:], ALU.add, [psl, brt], [lg])
            RED("dve", col(2), lg[:, 0:8], ALU.max, [lg], [sm])
            TSs(col(3), col(2), -1.0, ALU.mult, [sm], [sm])
            ACT(eg8[:], lg[:, 0:8], AF.Exp, [lg, sm], [eg8, sm], bias=col(3), accum_out=col(4))
            P.op("dve", lambda e: e.reciprocal(out=col(5), in_=col(4)), reads=[sm], writes=[sm])
            TSs(ohG[:], lg[:, 0:8], col(2), ALU.is_equal, [lg, sm], [ohG])
            lgE = lg[:, 8:72].rearrange("p (g e) -> p g e", g=8)
            TT("dve", msk[:], lgE, ohG[:].unsqueeze(2).to_broadcast([128, 8, 8]), ALU.mult, [lg, ohG], [msk])
            RED("dve", le[:], msk[:].rearrange("p g e -> p e g"), ALU.add, [msk], [le])
            RED("dve", col(6), le[:], ALU.max, [le], [sm])
            TSs(oh1[:], le[:], col(6), ALU.is_equal, [le, sm], [oh1])
            STT(le2[:], oh1[:], -1.0e30, le[:], ALU.mult, ALU.add, [oh1, le], [le2])
            RED("dve", col(7), le2[:], ALU.max, [le2], [sm])
            TSs(oh2[:], le2[:], col(7), ALU.is_equal, [le2, sm], [oh2])
            TSs(col(8), col(6), -1.0, ALU.mult, [sm], [sm])
            ACT(col(9), col(7), AF.Exp, [sm], [sm], bias=col(8))
            TSs(col(10), col(9), 1.0, ALU.add, [sm], [sm])
            P.op("dve", lambda e: e.reciprocal(out=col(10), in_=col(10)), reads=[sm], writes=[sm])
            TT("dve", wts[:, t, 0:1], col(5), col(10), ALU.mult, [sm], [wts])
            TT("dve", wts[:, t, 1:2], wts[:, t, 0:1], col(9), ALU.mult, [sm, wts], [wts])
            gB = ohG[:].unsqueeze(2).to_broadcast([128, 8, 8])
            TT("dve", o64a[:], oh1[:].unsqueeze(1).to_broadcast([128, 8, 8]), gB, ALU.mult, [oh1, ohG], [o64a])
            TT("dve", o64b[:], oh2[:].unsqueeze(1).to_broadcast([128, 8, 8]), gB, ALU.mult, [oh2, ohG], [o64b])
            a64 = o64a[:].rearrange("p g e -> p (g e)")
            b64 = o64b[:].rearrange("p g e -> p (g e)")
            TT("dve", Cf[:], a64, b64, ALU.add, [o64a, o64b], [Cf])
            CP("dve", Cb[:], Cf[:], [Cf], [Cb])
            psx = pss4.next()
            MM(psx[:, 0:64], cst_b[:, SL, :], Cb[:], True, True, [cst_b, Cb], [psx])
            TT("dve", pex[:], psx[:, 0:64], carry[:], ALU.add, [psx, carry], [pex])
            pst = pss4.next()
            MM(pst[:, 0:64], cst_b[:, ONES, :], Cb[:], True, True, [cst_b, Cb], [pst])
            TT("dve", carry[:], carry[:], pst[:, 0:64], ALU.add, [carry, pst], [carry])
            for k, o64 in enumerate((a64, b64)):
                src_t = o64a if k == 0 else o64b
                TT("dve", t64[:], o64, pex[:], ALU.mult, [src_t, pex], [t64])
                RED("dve", col(12 + k), t64[:], ALU.add, [t64], [sm])
                TT("dve", t64[:], o64, iot[:], ALU.mult, [src_t, iot], [t64])
                RED("dve", col(14 + k), t64[:], ALU.add, [t64], [sm])
                TSs(col(16 + k), col(12 + k), float(CAP), ALU.is_ge, [sm], [sm], s2=1.0e6, op1=ALU.mult)
                STT(dstf[:, k:k + 1], col(14 + k), float(CAP), col(12 + k), ALU.mult, ALU.add, [sm], [dstf])
                TT("dve", dstf[:, k:k + 1], dstf[:, k:k + 1], col(16 + k), ALU.add, [dstf, sm], [dstf])
                TSs(dstf[:, k:k + 1], dstf[:, k:k + 1], float(NEXP * CAP), ALU.min, [dstf], [dstf])
            CP("dve", destI[:, t, :], dstf[:], [dstf], [destI])
            for k in range(2):
                P.dma("pool", (lambda e, t=t, k=k, hh=hp: e.indirect_dma_start(
                    out=xdisp[:, :], out_offset=bass.IndirectOffsetOnAxis(ap=destI[:, t, k:k + 1], axis=0),
                    in_=hh[:], in_offset=None)),
                    reads=[hp, destI], writes=[xdisp])
        DMA("sp", cnt_s[:], carry[0:1, :], [carry], [cnt_s])
        P.barrier()
        if stop_after == 5:
            P.emit()
            return nc

        A.off = m4b
        NBLK = CAP // 128
        NE_RUN = int(os.environ.get("NE_RUN", str(NEXP)))
        MODE4 = os.environ.get("MODE4", "")
        wgr = A.ring(2, [16, 512], BF16, "wg")
        wur = A.ring(2, [16, 512], BF16, "wu")
        wdr = A.ring(2, [4, 2048], BF16, "wd")
        xer = A.ring(2, [NBLK, D], BF16, "xe")
        xeTr = A.ring(2, [16, CAP], BF16, "xeT")
        hTe = A.ring(2, [4, CAP], BF16, "hTe")
        sgt = A.ring(2, [CAP], F32, "sgt")
        tgt = A.ring(2, [CAP], F32, "tgt")
        yer = A.ring(2, [D], F32, "ye")
        def load_weights(ex):
            Wg = wgr.next()
            Wu = wur.next()
            Wd = wdr.next()
            if not (MODE4 == "cmp" and ex >= 2):
                DMA("pool", Wg[:], w_eg[ex].rearrange("(p kc) n -> p kc n", kc=16), [], [Wg])
                DMA("pool", Wu[:], w_eu[ex].rearrange("(p kc) n -> p kc n", kc=16), [], [Wu])
                DMA("pool", Wd[:], w_ed[ex].rearrange("(kc p) n -> p kc n", p=128), [], [Wd])
            return Wg, Wu, Wd

        def load_xe(ex):
            xe = xer.next()
            DMA("sp", xe[:], xdisp[ex * CAP:(ex + 1) * CAP, :].rearrange("(b p) d -> p b d", p=128), [xdisp], [xe])
            return xe

        def transposes(xe):
            xeT = xeTr.next()
            for b in range(NBLK):
                for g8 in range(0, 16, 8):
                    pt = psTr.next()
                    for j in range(8):
                        c = g8 + j
                        TR(pt[:, j, :], xe[:, b, c * 128:(c + 1) * 128], ident_b, [xe, identb_t], [pt])
                    CP(evac_eng(), xeT[:, g8:g8 + 8, b * 128:(b + 1) * 128], pt[:], [pt], [xeT])
            return xeT

        W_cur = load_weights(0)
        xe_cur = load_xe(0)
        xeT_cur = transposes(xe_cur)
        for ex in range(NE_RUN):
            Wg, Wu, Wd = W_cur
            xeT = xeT_cur
            if ex + 1 < NE_RUN:
                W_cur = load_weights(ex + 1)
                xe_nxt = load_xe(ex + 1)
            hT = hTe.next()
            for mc in range(4):
                psg = pss4.next()
                for kc in range(16):
                    MM(psg[:, 0:CAP], Wg[:, kc, mc * 128:(mc + 1) * 128], xeT[:, kc, :], kc == 0, kc == 15, [Wg, xeT], [psg])
                psu = pss4.next()
                for kc in range(16):
                    MM(psu[:, 0:CAP], Wu[:, kc, mc * 128:(mc + 1) * 128], xeT[:, kc, :], kc == 0, kc == 15, [Wu, xeT], [psu])
                sg = sgt.next()
                tg = tgt.next()
                ACT(sg[:], psg[:, 0:CAP], AF.Sigmoid, [psg], [sg])
                TT("dve", tg[:], psg[:, 0:CAP], sg[:], ALU.mult, [psg, sg], [tg])
                TT("dve", hT[:, mc, :], psu[:, 0:CAP], tg[:], ALU.mult, [psu, tg], [hT])
            if ex + 1 < NE_RUN:
                xeT_cur = transposes(xe_nxt)
            for b in range(NBLK):
                ye = yer.next()
                for nb in range(4):
                    ps = pss4.next()
                    for kc in range(4):
                        MM(ps[:, :], hT[:, kc, b * 128:(b + 1) * 128], Wd[:, kc, nb * 512:(nb + 1) * 512], kc == 0, kc == 3,
                           [hT, Wd], [ps])
                    CP(evac_eng(), ye[:, nb * 512:(nb + 1) * 512], ps[:, :], [ps], [ye])
                DMA("sp", ydisp[ex * CAP + b * 128:ex * CAP + (b + 1) * 128, :], ye[:], [ye], [ydisp])
        P.barrier()

        A.off = m4b
        y1r = A.ring(2, [D], F32, "y1")
        y2r = A.ring(2, [D], F32, "y2")
        x1r = A.ring(2, [D], F32, "x1r")
        outr = A.ring(2, [D], F32, "outr")
        def loads4c(t):
            rows = slice(t * 128, (t + 1) * 128)
            y1 = y1r.next()
            y2 = y2r.next()
            for k, yk in enumerate((y1, y2)):
                P.dma("pool", (lambda e, t=t, k=k, yk=yk: e.indirect_dma_start(
                    out=yk[:], out_offset=None, in_=ydisp[:, :],
                    in_offset=bass.IndirectOffsetOnAxis(ap=destI[:, t, k:k + 1], axis=0))),
                    reads=[ydisp, destI], writes=[yk])
            xt = x1r.next()
            DMA("sp", xt[:], x1_s[rows, :], [x1_s], [xt])
            return y1, y2, xt
        nxt4 = loads4c(0)
        for t in range(NT):
            rows = slice(t * 128, (t + 1) * 128)
            y1, y2, xt = nxt4
            if t + 1 < NT:
                nxt4 = loads4c(t + 1)
            TS("dve", y1[:], y1[:], wts[:, t, 0:1], None, ALU.mult, None, [y1, wts], [y1])
            STT(y1[:], y2[:], wts[:, t, 1:2], y1[:], ALU.mult, ALU.add, [y2, wts, y1], [y1])
            TT("pool", y2[:], y1[:], Bgt2[:], ALU.mult, [y1, Bgt2], [y2])
            ot = outr.next()
            TT("dve", ot[:], y2[:], xt[:], ALU.add, [y2, xt], [ot])
            DMA("sp", out_d[rows, :], ot[:], [ot], [])
        if stop_after == 99:
            pass
        P.emit()
    return nc


def _rope_tables():
    t = np.arange(4096)
    row = (t // 64).astype(np.float32)
    col = (t % 64).astype(np.float32)
    inv = (1.0 / (np.float32(10000.0) ** (np.arange(16, dtype=np.float32) / np.float32(16)))).astype(np.float32)
    ar = (row[:, None] * inv).astype(np.float32)
    ac = (col[:, None] * inv).astype(np.float32)
    cos = np.concatenate([np.cos(ar), np.cos(ar), np.cos(ac), np.cos(ac)], axis=1).T.astype(np.float32)
    sin = np.concatenate([np.sin(ar), np.sin(ar), np.sin(ac), np.sin(ac)], axis=1).T.astype(np.float32)
    return np.ascontiguousarray(cos), np.ascontiguousarray(sin)


def _consts():
    j = np.arange(128)[:, None]
    i = np.arange(128)[None, :]
    c = np.zeros((8, 128, 128), np.float32)
    c[0] = np.eye(128)
    c[1] = (j > i)
    c[2] = (j <= i)
    c[3] = (j <= i)
    c[4] = (j < i)
    c[5] = (j >= i)
    c[6] = (j >= i)
    c[7] = 1.0
    R = np.zeros((64, 64), np.float32)
    for base in (0, 32):
        for m in range(16):
            R[base + m, base + m + 16] = -1.0
            R[base + 16 + m, base + m] = 1.0
    rt = np.ascontiguousarray(R.T)
    iota = np.tile(np.arange(64, dtype=np.float32)[None, :], (128, 1))
    return c, rt, iota


def _prep(inp, cores):
    f = lambda k: np.asarray(inp[k], dtype=np.float32)
    x, c, ctx, c_ctx = f("x"), f("c"), f("ctx"), f("c_ctx")
    w_in = f("w_in")[0]
    cos, sin = _rope_tables()
    cst, rt, iota = _consts()
    w_uq = f("w_uq")[0].reshape(512, 16, 192)
    w_ukv = f("w_ukv")[0].reshape(512, 16, 256)
    shared = {
        "w_mod": f("w_mod")[0],
        "bmod2": np.ascontiguousarray(np.stack([f("b_mod")[0]] * 2)),
        "norm1_g": f("norm1_g"), "norm2_g": f("norm2_g"),
        "w_in": w_in,
        "gla_g": f("gla_norm_g"),
        "qa_g": np.ascontiguousarray(f("q_a_norm_g")[0].reshape(4, 128).T),
        "kva_g": np.ascontiguousarray(f("kv_a_norm_g")[0].reshape(4, 128).T),
        "w_uq_n": np.ascontiguousarray(w_uq[:, :, :128].reshape(512, 2048)),
        "w_uq_r": np.ascontiguousarray(w_uq[:, :, 128:].reshape(512, 1024)),
        "w_ukv_k": np.ascontiguousarray(w_ukv[:, :, :128].reshape(512, 2048)),
        "w_ukv_v": np.ascontiguousarray(w_ukv[:, :, 128:].reshape(512, 2048)),
        "qn_g": np.ascontiguousarray(f("q_norm_g")[0].reshape(192, 1)),
        "kn_g": np.ascontiguousarray(f("k_norm_g")[0].reshape(192, 1)),
        "w_o_gla": f("w_o_gla")[0], "w_o_mla": f("w_o_mla")[0], "w_out": f("w_out")[0],
        "w_rt": np.ascontiguousarray(np.concatenate([f("w_router_group")[0], f("w_router_expert")[0]], axis=1)),
        "b_rt": np.ascontiguousarray(np.concatenate([f("b_router_group")[0], f("b_router_expert")[0]])[None, :]),
        "w_eg": f("w_exp_gate")[0], "w_eu": f("w_exp_up")[0], "w_ed": f("w_exp_down")[0],
        "consts": cst, "rt_c": rt, "iota_c": iota,
    }
    af = w_in[:, 6144:6160]
    ab = w_in[:, 6160:6176]
    wdf = np.concatenate([f("w_decay_f")[0], f("b_decay_f")], axis=0)
    wdb = np.concatenate([f("w_decay_b")[0], f("b_decay_b")], axis=0)
    maps, orders = [], []
    for core in cores:
        b, hf = core // 2, core % 2
        if hf == 1:
            loc = np.arange(2048, 4096)
            oth = np.arange(0, 2048)
            ctxA, ctxB = ctx[b], ctx[b][::-1]
            a1, a2, wd1, wd2 = af, ab, wdf, wdb
        else:
            loc = np.arange(2047, -1, -1)
            oth = np.arange(4095, 2047, -1)
            ctxA, ctxB = ctx[b][::-1], ctx[b]
            a1, a2, wd1, wd2 = ab, af, wdb, wdf
        m = dict(shared)
        m["x_loc"] = np.ascontiguousarray(x[b][loc])
        m["x_A"] = np.ascontiguousarray(np.concatenate([ctxA, x[b][oth]], axis=0))
        m["x_B"] = np.ascontiguousarray(ctxB)
        cc = np.stack([c[b], c_ctx], axis=1)
        m["cT"] = np.ascontiguousarray(cc.reshape(16, 128, 2).transpose(1, 0, 2))
        m["w_a12"] = np.ascontiguousarray(np.concatenate([a1, a2], axis=1))
        m["wd1"] = np.ascontiguousarray(wd1)
        m["wd2"] = np.ascontiguousarray(wd2)
        m["cosq"] = np.ascontiguousarray(cos[:, loc])
        m["sinq"] = np.ascontiguousarray(sin[:, loc])
        m["cosk"] = np.ascontiguousarray(np.concatenate([np.ones((64, 256), np.float32), cos[:, oth], cos[:, loc]], axis=1))
        m["sink"] = np.ascontiguousarray(np.concatenate([np.zeros((64, 256), np.float32), sin[:, oth], sin[:, loc]], axis=1))
        maps.append(m)
        orders.append((b, loc))
    return maps, orders


def kernel(**inputs):
    cores = list(range(8))
    maps, orders = _prep(inputs, cores)
    nc = build()
    res = run_bass_kernel_spmd(nc, maps, core_ids=cores)
    out = np.zeros((4, 4096, 2048), np.float32)
    for (b, loc), r in zip(orders, res.results):
        out[b, loc] = r["out"]
    return out
```

```python
import contextlib
import numpy as np
import concourse.bass as bass
import concourse.mybir as mybir
from concourse.bass_utils import run_bass_kernel_spmd

F32 = mybir.dt.float32
BF16 = mybir.dt.bfloat16
I32 = mybir.dt.int32
AF = mybir.ActivationFunctionType
ALU = mybir.AluOpType
AX = mybir.AxisListType

D = 2048
NLOC = 2048
NA = 2304
NB = 256
NKEY = NA + NLOC
EPS = 1e-6
NEXP = 64
CAP = 384
D_IN = 11360

ENGS = ("pe", "act", "dve", "pool", "sp")
NDSEM = 6
import os
POOL_ENG = os.environ.get("POOL_ENG", "pool")


class Buf:
    __slots__ = ("name", "writers", "readers", "excl")

    def __init__(self, name=""):
        self.name = name
        self.writers = {}
        self.readers = {}
        self.excl = False


class Op:
    __slots__ = ("eng", "fn", "deps", "is_dma", "dma_sem", "dma_val", "inc", "cnt")

    def __init__(self, eng, fn, is_dma):
        self.eng = eng
        self.fn = fn
        self.deps = []
        self.is_dma = is_dma
        self.dma_sem = None
        self.dma_val = None
        self.inc = False
        self.cnt = None


class Prog:
    def __init__(self, nc):
        self.nc = nc
        self.ops = {e: [] for e in ENGS}
        self.ndma = {e: 0 for e in ENGS}
        self.last_dma = {}
        self.pending = {e: None for e in ENGS}

    def _add(self, eng, fn, reads, writes, is_dma):
        op = Op(eng, fn, is_dma)
        deps = {}
        reads = [getattr(b, "buf", b) for b in reads]
        writes = [getattr(b, "buf", b) for b in writes]
        writes = writes + [b for b in reads if b.excl and b not in writes]
        reads = [b for b in reads if not b.excl]
        if self.pending[eng] is not None:
            for t in self.pending[eng]:
                deps[id(t)] = t
            self.pending[eng] = None
        for b in reads:
            b = getattr(b, "buf", b)
            for t in b.writers.values():
                deps[id(t)] = t
        for b in writes:
            b = getattr(b, "buf", b)
            for t in b.writers.values():
                deps[id(t)] = t
            for t in b.readers.values():
                deps[id(t)] = t
        if is_dma:
            k = self.ndma[eng]
            self.ndma[eng] += 1
            op.dma_sem = (eng, k % NDSEM)
            op.dma_val = 16 * (k // NDSEM + 1)
            self.last_dma[op.dma_sem] = op
            key = ("dma", id(op))
        else:
            key = eng
        op.deps = list(deps.values())
        for b in reads:
            b = getattr(b, "buf", b)
            b.readers[key] = op
        for b in writes:
            b = getattr(b, "buf", b)
            b.writers = {key: op}
            b.readers = {}
        self.ops[eng].append(op)
        return op

    def op(self, eng, fn, reads=(), writes=()):
        return self._add(eng, fn, reads, writes, False)

    def dma(self, eng, fn, reads=(), writes=()):
        return self._add(eng, fn, reads, writes, True)

    def barrier(self):
        toks = []
        for e in ENGS:
            for op in reversed(self.ops[e]):
                if not op.is_dma:
                    toks.append(op)
                    break
        toks += list(self.last_dma.values())
        for e in ENGS:
            self.pending[e] = list(toks) + (self.pending[e] or [])

    def emit(self):
        nc = self.nc
        for e in ENGS:
            for op in self.ops[e]:
                for d in op.deps:
                    if not d.is_dma and not (d.eng == e and e == "pe"):
                        d.inc = True
        for e in ENGS:
            c = 0
            for op in self.ops[e]:
                if not op.is_dma and op.inc:
                    c += 1
                    op.cnt = c
        with contextlib.ExitStack() as st:
            esem = {e: st.enter_context(nc.semaphore("s_" + e)) for e in ENGS if e != "sp"}
            dsem = {}
            for e in ENGS:
                if self.ndma[e] > 0:
                    for i in range(NDSEM):
                        dsem[(e, i)] = st.enter_context(nc.semaphore(f"d_{e}{i}"))
            block = st.enter_context(nc.Block())

            def run(e, eng):
                waited = {}

                def wait(sem, val, key):
                    if waited.get(key, 0) >= val:
                        return
                    waited[key] = val
                    eng.wait_ge(sem, val)

                for op in self.ops[e]:
                    for d in op.deps:
                        if d.is_dma:
                            wait(dsem[d.dma_sem], d.dma_val, d.dma_sem)
                        elif not (d.eng == e and e == "pe"):
                            wait(esem[d.eng], d.cnt, d.eng)
                    if op.is_dma:
                        if op.dma_val > 16:
                            wait(dsem[op.dma_sem], op.dma_val - 16, op.dma_sem)
                        op.fn(eng).then_inc(dsem[op.dma_sem], 16)
                    else:
                        ins = op.fn(eng)
                        if op.inc:
                            ins.then_inc(esem[e], 1)
                last = {}
                for op in self.ops[e]:
                    if op.is_dma:
                        last[op.dma_sem] = op.dma_val
                for k, v in last.items():
                    wait(dsem[k], v, k)

            if self.ops["sp"]:
                @block.sync
                def _(eng):
                    run("sp", eng)
            if self.ops["act"]:
                @block.scalar
                def _(eng):
                    run("act", eng)
            if self.ops["dve"]:
                @block.vector
                def _(eng):
                    run("dve", eng)
            if self.ops["pool"]:
                @block.gpsimd
                def _(eng):
                    run("pool", eng)
            if self.ops["pe"]:
                @block.tensor
                def _(eng):
                    run("pe", eng)


class Tl:
    def __init__(self, ap, name=""):
        self.ap = ap
        self.buf = Buf(name)

    def __getitem__(self, k):
        return self.ap[k]


class Ring:
    def __init__(self, tiles):
        self.t = tiles
        self.i = 0

    def next(self):
        t = self.t[self.i % len(self.t)]
        self.i += 1
        return t


_DSZ = {F32: 4, BF16: 2, I32: 4}


class Arena:
    def __init__(self, nc, st, nbytes):
        self.t = st.enter_context(nc.sbuf_tensor("arena", [128, nbytes // 4], F32))
        self.cap = nbytes
        self.off = 0

    def reset(self):
        self.off = 0

    def alloc(self, free, dt, name=""):
        free = tuple(free)
        n = int(np.prod(free))
        nb = (n * _DSZ[dt] + 31) // 32 * 32
        assert self.off + nb <= self.cap, (name, self.off, nb, self.cap)
        ap = self.t[:, self.off // 4:(self.off + nb) // 4]
        self.off += nb
        if dt != F32:
            ap = ap.bitcast(dt)
        ap = ap[:, 0:n]
        if len(free) == 2:
            ap = ap.rearrange("p (a b) -> p a b", a=free[0])
        elif len(free) == 3:
            ap = ap.rearrange("p (a b c) -> p a b c", a=free[0], b=free[1])
        return Tl(ap, name)

    def ring(self, k, free, dt, name=""):
        return Ring([self.alloc(free, dt, f"{name}{i}") for i in range(k)])


def build(dbg=(), stop_after=99, nexp=NEXP):
    nc = bass.Bass("TRN2", target_bir_lowering=False)
    dbg = set(dbg)

    def din(name, shape, dt=F32):
        return nc.dram_tensor(name, list(shape), dt, kind="ExternalInput").ap()

    def dscr(name, shape, dt):
        kind = "ExternalOutput" if name in dbg else "Internal"
        return Tl(nc.dram_tensor(name, list(shape), dt, kind=kind).ap(), name)

    x_loc = din("x_loc", [NLOC, D])
    x_A = din("x_A", [NA, D])
    x_B = din("x_B", [NB, D])
    cT = din("cT", [128, 16, 2])
    w_mod = din("w_mod", [D, 6 * D])
    bmod2 = din("bmod2", [2, 6 * D])
    norm1_g = din("norm1_g", [1, D])
    norm2_g = din("norm2_g", [1, D])
    w_in = din("w_in", [D, D_IN])
    w_a12 = din("w_a12", [D, 32])
    wd1 = din("wd1", [17, 1024])
    wd2 = din("wd2", [17, 1024])
    gla_g = din("gla_g", [1, 512])
    qa_g = din("qa_g", [128, 4])
    kva_g = din("kva_g", [128, 4])
    w_uq_n = din("w_uq_n", [512, 2048])
    w_uq_r = din("w_uq_r", [512, 1024])
    w_ukv_k = din("w_ukv_k", [512, 2048])
    w_ukv_v = din("w_ukv_v", [512, 2048])
    qn_g = din("qn_g", [192, 1])
    kn_g = din("kn_g", [192, 1])
    w_o_gla = din("w_o_gla", [D, D])
    w_o_mla = din("w_o_mla", [D, D])
    w_out = din("w_out", [D, D])
    w_rt = din("w_rt", [D, 72])
    b_rt = din("b_rt", [1, 72])
    w_eg = din("w_eg", [nexp, D, 512])
    w_eu = din("w_eu", [nexp, D, 512])
    w_ed = din("w_ed", [nexp, 512, D])
    cosq = din("cosq", [64, NLOC])
    sinq = din("sinq", [64, NLOC])
    cosk = din("cosk", [64, NKEY])
    sink = din("sink", [64, NKEY])
    consts = din("consts", [8, 128, 128])
    rt_c = din("rt_c", [64, 64])
    iota_c = din("iota_c", [128, 64])
    out_d = nc.dram_tensor("out", [NLOC, D], F32, kind="ExternalOutput").ap()

    mod_s = dscr("mod_s", [2, 6 * D], F32)
    qT_s = dscr("qT_s", [1024, NLOC], BF16)
    kT_s = dscr("kT_s", [1024, NLOC], BF16)
    k_s = dscr("k_s", [NLOC, 1024], BF16)
    v_s = dscr("v_s", [NLOC, 2048], BF16)
    r_s = dscr("r_s", [NLOC, 2048], BF16)
    aT_s = dscr("aT_s", [32, NLOC], F32)
    cqnT_s = dscr("cqnT_s", [512, NLOC], BF16)
    ckvnT_s = dscr("ckvnT_s", [512, NKEY], BF16)
    krT_s = dscr("krT_s", [64, NKEY], F32)
    gaT_s = dscr("gaT_s", [D, NLOC], F32)
    gbT_s = dscr("gbT_s", [D, NLOC], F32)
    kA_s = dscr("kA_s", [NA, 1024], BF16)
    vA_s = dscr("vA_s", [NA, 2048], BF16)
    aA_s = dscr("aA_s", [16, NA], F32)
    kB_s = dscr("kB_s", [NB, 1024], BF16)
    vB_s = dscr("vB_s", [NB, 2048], BF16)
    aB_s = dscr("aB_s", [16, NB], F32)
    of_s = dscr("of_s", [NLOC, 2048], F32)
    yglaT_s = dscr("yglaT_s", [D, NLOC], BF16)
    ymlaT_s = dscr("ymlaT_s", [D, NLOC], BF16)
    x1_s = dscr("x1_s", [NLOC, D], F32)
    h2_s = dscr("h2_s", [NLOC, D], BF16)
    xdisp = dscr("xdisp", [NEXP * CAP + 128, D], BF16)
    cnt_s = dscr("cnt_s", [1, 64], F32)
    ydisp = dscr("ydisp", [NEXP * CAP + 128, D], F32)

    P = Prog(nc)
    with contextlib.ExitStack() as st:
        A = Arena(nc, st, 200 * 1024)
        def sbt(name, shape, dt):
            return Tl(st.enter_context(nc.sbuf_tensor(name, list(shape), dt)), name)
        cst_f = sbt("cst_f", [128, 8, 128], F32)
        cst_b = sbt("cst_b", [128, 8, 128], BF16)
        psb = [Tl(st.enter_context(nc.psum_tensor(f"ps{i}", [128, 512], F32)), f"ps{i}") for i in range(5)]
        psT = [Tl(st.enter_context(nc.psum_tensor(f"psT{i}", [128, 8, 128], BF16)), f"psT{i}") for i in range(2)]
        psS = Tl(st.enter_context(nc.psum_tensor("psS", [128, 512], F32)), "psS")
        for _t in psb + psT + [psS]:
            _t.buf.excl = True
        psr = Ring(psb)
        psTr = Ring(psT[:int(os.environ.get('NPST', '2'))])

        IDENT, SU, UI, MASKF, SL, LI, MASKB, ONES = range(8)

        P.dma("sp", lambda e: e.dma_start(out=cst_f[:], in_=consts.rearrange("c p n -> p c n")), writes=[cst_f])
        P.op("dve", lambda e: e.tensor_copy(out=cst_b[:], in_=cst_f[:]), reads=[cst_f], writes=[cst_b])

        def ACT(out_ap, in_ap, func, reads, writes, **kw):
            P.op("act", lambda e: e.activation(out=out_ap, in_=in_ap, func=func, **kw), reads=reads, writes=writes)

        def CP(eng, out_ap, in_ap, reads, writes):
            if eng == "act":
                P.op("act", lambda e: e.copy(out=out_ap, in_=in_ap), reads=reads, writes=writes)
            else:
                P.op(eng, lambda e: e.tensor_copy(out=out_ap, in_=in_ap), reads=reads, writes=writes)

        def TT(eng, out_ap, in0, in1, op, reads, writes):
            P.op(eng, lambda e: e.tensor_tensor(out=out_ap, in0=in0, in1=in1, op=op), reads=reads, writes=writes)

        def TS(eng, out_ap, in0, s1, s2, op0, op1, reads, writes):
            if s2 is None:
                P.op(eng, lambda e: e.tensor_scalar(out=out_ap, in0=in0, scalar1=s1, scalar2=None, op0=op0),
                     reads=reads, writes=writes)
            else:
                P.op(eng, lambda e: e.tensor_scalar(out=out_ap, in0=in0, scalar1=s1, scalar2=s2, op0=op0, op1=op1),
                     reads=reads, writes=writes)

        def STT(out_ap, in0, scalar, in1, op0, op1, reads, writes):
            P.op("dve", lambda e: e.scalar_tensor_tensor(out=out_ap, in0=in0, scalar=scalar, in1=in1, op0=op0, op1=op1),
                 reads=reads, writes=writes)

        def MM(ps_ap, lhsT, rhs, start, stop, reads, writes):
            P.op("pe", lambda e: e.matmul(ps_ap, lhsT=lhsT, rhs=rhs, start=start, stop=stop), reads=reads, writes=writes)

        def TR(ps_ap, in_ap, ident, reads, writes):
            P.op("pe", lambda e: e.transpose(out=ps_ap, in_=in_ap, identity=ident), reads=reads, writes=writes)

        def DMA(eng, out_ap, in_ap, reads, writes):
            P.dma(eng, lambda e: e.dma_start(out=out_ap, in_=in_ap), reads=reads, writes=writes)

        def RED(eng, out_ap, in_ap, op, reads, writes):
            P.op(eng, lambda e: e.tensor_reduce(out=out_ap, in_=in_ap, axis=AX.X, op=op), reads=reads, writes=writes)

        flip = [0]

        EVAC = os.environ.get("EVAC", "both")

        def evac_eng():
            flip[0] ^= 1
            if EVAC != "both":
                return EVAC
            return "act" if flip[0] else "dve"

        def rstd_ops(sst, ssa, rst, rsa, n, extra=1.0):
            ACT(rsa, ssa, AF.Sqrt, [sst], [rst], scale=1.0 / n, bias=EPS)
            P.op("dve", lambda e: e.reciprocal(out=rsa, in_=rsa), reads=[rst], writes=[rst])
            if extra != 1.0:
                TS("dve", rsa, rsa, extra, None, ALU.mult, None, [rst], [rst])

        IDENT, SU, UI, MASKF, SL, LI, MASKB, ONES = range(8)
        DMA("sp", cst_f[:], consts.rearrange("c p n -> p c n"), [], [cst_f])
        CP("dve", cst_b[:], cst_f[:], [cst_f], [cst_b])
        identb_t = sbt("identb", [128, 128], BF16)
        CP("dve", identb_t[:], cst_f[:, IDENT, :], [cst_f], [identb_t])
        ident_b = identb_t[:] if os.environ.get("IDSEP", "1") == "1" else cst_b[:, IDENT, :]
        ident_f = cst_f[:, IDENT, :]

        A.reset()
        cTf = A.alloc([16, 2], F32, "cTf")
        cTe = A.alloc([16, 2], F32, "cTe")
        cTs = A.alloc([16, 2], BF16, "cTs")
        bm = A.alloc([6 * D], F32, "bm")
        mrow = A.alloc([6 * D], F32, "mrow")
        wtm = A.ring(3, [16, 512], BF16, "wtm")
        DMA("sp", cTf[:], cT, [], [cTf])
        DMA("sp", bm[0:2, :], bmod2, [], [bm])
        ACT(cTe[:], cTf[:], AF.Sigmoid, [cTf], [cTe])
        TT("dve", cTs[:], cTf[:], cTe[:], ALU.mult, [cTf, cTe], [cTs])
        for nb in range(24):
            wt = wtm.next()
            DMA("pool", wt[:], w_mod[:, nb * 512:(nb + 1) * 512].rearrange("(kc p) n -> p kc n", p=128), [], [wt])
            ps = psr.next()
            for kc in range(16):
                MM(ps[0:2, :], cTs[:, kc, :], wt[:, kc, :], kc == 0, kc == 15, [cTs, wt], [ps])
            TT("dve", mrow[0:2, nb * 512:(nb + 1) * 512], ps[0:2, :], bm[0:2, nb * 512:(nb + 1) * 512], ALU.add,
               [ps, bm], [mrow])
        DMA("sp", mod_s[:], mrow[0:2, :], [mrow], [mod_s])
        P.barrier()
        if stop_after == 0:
            P.emit()
            return nc

        def mod_tile(dst, tmp1, tmp2, row, chunk, gain_ap):
            DMA("sp", tmp1[:], mod_s[row, chunk * D:(chunk + 1) * D].partition_broadcast(128), [mod_s], [tmp1])
            DMA("sp", tmp2[:], gain_ap.partition_broadcast(128), [], [tmp2])
            STT(dst[:], tmp1[:], 1.0, tmp2[:], ALU.add, ALU.mult, [tmp1, tmp2], [dst])

        def shift_tile(dst, row, chunk):
            DMA("sp", dst[:], mod_s[row, chunk * D:(chunk + 1) * D].partition_broadcast(128), [mod_s], [dst])

        STEPS = int(os.environ.get("P1A_STEPS", "9"))

        def norm_mod_tile(xt, Bsc_, Bsh_, hb, junk, ss, rs, tmp, n=D):
            if STEPS >= 2:
                ACT(junk[:], xt[:], AF.Square, [xt], [junk, ss], accum_out=ss[:])
            if STEPS >= 3:
                rstd_ops(ss, ss[:], rs, rs[:], n)
            if STEPS >= 4:
                STT(tmp[:], xt[:], rs[:, 0:1], Bsc_[:], ALU.mult, ALU.mult, [xt, rs, Bsc_], [tmp])
            if STEPS >= 5:
                TT(POOL_ENG, hb[:], tmp[:], Bsh_[:], ALU.add, [tmp, Bsh_], [hb])

        dbg_t = sbt('dbg_t', [128, 128], BF16)

        def transpose_tile(src, dstT, col0, nch=16):
            for g in range(0, nch, 8):
                pt = psTr.next()
                for j in range(8):
                    c = g + j
                    TR(pt[:, j, :], src[:, c * 128:(c + 1) * 128], ident_b, [src, cst_b, identb_t], [pt])
                CP(evac_eng(), dstT[:, g:g + 8, col0:col0 + 128], pt[:], [pt], [dstT])

        A.reset()
        hT = A.alloc([16, NA], BF16, "hT")
        Bsc = A.alloc([D], F32, "Bsc")
        Bsh = A.alloc([D], F32, "Bsh")
        Bscc = A.alloc([D], F32, "Bscc")
        Bshc = A.alloc([D], F32, "Bshc")
        mark1 = A.off
        t1 = A.alloc([D], F32, "t1")
        t2 = A.alloc([D], F32, "t2")
        mod_tile(Bsc, t1, t2, 0, 1, norm1_g[0, :])
        shift_tile(Bsh, 0, 0)
        t1b = A.alloc([D], F32, "t1b")
        t2b = A.alloc([D], F32, "t2b")
        mod_tile(Bscc, t1b, t2b, 1, 1, norm1_g[0, :])
        shift_tile(Bshc, 1, 0)
        P.barrier()
        if stop_after == 10:
            P.emit()
            return nc
        A.off = mark1
        mark_u = A.off
        xtr = A.ring(2, [D], F32, "xt")
        tmpr = A.ring(1, [D], F32, "tmp")
        hbr = A.ring(2, [D], BF16, "hb")
        junk = A.alloc([D], BF16, "junk")
        ssr = A.ring(2, [1], F32, "ss")
        rsr = A.ring(2, [1], F32, "rs")
        A.off = mark_u
        wtr = A.ring(2, [16, 512], BF16, "wt")
        stT = A.ring(2, [NA], F32, "stT")
        stN = A.ring(2, [9, 512], BF16, "stN")
        raw4 = A.alloc([4, 512], F32, "raw4")
        sq4 = A.ring(2, [512], BF16, "sq")
        sgr = A.ring(2, [512], F32, "sg")
        rbc = A.alloc([512], F32, "rbc")
        gcol_q = A.alloc([4], F32, "gcq")
        gcol_kv = A.alloc([4], F32, "gckv")

        def p1a(x_d, ntok, nctx_tiles):
            for t in range(min(ntok // 128, int(os.environ.get('P1A_TILES', '99')))):
                xt = xtr.next()
                DMA("sp", xt[:], x_d[t * 128:(t + 1) * 128, :], [], [xt])
                hb = hbr.next()
                isc = t < nctx_tiles
                norm_mod_tile(xt, Bscc if isc else Bsc, Bshc if isc else Bsh, hb, junk, ssr.next(), rsr.next(), tmpr.next())
                if STEPS >= 6:
                    transpose_tile(hb, hT, t * 128)

        def load_w(src2d, ncols):
            wt = wtr.next()
            DMA("pool", wt[:, :, 0:ncols], src2d.rearrange("(kc p) n -> p kc n", p=128), [], [wt])
            return wt

        def mm_T(wt, m0, mc, g0, gn):
            ps = psr.next()
            for kc in range(16):
                MM(ps[0:mc, 0:gn], wt[:, kc, m0:m0 + mc], hT[:, kc, g0:g0 + gn], kc == 0, kc == 15, [wt, hT], [ps])
            return ps

        def mm_N(wt, ncols, t):
            ps = psr.next()
            for kc in range(16):
                MM(ps[:, 0:ncols], hT[:, kc, t * 128:(t + 1) * 128], wt[:, kc, 0:ncols], kc == 0, kc == 15, [wt, hT], [ps])
            return ps

        def groups(ntok):
            return [(g0, min(512, ntok - g0)) for g0 in range(0, ntok, 512)]

        def job_T(wsrc, ncols, ntok, dst, dst_row0, dst_col0, dt, func=None, scale=1.0):
            wt = load_w(wsrc, ncols)
            for m0 in range(0, ncols, 128):
                mc = min(128, ncols - m0)
                stg = stT.next()
                sap = stg[:] if dt == F32 else stg[:].bitcast(BF16)
                for (g0, gn) in groups(ntok):
                    ps = mm_T(wt, m0, mc, g0, gn)
                    if func is not None:
                        ACT(sap[0:mc, g0:g0 + gn], ps[0:mc, 0:gn], func, [ps], [stg], scale=scale)
                    elif scale != 1.0:
                        P_mul(sap[0:mc, g0:g0 + gn], ps[0:mc, 0:gn], scale, [ps], [stg])
                    else:
                        CP(evac_eng(), sap[0:mc, g0:g0 + gn], ps[0:mc, 0:gn], [ps], [stg])
                DMA("sp", dst[dst_row0 + m0:dst_row0 + m0 + mc, dst_col0:dst_col0 + ntok], sap[0:mc, 0:ntok], [stg], [dst])
            return wt

        def P_mul(out_ap, in_ap, scale, reads, writes):
            P.op("act", lambda e: e.mul(out=out_ap, in_=in_ap, mul=scale), reads=reads, writes=writes)

        def job_N(wsrc, ncols, ntok, dst, dst_col0, silu=False, wt=None):
            if wt is None:
                wt = load_w(wsrc, ncols)
            nt = ntok // 128
            for t0 in range(0, nt, 9):
                tn = min(9, nt - t0)
                stg = stN.next()
                for tt in range(tn):
                    t = t0 + tt
                    ps = mm_N(wt, ncols, t)
                    if silu:
                        sg = sgr.next()
                        ACT(sg[:, 0:ncols], ps[:, 0:ncols], AF.Sigmoid, [ps], [sg])
                        TT("dve", stg[:, tt, 0:ncols], ps[:, 0:ncols], sg[:, 0:ncols], ALU.mult, [ps, sg], [stg])
                    else:
                        CP(evac_eng(), stg[:, tt, 0:ncols], ps[:, 0:ncols], [ps], [stg])
                DMA("sp", dst[t0 * 128:(t0 + tn) * 128, dst_col0:dst_col0 + ncols].rearrange("(t p) c -> p t c", p=128),
                    stg[:, 0:tn, 0:ncols], [stg], [dst])

        def job_Tnorm(wsrc, ntok, gcol, dst, dst_col0):
            wt = load_w(wsrc, 512)
            stgs = [stT.next(), stT.next()]
            for (g0, gn) in groups(ntok):
                for m in range(4):
                    ps = mm_T(wt, m * 128, 128, g0, gn)
                    sq = sq4.next()
                    CP("dve", raw4[:, m, 0:gn], ps[:, 0:gn], [ps], [raw4])
                    ACT(sq[:, 0:gn], raw4[:, m, 0:gn], AF.Square, [raw4], [sq])
                    MM(psS[:, 0:gn], cst_b[:, ONES, :], sq[:, 0:gn], m == 0, m == 3, [sq, cst_b], [psS])
                rstd_ops(psS, psS[:, 0:gn], rbc, rbc[:, 0:gn], 512)
                for m in range(4):
                    stg = stgs[m // 2]
                    sap = stg[:].bitcast(BF16)
                    off = (m % 2) * NA
                    STT(sap[:, off + g0:off + g0 + gn], raw4[:, m, 0:gn], gcol[:, m:m + 1], rbc[:, 0:gn],
                        ALU.mult, ALU.mult, [raw4, gcol, rbc], [stg])
            for m in range(4):
                stg = stgs[m // 2]
                sap = stg[:].bitcast(BF16)
                off = (m % 2) * NA
                DMA("sp", dst[m * 128:(m + 1) * 128, dst_col0:dst_col0 + ntok], sap[:, off:off + ntok], [stg], [dst])

        O_Q, O_K, O_V, O_R, O_CQ, O_CKV, O_KR, O_GA, O_GB = 0, 1024, 2048, 4096, 6176, 6688, 7200, 7264, 9312

        def load_gcols():
            DMA("sp", gcol_q[:], qa_g, [], [gcol_q])
            DMA("sp", gcol_kv[:], kva_g, [], [gcol_kv])

        p1a(x_A, NA, 2)
        P.barrier()
        if stop_after == 11:
            P.emit()
            return nc
        load_gcols()
        for b in range(2):
            job_N(w_in[:, O_K + b * 512:O_K + (b + 1) * 512], 512, NA, kA_s, b * 512)
        for b in range(4):
            job_N(w_in[:, O_V + b * 512:O_V + (b + 1) * 512], 512, NA, vA_s, b * 512)
        if stop_after == 12:
            P.emit()
            return nc
        job_T(w_a12[:, 0:16], 16, NA, aA_s, 0, 0, F32)
        if stop_after == 13:
            P.emit()
            return nc
        job_Tnorm(w_in[:, O_CKV:O_CKV + 512], NA, gcol_kv, ckvnT_s, 0)
        if stop_after == 14:
            P.emit()
            return nc
        job_T(w_in[:, O_KR:O_KR + 64], 64, NA, krT_s, 0, 0, F32)
        if stop_after == 15:
            P.emit()
            return nc
        P.barrier()
        p1a(x_B, NB, 2)
        P.barrier()
        load_gcols()
        for b in range(2):
            job_N(w_in[:, O_K + b * 512:O_K + (b + 1) * 512], 512, NB, kB_s, b * 512)
        for b in range(4):
            job_N(w_in[:, O_V + b * 512:O_V + (b + 1) * 512], 512, NB, vB_s, b * 512)
        job_T(w_a12[:, 16:32], 16, NB, aB_s, 0, 0, F32)
        P.barrier()
        p1a(x_loc, NLOC, 0)
        P.barrier()
        load_gcols()
        for b in range(2):
            job_T(w_in[:, O_Q + b * 512:O_Q + (b + 1) * 512], 512, NLOC, qT_s, b * 512, 0, BF16, scale=1.0 / 16.0)
        for b in range(2):
            wtk = job_T(w_in[:, O_K + b * 512:O_K + (b + 1) * 512], 512, NLOC, kT_s, b * 512, 0, BF16)
            job_N(None, 512, NLOC, k_s, b * 512, wt=wtk)
        for b in range(4):
            job_N(w_in[:, O_V + b * 512:O_V + (b + 1) * 512], 512, NLOC, v_s, b * 512)
        for b in range(4):
            job_N(w_in[:, O_R + b * 512:O_R + (b + 1) * 512], 512, NLOC, r_s, b * 512, silu=True)
        job_T(w_a12, 32, NLOC, aT_s, 0, 0, F32)
        job_Tnorm(w_in[:, O_CQ:O_CQ + 512], NLOC, gcol_q, cqnT_s, 0)
        job_Tnorm(w_in[:, O_CKV:O_CKV + 512], NLOC, gcol_kv, ckvnT_s, NA)
        job_T(w_in[:, O_KR:O_KR + 64], 64, NLOC, krT_s, 0, NA, F32)
        for b in range(4):
            job_T(w_in[:, O_GA + b * 512:O_GA + (b + 1) * 512], 512, NLOC, gaT_s, b * 512, 0, F32, func=AF.Sigmoid)
        for b in range(4):
            job_T(w_in[:, O_GB + b * 512:O_GB + (b + 1) * 512], 512, NLOC, gbT_s, b * 512, 0, F32, func=AF.Sigmoid)
        P.barrier()
        if stop_after == 1:
            P.emit()
            return nc
        A.reset()
        NKT = NKEY // 128
        ckvnT = A.alloc([4, NKEY], BF16, "ckvnT")
        cqnT = A.alloc([4, NLOC], BF16, "cqnT")
        KrT = A.alloc([NKEY], BF16, "KrT")
        krss = A.alloc([NKT], F32, "krss")
        gcols = A.alloc([4], F32, "gcols")
        RTf = A.alloc([64], F32, "RTf")
        RTb = A.alloc([64], BF16, "RTb")
        wk = A.alloc([4, 256], BF16, "wk")
        wv = A.alloc([4, 256], BF16, "wv")
        wqn = A.alloc([4, 256], BF16, "wqn")
        wqr = A.alloc([4, 128], BF16, "wqr")
        KT = A.alloc([2, NKEY], BF16, "KT")
        Vt = A.alloc([NKT, 256], BF16, "V")
        kscale = A.alloc([2, NKT], F32, "kscale")
        QTn = A.alloc([2, NLOC], BF16, "QTn")
        QTr = A.alloc([2, NLOC], BF16, "QTr")
        rawn = A.alloc([512], F32, "rawn")
        rawr = A.alloc([512], F32, "rawr")
        sqn = A.alloc([512], BF16, "sqn")
        sqr = A.alloc([512], BF16, "sqr")
        rbc2 = A.alloc([512], F32, "rbc2")
        qrg = A.alloc([512], BF16, "qrg")
        tA = A.alloc([512], F32, "tA")
        tB = A.alloc([512], F32, "tB")
        cosc = A.alloc([512], F32, "cosc")
        sinc = A.alloc([512], F32, "sinc")
        krf = A.alloc([512], F32, "krf")
        tmpk = A.alloc([8], F32, "tmpk")
        pTr = A.ring(3, [512], BF16, "pT")
        recip = A.alloc([512], F32, "recip")
        racc = A.alloc([512], F32, "racc")
        rhi = A.alloc([512], BF16, "rhi")
        rlo = A.alloc([512], BF16, "rlo")
        ystg = A.ring(2, [NLOC], BF16, "ystg")
        pss4 = Ring(psb[0:4])
        po = psb[4]
        ones_b = cst_b[:, ONES, :]

        DMA("sp", ckvnT[:], ckvnT_s[:].rearrange("(rc p) n -> p rc n", p=128), [ckvnT_s], [ckvnT])
        DMA("sp", cqnT[:], cqnT_s[:].rearrange("(rc p) n -> p rc n", p=128), [cqnT_s], [cqnT])
        DMA("sp", gcols[:, 0:1], kn_g[0:128, :], [], [gcols])
        DMA("sp", gcols[0:64, 1:2], kn_g[128:192, :], [], [gcols])
        DMA("sp", gcols[:, 2:3], qn_g[0:128, :], [], [gcols])
        DMA("sp", gcols[0:64, 3:4], qn_g[128:192, :], [], [gcols])
        DMA("sp", RTf[0:64, :], rt_c, [], [RTf])
        CP("dve", RTb[0:64, :], RTf[0:64, :], [RTf], [RTb])

        def kchunks():
            return [(c0, min(512, NKEY - c0)) for c0 in range(0, NKEY, 512)]

        def rope_apply(dst_ap, dst_t, xg_bf, xg_t, cos_ap, sin_ap, tabs, n):
            ps2 = pss4.next()
            MM(ps2[0:64, 0:n], RTb[0:64, :], xg_bf, True, True, [RTb, xg_t], [ps2])
            TT("dve", tA[0:64, 0:n], xg_bf, cos_ap, ALU.mult, [xg_t] + tabs, [tA])
            TT("dve", tB[0:64, 0:n], ps2[0:64, 0:n], sin_ap, ALU.mult, [ps2] + tabs, [tB])
            TT("pool", dst_ap, tA[0:64, 0:n], tB[0:64, 0:n], ALU.add, [tA, tB], [dst_t])

        for (c0, cn) in kchunks():
            kt0, nt = c0 // 128, cn // 128
            DMA("sp", krf[0:64, 0:cn], krT_s[:, c0:c0 + cn], [krT_s], [krf])
            DMA("sp", cosc[0:64, 0:cn], cosk[:, c0:c0 + cn], [], [cosc])
            DMA("sp", sinc[0:64, 0:cn], sink[:, c0:c0 + cn], [], [sinc])
            ACT(sqr[0:64, 0:cn], krf[0:64, 0:cn], AF.Square, [krf], [sqr])
            for j in range(nt):
                MM(psS[:, j:j + 1], sqr[0:64, j * 128:(j + 1) * 128], ones_b[0:64, 0:1], True, True, [sqr, cst_b], [psS])
            CP("dve", krss[:, kt0:kt0 + nt], psS[:, 0:nt], [psS], [krss])
            TS("dve", qrg[0:64, 0:cn], krf[0:64, 0:cn], gcols[0:64, 1:2], None, ALU.mult, None, [krf, gcols], [qrg])
            rope_apply(KrT[0:64, c0:c0 + cn], KrT, qrg[0:64, 0:cn], qrg, cosc[0:64, 0:cn], sinc[0:64, 0:cn], [cosc, sinc], cn)

        zrow2 = A.alloc([D], BF16, "zrow2")
        zrowf = A.alloc([D], F32, "zrowf")
        P.op("pool", lambda e: e.memset(zrow2[:], 0.0), reads=[], writes=[zrow2])
        P.op("pool", lambda e: e.memset(zrowf[:], 0.0), reads=[], writes=[zrowf])
        SCL = 192.0 ** -0.5
        NGRP = int(os.environ.get("MLA_GROUPS", "8"))
        for g in range(NGRP):
            DMA("pool", wk[:], w_ukv_k[:, g * 256:(g + 1) * 256].rearrange("(rc p) n -> p rc n", p=128), [], [wk])
            DMA("pool", wv[:], w_ukv_v[:, g * 256:(g + 1) * 256].rearrange("(rc p) n -> p rc n", p=128), [], [wv])
            DMA("pool", wqn[:], w_uq_n[:, g * 256:(g + 1) * 256].rearrange("(rc p) n -> p rc n", p=128), [], [wqn])
            DMA("pool", wqr[:], w_uq_r[:, g * 128:(g + 1) * 128].rearrange("(rc p) n -> p rc n", p=128), [], [wqr])
            if g == 0:
                for e_ in range((NEXP * CAP) // 128 + 1):
                    DMA("pool", xdisp[e_ * 128:(e_ + 1) * 128, :], zrow2[:], [zrow2], [])
                DMA("pool", ydisp[NEXP * CAP:NEXP * CAP + 128, :], zrowf[:], [zrowf], [])
            for (c0, cn) in kchunks():
                kt0, nt = c0 // 128, cn // 128
                for hl in range(2):
                    ps = pss4.next()
                    for rc in range(4):
                        MM(ps[:, 0:cn], wk[:, rc, hl * 128:(hl + 1) * 128], ckvnT[:, rc, c0:c0 + cn], rc == 0, rc == 3,
                           [wk, ckvnT], [ps])
                    TS("dve", KT[:, hl, c0:c0 + cn], ps[:, 0:cn], gcols[:, 0:1], None, ALU.mult, None, [ps, gcols], [KT])
                    ACT(sqn[:, 0:cn], ps[:, 0:cn], AF.Square, [ps], [sqn])
                    for j in range(nt):
                        MM(psS[:, hl * 4 + j:hl * 4 + j + 1], sqn[:, j * 128:(j + 1) * 128], ones_b[:, 0:1], True, True,
                           [sqn, cst_b], [psS])
                for hl in range(2):
                    TT("dve", tmpk[:, 0:nt], psS[:, hl * 4:hl * 4 + nt], krss[:, kt0:kt0 + nt], ALU.add, [psS, krss], [tmpk])
                    rstd_ops(tmpk, tmpk[:, 0:nt], kscale, kscale[:, hl, kt0:kt0 + nt], 192, extra=SCL)
            for kt in range(NKT):
                ps = pss4.next()
                for rc in range(4):
                    MM(ps[:, 0:256], ckvnT[:, rc, kt * 128:(kt + 1) * 128], wv[:, rc, :], rc == 0, rc == 3, [wv, ckvnT], [ps])
                CP(evac_eng(), Vt[:, kt, :], ps[:, 0:256], [ps], [Vt])
            for qc in range(4):
                q0 = qc * 512
                DMA("sp", cosc[0:64, :], cosq[:, q0:q0 + 512], [], [cosc])
                DMA("sp", sinc[0:64, :], sinq[:, q0:q0 + 512], [], [sinc])
                for hl in range(2):
                    psn = pss4.next()
                    for rc in range(4):
                        MM(psn[:, :], wqn[:, rc, hl * 128:(hl + 1) * 128], cqnT[:, rc, q0:q0 + 512], rc == 0, rc == 3,
                           [wqn, cqnT], [psn])
                    psq = pss4.next()
                    for rc in range(4):
                        MM(psq[0:64, :], wqr[:, rc, hl * 64:(hl + 1) * 64], cqnT[:, rc, q0:q0 + 512], rc == 0, rc == 3,
                           [wqr, cqnT], [psq])
                    CP("dve", rawn[:], psn[:, :], [psn], [rawn])
                    CP("act", rawr[0:64, :], psq[0:64, :], [psq], [rawr])
                    ACT(sqn[:], rawn[:], AF.Square, [rawn], [sqn])
                    ACT(sqr[0:64, :], rawr[0:64, :], AF.Square, [rawr], [sqr])
                    MM(psS[:, :], ones_b, sqn[:], True, False, [sqn, cst_b], [psS])
                    MM(psS[:, :], cst_b[0:64, ONES, :], sqr[0:64, :], False, True, [sqr, cst_b], [psS])
                    rstd_ops(psS, psS[:, :], rbc2, rbc2[:], 192)
                    STT(QTn[:, hl, q0:q0 + 512], rawn[:], gcols[:, 2:3], rbc2[:], ALU.mult, ALU.mult, [rawn, gcols, rbc2], [QTn])
                    STT(qrg[0:64, :], rawr[0:64, :], gcols[0:64, 3:4], rbc2[0:64, :], ALU.mult, ALU.mult,
                        [rawr, gcols, rbc2], [qrg])
                    rope_apply(QTr[0:64, hl, q0:q0 + 512], QTr, qrg[0:64, :], qrg, cosc[0:64, :], sinc[0:64, :], [cosc, sinc], 512)
            for hl in range(0 if os.environ.get('MLA_NOATT') else 2):
                h = 2 * g + hl
                stg = ystg.next()
                for qc in range(4):
                    q0 = qc * 512
                    def qk(kt, hl=hl, q0=q0):
                        pss = pss4.next()
                        MM(pss[:, :], KT[:, hl, kt * 128:(kt + 1) * 128], QTn[:, hl, q0:q0 + 512], True, False, [KT, QTn], [pss])
                        MM(pss[:, :], KrT[0:64, kt * 128:(kt + 1) * 128], QTr[0:64, hl, q0:q0 + 512], False, True,
                           [KrT, QTr], [pss])
                        return pss
                    LA = 2
                    pend = [qk(i) for i in range(LA)]
                    for kt in range(NKT):
                        pss = pend.pop(0)
                        if kt + LA < NKT:
                            pend.append(qk(kt + LA))
                        pT = pTr.next()
                        ACT(pT[:], pss[:, :], AF.Exp, [pss, kscale], [pT], scale=kscale[:, hl, kt:kt + 1])
                        MM(po[:, :], Vt[:, kt, hl * 128:(hl + 1) * 128], pT[:], kt == 0, kt == NKT - 1, [Vt, pT], [po])
                        MM(psS[:, :], ones_b, pT[:], kt == 0, kt == NKT - 1, [pT, cst_b], [psS])
                    P.op("dve", lambda e: e.reciprocal(out=recip[:], in_=psS[:, :]), reads=[psS], writes=[recip])
                    TT("dve", stg[:, q0:q0 + 512], po[:, :], recip[:], ALU.mult, [po, recip], [stg])
                DMA("sp", ymlaT_s[h * 128:(h + 1) * 128, :], stg[:], [stg], [ymlaT_s])
        P.barrier()
        if stop_after == 2:
            P.emit()
            return nc
        A.reset()
        S = A.alloc([4, 2, 512], F32, "S")
        Sb = A.alloc([4, 2, 512], BF16, "Sb")
        wdf = A.alloc([1024], F32, "wdf")
        wdh = A.alloc([1024], BF16, "wdh")
        wdl = A.alloc([1024], BF16, "wdl")
        wdt = A.alloc([1024], F32, "wdt")
        glg = A.alloc([512], F32, "glg")
        aTr = A.ring(2, [128], F32, "aT")
        ahr = A.ring(2, [128], BF16, "ah")
        alr = A.ring(2, [128], BF16, "al")
        att_ = A.alloc([128], F32, "att_")
        gex = A.alloc([1024], F32, "gex")
        gtm = A.alloc([1024], F32, "gtm")
        ghi = A.alloc([1024], BF16, "ghi")
        glo = A.alloc([1024], BF16, "glo")
        gt2 = A.alloc([1024], F32, "gt2")
        qTr = A.ring(2, [8, 128], BF16, "qTt")
        kTr = A.ring(2, [8, 128], BF16, "kTt")
        ktr = A.ring(2, [1024], BF16, "kt")
        vtr = A.ring(2, [2048], BF16, "vt")
        ekt = A.alloc([256], F32, "ekt")
        ektr = A.ring(2, [256], F32, "ektr")
        kend = A.ring(2, [256], BF16, "kend")
        eb = A.ring(2, [256], F32, "eb")
        enb = A.ring(2, [256], F32, "enb")
        qdec = A.ring(2, [2, 128], BF16, "qdec")
        kinv = A.ring(2, [2, 128], BF16, "kinv")
        attm = A.ring(2, [128], BF16, "attm")
        de2 = A.ring(2, [2], F32, "de2")
        ofst = A.ring(2, [2048], F32, "ofst")
        rtl = A.ring(2, [2048], BF16, "rt")
        ssg = A.ring(2, [4], F32, "ssg")
        rsg = A.ring(2, [4], F32, "rsg")
        junkg = A.alloc([512], BF16, "junkg")
        ytmp = A.alloc([512], F32, "ytmp")
        ybf = A.ring(2, [2048], BF16, "ybf")
        yTs = A.ring(2, [16, 128], BF16, "yTs")

        DMA("sp", glg[:], gla_g[0, :].partition_broadcast(128), [], [glg])

        def MEMSET(eng, ap, val, writes):
            P.op(eng, lambda e: e.memset(ap, val), reads=[], writes=writes)

        def hilo(eng, src_ap, hi_ap, lo_ap, tmp_ap, src_t, hi_t, lo_t, tmp_t):
            CP(eng, hi_ap, src_ap, [src_t], [hi_t])
            TT(eng, tmp_ap, src_ap, hi_ap, ALU.subtract, [src_t, hi_t], [tmp_t])
            CP(eng, lo_ap, tmp_ap, [tmp_t], [lo_t])

        def load_wd(wd_d):
            DMA("sp", wdf[0:17, :], wd_d, [], [wdf])
            hilo("dve", wdf[0:17, :], wdh[0:17, :], wdl[0:17, :], wdt[0:17, :], wdf, wdh, wdl, wdt)

        def load_a(a_src_ap, a_src_t):
            aT = aTr.next()
            MEMSET("pool", aT[0:32, :], 1.0, [aT])
            DMA("sp", aT[0:16, :], a_src_ap, [a_src_t], [aT])
            return aT

        def gates(aT):
            ah = ahr.next()
            al = alr.next()
            hilo("pool", aT[0:32, :], ah[0:32, :], al[0:32, :], att_[0:32, :], aT, ah, al, att_)
            for half in range(2):
                ps = pss4.next()
                cs = slice(half * 512, (half + 1) * 512)
                MM(ps[:, :], ah[0:17, :], wdh[0:17, cs], True, False, [ah, wdh], [ps])
                MM(ps[:, :], ah[0:17, :], wdl[0:17, cs], False, False, [ah, wdl], [ps])
                MM(ps[:, :], al[0:17, :], wdh[0:17, cs], False, True, [al, wdh], [ps])
                ACT(gex[:, cs], ps[:, :], AF.Exp, [ps], [gex], scale=-1.0)
            ACT(gtm[:], gex[:], AF.Ln, [gex], [gtm], bias=1.0)
            TS("dve", gtm[:], gtm[:], -1.0 / 16.0, None, ALU.mult, None, [gtm], [gtm])
            hilo("dve", gtm[:], ghi[:], glo[:], gt2[:], gtm, ghi, glo, gt2)

        def mm_hl(ps_ap, ps_t, lhs_fn, rhs_fn):
            for i, gx in enumerate((ghi, glo)):
                MM(ps_ap, lhs_fn(gx), rhs_fn(gx), i == 0, i == 1, [gx, cst_b], [ps_t])

        def kend_for(h, kt_tile, EM):
            ps = pss4.next()
            mm_hl(ps[:, 0:256], ps, lambda gx: cst_b[:, EM, :], lambda gx: gx[:, h * 256:(h + 1) * 256])
            ACT(ekt[:], ps[:, 0:256], AF.Exp, [ps], [ekt])
            ke = kend.next()
            TT("dve", ke[:], kt_tile[:, h * 256:(h + 1) * 256], ekt[:], ALU.mult, [kt_tile, ekt], [ke])
            return ke

        def state_update(h, ke, vt, de_ap, de_t):
            for dkc in range(2):
                ps = pss4.next()
                MM(ps[:, :], ke[:, dkc * 128:(dkc + 1) * 128], vt[:, h * 512:(h + 1) * 512], True, True, [ke, vt], [ps])
                STT(S[:, h, dkc, :], S[:, h, dkc, :], de_ap(dkc), ps[:, :], ALU.mult, ALU.add, [S, de_t, ps], [S])
            CP("act", Sb[:, h, :, :], S[:, h, :, :], [S], [Sb])

        def state_pass(k_d, v_d, a_d, ntok):
            def loads(t):
                kt_tile = ktr.next()
                vt = vtr.next()
                DMA("sp", kt_tile[:], k_d[t * 128:(t + 1) * 128, :], [k_d], [kt_tile])
                DMA("sp", vt[:], v_d[t * 128:(t + 1) * 128, :], [v_d], [vt])
                aT = load_a(a_d[0:16, t * 128:(t + 1) * 128], a_d)
                return kt_tile, vt, aT
            nt_ = ntok // 128
            nxt = loads(0)
            for t in range(nt_):
                kt_tile, vt, aT = nxt
                if t + 1 < nt_:
                    nxt = loads(t + 1)
                gates(aT)
                for h in range(4):
                    ke = kend_for(h, kt_tile, SU)
                    d2 = de2.next()
                    ps = pss4.next()
                    for dkc in range(2):
                        c0 = h * 256 + dkc * 128
                        mm_hl(ps[:, dkc:dkc + 1], ps, lambda gx, c0=c0: gx[:, c0:c0 + 128], lambda gx: cst_b[:, ONES, 0:1])
                    ACT(d2[:], ps[:, 0:2], AF.Exp, [ps], [d2])
                    state_update(h, ke, vt, lambda dkc, d2=d2: d2[:, dkc:dkc + 1], d2)

        def local_pass(direction):
            EM, CM = (SU, UI) if direction == 0 else (SL, LI)
            decol = 127 if direction == 0 else 0
            a_row0 = 0 if direction == 0 else 16
            tiles = range(NLOC // 128) if direction == 0 else range(NLOC // 128 - 1, -1, -1)
            def loads(t):
                ts_ = slice(t * 128, (t + 1) * 128)
                qTt = qTr.next()
                kTt = kTr.next()
                kt_tile = ktr.next()
                vt = vtr.next()
                DMA("sp", qTt[:], qT_s[:, ts_].rearrange("(c p) t -> p c t", p=128), [qT_s], [qTt])
                DMA("sp", kTt[:], kT_s[:, ts_].rearrange("(c p) t -> p c t", p=128), [kT_s], [kTt])
                DMA("sp", kt_tile[:], k_s[ts_, :], [k_s], [kt_tile])
                DMA("sp", vt[:], v_s[ts_, :], [v_s], [vt])
                aT = load_a(aT_s[a_row0:a_row0 + 16, ts_], aT_s)
                of = ofst.next()
                rt = None
                if direction == 1:
                    DMA("sp", of[:], of_s[ts_, :], [of_s], [of])
                    rt = rtl.next()
                    DMA("sp", rt[:], r_s[ts_, :], [r_s], [rt])
                return qTt, kTt, kt_tile, vt, aT, of, rt
            tiles = list(tiles)
            nxt = loads(tiles[0])
            for ti, t in enumerate(tiles):
                ts_ = slice(t * 128, (t + 1) * 128)
                qTt, kTt, kt_tile, vt, aT, of, rt = nxt
                if ti + 1 < len(tiles):
                    nxt = loads(tiles[ti + 1])
                gates(aT)
                if direction == 1:
                    ss = ssg.next()
                    rs = rsg.next()
                for hp_ in range(0, 4, 2):
                    pair = (hp_, hp_ + 1)
                    psE, psB = {}, {}
                    for h in pair:
                        psE[h] = pss4.next()
                        mm_hl(psE[h][:, 0:256], psE[h], lambda gx: cst_b[:, EM, :], lambda gx, h=h: gx[:, h * 256:(h + 1) * 256])
                    for h in pair:
                        psB[h] = pss4.next()
                        for dkc in range(2):
                            c0 = h * 256 + dkc * 128
                            mm_hl(psB[h][:, dkc * 128:(dkc + 1) * 128], psB[h], lambda gx, c0=c0: gx[:, c0:c0 + 128],
                                  lambda gx: cst_b[:, CM, :])
                    ek, e1s, e2s = {}, {}, {}
                    for h in pair:
                        ek[h] = ektr.next()
                        ACT(ek[h][:], psE[h][:, 0:256], AF.Exp, [psE[h]], [ek[h]])
                    for h in pair:
                        e1s[h] = eb.next()
                        e2s[h] = enb.next()
                        ACT(e1s[h][:], psB[h][:, 0:256], AF.Exp, [psB[h]], [e1s[h]])
                        ACT(e2s[h][:], psB[h][:, 0:256], AF.Exp, [psB[h]], [e2s[h]], scale=-1.0)
                    kes, qds, kis = {}, {}, {}
                    for h in pair:
                        kes[h] = kend.next()
                        TT("dve", kes[h][:], kt_tile[:, h * 256:(h + 1) * 256], ek[h][:], ALU.mult, [kt_tile, ek[h]], [kes[h]])
                        qds[h] = qdec.next()
                        kis[h] = kinv.next()
                        TT("dve", qds[h][:], qTt[:, 2 * h:2 * h + 2, :], e1s[h][:].rearrange("p (a b) -> p a b", a=2), ALU.mult,
                           [qTt, e1s[h]], [qds[h]])
                        TT("pool", kis[h][:], kTt[:, 2 * h:2 * h + 2, :], e2s[h][:].rearrange("p (a b) -> p a b", a=2), ALU.mult,
                           [kTt, e2s[h]], [kis[h]])
                    ams = {}
                    psA = {}
                    for h in pair:
                        psA[h] = pss4.next()
                        for dkc in range(2):
                            MM(psA[h][:, 0:128], kis[h][:, dkc, :], qds[h][:, dkc, :], dkc == 0, dkc == 1, [kis[h], qds[h]], [psA[h]])
                    for h in pair:
                        ams[h] = attm.next()
                        TT("dve", ams[h][:], psA[h][:, 0:128], cst_f[:, CM, :], ALU.mult, [psA[h], cst_f], [ams[h]])
                    for h in pair:
                        pO = po if h % 2 == 0 else psS
                        MM(pO[:, :], ams[h][:], vt[:, h * 512:(h + 1) * 512], True, False, [ams[h], vt], [pO])
                        for dkc in range(2):
                            MM(pO[:, :], qds[h][:, dkc, :], Sb[:, h, dkc, :], False, dkc == 1, [qds[h], Sb], [pO])
                    for h in pair:
                        pO = po if h % 2 == 0 else psS
                        if direction == 0:
                            CP("act", of[:, h * 512:(h + 1) * 512], pO[:, :], [pO], [of])
                        else:
                            TT("dve", of[:, h * 512:(h + 1) * 512], pO[:, :], of[:, h * 512:(h + 1) * 512], ALU.add, [pO, of], [of])
                            ACT(junkg[:], of[:, h * 512:(h + 1) * 512], AF.Square, [of], [junkg, ss], accum_out=ss[:, h:h + 1])
                    for h in pair:
                        state_update(h, kes[h], vt, lambda dkc, e1=e1s[h]: e1[:, dkc * 128 + decol:dkc * 128 + decol + 1], e1s[h])
                if direction == 0:
                    DMA("sp", of_s[ts_, :], of[:], [of], [of_s])
                else:
                    rstd_ops(ss, ss[:], rs, rs[:], 512)
                    yb = ybf.next()
                    for h in range(4):
                        hs = slice(h * 512, (h + 1) * 512)
                        STT(ytmp[:], of[:, hs], rs[:, h:h + 1], glg[:], ALU.mult, ALU.mult, [of, rs, glg], [ytmp])
                        TT("pool", yb[:, hs], ytmp[:], rt[:, hs], ALU.mult, [ytmp, rt], [yb])
                    yT = yTs.next()
                    transpose_tile(yb, yT, 0)
                    DMA("sp", yglaT_s[:, ts_].rearrange("(c p) t -> p c t", p=128), yT[:], [yT], [yglaT_s])

        def zero_state():
            MEMSET("dve", S[:], 0.0, [S])
            MEMSET("pool", Sb[:], 0.0, [Sb])

        load_wd(wd1)
        zero_state()
        state_pass(kA_s, vA_s, aA_s, NA)
        local_pass(0)
        load_wd(wd2)
        zero_state()
        state_pass(kB_s, vB_s, aB_s, NB)
        local_pass(1)
        P.barrier()
        if stop_after == 3:
            P.emit()
            return nc
        A.reset()
        Bgt1 = A.alloc([D], F32, "Bgt1")
        shift_tile(Bgt1, 0, 2)
        GT = 1024
        ygT = A.alloc([16, GT], BF16, "ygT")
        ymT = A.alloc([16, GT], BF16, "ymT")
        yT3 = A.alloc([16, GT], BF16, "yT3")
        w3r = A.ring(2, [16, 512], BF16, "w3")
        gar = A.ring(2, [4, 512], F32, "ga")
        gbr = A.ring(2, [4, 512], F32, "gb")
        t3a = A.ring(2, [512], F32, "t3a")
        t3b = A.ring(2, [512], F32, "t3b")
        x3r = A.ring(4, [512], F32, "x3")
        x3o = A.ring(2, [512], F32, "x3o")

        def load_w3(src2d):
            wt = w3r.next()
            DMA("pool", wt[:], src2d.rearrange("(kc p) n -> p kc n", p=128), [], [wt])
            return wt

        for g in range(NLOC // GT):
            gs = slice(g * GT, (g + 1) * GT)
            DMA("sp", ygT[:], yglaT_s[:, gs].rearrange("(c p) t -> p c t", p=128), [yglaT_s], [ygT])
            DMA("sp", ymT[:], ymlaT_s[:, gs].rearrange("(c p) t -> p c t", p=128), [ymlaT_s], [ymT])
            for mb in range(4):
                ms = slice(mb * 512, (mb + 1) * 512)
                Wg = load_w3(w_o_gla[:, ms])
                Wm = load_w3(w_o_mla[:, ms])
                for hf in range(GT // 512):
                    hs = slice(hf * 512, (hf + 1) * 512)
                    ts_ = slice(g * GT + hf * 512, g * GT + (hf + 1) * 512)
                    ga = gar.next()
                    gb = gbr.next()
                    DMA("sp", ga[:], gaT_s[ms, ts_].rearrange("(m p) t -> p m t", p=128), [gaT_s], [ga])
                    DMA("sp", gb[:], gbT_s[ms, ts_].rearrange("(m p) t -> p m t", p=128), [gbT_s], [gb])
                    for m in range(4):
                        ps1 = pss4.next()
                        for kc in range(16):
                            MM(ps1[:, :], Wg[:, kc, m * 128:(m + 1) * 128], ygT[:, kc, hs], kc == 0, kc == 15, [Wg, ygT], [ps1])
                        ps2 = pss4.next()
                        for kc in range(16):
                            MM(ps2[:, :], Wm[:, kc, m * 128:(m + 1) * 128], ymT[:, kc, hs], kc == 0, kc == 15, [Wm, ymT], [ps2])
                        ta = t3a.next()
                        tb = t3b.next()
                        TT("dve", ta[:], ps1[:, :], ga[:, m, :], ALU.mult, [ps1, ga], [ta])
                        TT("dve", tb[:], ps2[:, :], gb[:, m, :], ALU.mult, [ps2, gb], [tb])
                        TT("pool", yT3[:, mb * 4 + m, hs], ta[:], tb[:], ALU.add, [ta, tb], [yT3])
            for nb in range(4):
                ns = slice(nb * 512, (nb + 1) * 512)
                Wo = load_w3(w_out[:, ns])
                for t4 in range(0, GT // 128, 4):
                    xts = []
                    for tt in range(t4, t4 + 4):
                        rows = slice(g * GT + tt * 128, g * GT + (tt + 1) * 128)
                        xt = x3r.next()
                        DMA("sp", xt[:], x_loc[rows, ns], [], [xt])
                        xts.append(xt)
                    for tt in range(t4, t4 + 4):
                        rows = slice(g * GT + tt * 128, g * GT + (tt + 1) * 128)
                        xt = xts[tt - t4]
                        ps = pss4.next()
                        for kc in range(16):
                            MM(ps[:, :], yT3[:, kc, tt * 128:(tt + 1) * 128], Wo[:, kc, :], kc == 0, kc == 15, [yT3, Wo], [ps])
                        ta = t3a.next()
                        TT("dve", ta[:], ps[:, :], Bgt1[:, ns], ALU.mult, [ps, Bgt1], [ta])
                        xo = x3o.next()
                        TT("pool", xo[:], ta[:], xt[:], ALU.add, [ta, xt], [xo])
                        DMA("sp", x1_s[rows, ns], xo[:], [xo], [x1_s])
        P.barrier()
        if stop_after == 4:
            P.emit()
            return nc

        A.reset()
        NT = NLOC // 128
        Bgt2 = A.alloc([D], F32, "Bgt2")
        shift_tile(Bgt2, 0, 5)
        destI = A.alloc([NT, 2], I32, "destI")
        wts = A.alloc([NT, 2], F32, "wts")
        carry = A.alloc([64], F32, "carry")
        iot = A.alloc([64], F32, "iot")
        brt = A.alloc([72], F32, "brt")
        wrf = A.alloc([16, 72], F32, "wrf")
        wrh = A.alloc([16, 72], BF16, "wrh")
        wrl = A.alloc([16, 72], BF16, "wrl")
        wrt = A.alloc([16, 72], F32, "wrt")
        m4b = A.off
        Bsc2 = A.alloc([D], F32, "Bsc2")
        Bsh2 = A.alloc([D], F32, "Bsh2")
        m4 = A.off
        u1 = A.alloc([D], F32, "u1")
        u2 = A.alloc([D], F32, "u2")
        mod_tile(Bsc2, u1, u2, 0, 4, norm2_g[0, :])
        shift_tile(Bsh2, 0, 3)
        P.barrier()
        A.off = m4
        x1t = A.ring(2, [D], F32, "x1t")
        tmp4 = A.alloc([D], F32, "tmp4")
        h2f = A.alloc([D], F32, "h2f")
        h2h = A.ring(2, [D], BF16, "h2h")
        h2l = A.alloc([D], BF16, "h2l")
        h2p = A.ring(2, [D], BF16, "h2p")
        junk4 = A.alloc([D], BF16, "junk4")
        h2hT = A.alloc([16, 128], BF16, "h2hT")
        h2lT = A.alloc([16, 128], BF16, "h2lT")
        zrow = A.alloc([D], BF16, "zrow")
        sm = A.alloc([64], F32, "sm")
        lg = A.alloc([72], F32, "lg")
        ohG = A.alloc([8], F32, "ohG")
        eg8 = A.alloc([8], F32, "eg8")
        msk = A.alloc([8, 8], F32, "msk")
        le = A.alloc([8], F32, "le")
        le2 = A.alloc([8], F32, "le2")
        oh1 = A.alloc([8], F32, "oh1")
        oh2 = A.alloc([8], F32, "oh2")
        o64a = A.alloc([8, 8], F32, "o64a")
        o64b = A.alloc([8, 8], F32, "o64b")
        Cb = A.alloc([64], BF16, "Cb")
        Cf = A.alloc([64], F32, "Cf")
        pex = A.alloc([64], F32, "pex")
        t64 = A.alloc([64], F32, "t64")
        dstf = A.alloc([2], F32, "dstf")

        DMA("sp", iot[:], iota_c, [], [iot])
        DMA("sp", brt[:], b_rt[0, :].partition_broadcast(128), [], [brt])
        DMA("sp", wrf[:], w_rt.rearrange("(kc p) n -> p kc n", p=128), [], [wrf])
        hilo("dve", wrf[:], wrh[:], wrl[:], wrt[:], wrf, wrh, wrl, wrt)
        MEMSET("dve", carry[:], 0.0, [carry])

        def col(i):
            return sm[:, i:i + 1]

        def TSs(out_ap, in0, s1, op0, reads, writes, s2=None, op1=None):
            TS("dve", out_ap, in0, s1, s2, op0, op1, reads, writes)

        for t in range(NT):
            rows = slice(t * 128, (t + 1) * 128)
            xt = x1t.next()
            DMA("sp", xt[:], x1_s[rows, :], [x1_s], [xt])
            ss = ssr_4 = None
            ACT(junk4[:], xt[:], AF.Square, [xt], [junk4, sm], accum_out=col(0))
            rstd_ops(sm, col(0), sm, col(1), D)
            STT(tmp4[:], xt[:], col(1), Bsc2[:], ALU.mult, ALU.mult, [xt, sm, Bsc2], [tmp4])
            TT("pool", h2f[:], tmp4[:], Bsh2[:], ALU.add, [tmp4, Bsh2], [h2f])
            hh = h2h.next()
            CP("act", hh[:], h2f[:], [h2f], [hh])
            TT("dve", tmp4[:], h2f[:], hh[:], ALU.subtract, [h2f, hh], [tmp4])
            CP("pool", h2l[:], tmp4[:], [tmp4], [h2l])
            hp = h2p.next()
            CP("pool", hp[:].rearrange("s (kc p) -> s kc p", kc=16), hh[:].rearrange("s (p kc) -> s kc p", kc=16), [hh], [hp])
            transpose_tile(hh, h2hT, 0)
            transpose_tile(h2l, h2lT, 0)
            psl = pss4.next()
            for kc in range(16):
                MM(psl[:, 0:72], h2hT[:, kc, :], wrh[:, kc, :], kc == 0, False, [h2hT, wrh], [psl])
                MM(psl[:, 0:72], h2hT[:, kc, :], wrl[:, kc, :], False, False, [h2hT, wrl], [psl])
                MM(psl[:, 0:72], h2lT[:, kc, :], wrh[:, kc, :], False, kc == 15, [h2lT, wrh], [psl])
            TT("dve", lg[:], psl[:, 0:72], brt[:], ALU.add, [psl, brt], [lg])
            RED("dve", col(2), lg[:, 0:8], ALU.max, [lg], [sm])
            TSs(col(3), col(2), -1.0, ALU.mult, [sm], [sm])
            ACT(eg8[:], lg[:, 0:8], AF.Exp, [lg, sm], [eg8, sm], bias=col(3), accum_out=col(4))
            P.op("dve", lambda e: e.reciprocal(out=col(5), in_=col(4)), reads=[sm], writes=[sm])
            TSs(ohG[:], lg[:, 0:8], col(2), ALU.is_equal, [lg, sm], [ohG])
            lgE = lg[:, 8:72].rearrange("p (g e) -> p g e", g=8)
            TT("dve", msk[:], lgE, ohG[:].unsqueeze(2).to_broadcast([128, 8, 8]), ALU.mult, [lg, ohG], [msk])
            RED("dve", le[:], msk[:].rearrange("p g e -> p e g"), ALU.add, [msk], [le])
            RED("dve", col(6), le[:], ALU.max, [le], [sm])
            TSs(oh1[:], le[:], col(6), ALU.is_equal, [le, sm], [oh1])
            STT(le2[:], oh1[:], -1.0e30, le[:], ALU.mult, ALU.add, [oh1, le], [le2])
            RED("dve", col(7), le2[:], ALU.max, [le2], [sm])
            TSs(oh2[:], le2[:], col(7), ALU.is_equal, [le2, sm], [oh2])
            TSs(col(8), col(6), -1.0, ALU.mult, [sm], [sm])
            ACT(col(9), col(7), AF.Exp, [sm], [sm], bias=col(8))
            TSs(col(10), col(9), 1.0, ALU.add, [sm], [sm])
            P.op("dve", lambda e: e.reciprocal(out=col(10), in_=col(10)), reads=[sm], writes=[sm])
            TT("dve", wts[:, t, 0:1], col(5), col(10), ALU.mult, [sm], [wts])
            TT("dve", wts[:, t, 1:2], wts[:, t, 0:1], col(9), ALU.mult, [sm, wts], [wts])
            gB = ohG[:].unsqueeze(2).to_broadcast([128, 8, 8])
            TT("dve", o64a[:], oh1[:].unsqueeze(1).to_broadcast([128, 8, 8]), gB, ALU.mult, [oh1, ohG], [o64a])
            TT("dve", o64b[:], oh2[:].unsqueeze(1).to_broadcast([128, 8, 8]), gB, ALU.mult, [oh2, ohG], [o64b])
            a64 = o64a[:].rearrange("p g e -> p (g e)")
            b64 = o64b[:].rearrange("p g e -> p (g e)")
            TT("dve", Cf[:], a64, b64, ALU.add, [o64a, o64b], [Cf])
            CP("dve", Cb[:], Cf[:], [Cf], [Cb])
            psx = pss4.next()
            MM(psx[:, 0:64], cst_b[:, SL, :], Cb[:], True, True, [cst_b, Cb], [psx])
            TT("dve", pex[:], psx[:, 0:64], carry[:], ALU.add, [psx, carry], [pex])
            pst = pss4.next()
            MM(pst[:, 0:64], cst_b[:, ONES, :], Cb[:], True, True, [cst_b, Cb], [pst])
            TT("dve", carry[:], carry[:], pst[:, 0:64], ALU.add, [carry, pst], [carry])
            for k, o64 in enumerate((a64, b64)):
                src_t = o64a if k == 0 else o64b
                TT("dve", t64[:], o64, pex[:], ALU.mult, [src_t, pex], [t64])
                RED("dve", col(12 + k), t64[:], ALU.add, [t64], [sm])
                TT("dve", t64[:], o64, iot[:], ALU.mult, [src_t, iot], [t64])
                RED("dve", col(14 + k), t64[:], ALU.add, [t64], [sm])
                TSs(col(16 + k), col(12 + k), float(CAP), ALU.is_ge, [sm], [sm], s2=1.0e6, op1=ALU.mult)
                STT(dstf[:, k:k + 1], col(14 + k), float(CAP), col(12 + k), ALU.mult, ALU.add, [sm], [dstf])
                TT("dve", dstf[:, k:k + 1], dstf[:, k:k + 1], col(16 + k), ALU.add, [dstf, sm], [dstf])
                TSs(dstf[:, k:k + 1], dstf[:, k:k + 1], float(NEXP * CAP), ALU.min, [dstf], [dstf])
            CP("dve", destI[:, t, :], dstf[:], [dstf], [destI])
            for k in range(2):
                P.dma("pool", (lambda e, t=t, k=k, hh=hp: e.indirect_dma_start(
                    out=xdisp[:, :], out_offset=bass.IndirectOffsetOnAxis(ap=destI[:, t, k:k + 1], axis=0),
                    in_=hh[:], in_offset=None)),
                    reads=[hp, destI], writes=[xdisp])
        DMA("sp", cnt_s[:], carry[0:1, :], [carry], [cnt_s])
        P.barrier()
        if stop_after == 5:
            P.emit()
            return nc

        A.off = m4b
        NBLK = CAP // 128
        NE_RUN = int(os.environ.get("NE_RUN", str(NEXP)))
        MODE4 = os.environ.get("MODE4", "")
        wgr = A.ring(2, [16, 512], BF16, "wg")
        wur = A.ring(2, [16, 512], BF16, "wu")
        wdr = A.ring(2, [4, 2048], BF16, "wd")
        xer = A.ring(2, [NBLK, D], BF16, "xe")
        xeTr = A.ring(2, [16, CAP], BF16, "xeT")
        hTe = A.ring(2, [4, CAP], BF16, "hTe")
        sgt = A.ring(2, [CAP], F32, "sgt")
        tgt = A.ring(2, [CAP], F32, "tgt")
        yer = A.ring(2, [D], F32, "ye")
        def load_weights(ex):
            Wg = wgr.next()
            Wu = wur.next()
            Wd = wdr.next()
            if not (MODE4 == "cmp" and ex >= 2):
                DMA("pool", Wg[:], w_eg[ex].rearrange("(p kc) n -> p kc n", kc=16), [], [Wg])
                DMA("pool", Wu[:], w_eu[ex].rearrange("(p kc) n -> p kc n", kc=16), [], [Wu])
                DMA("pool", Wd[:], w_ed[ex].rearrange("(kc p) n -> p kc n", p=128), [], [Wd])
            return Wg, Wu, Wd

        def load_xe(ex):
            xe = xer.next()
            DMA("sp", xe[:], xdisp[ex * CAP:(ex + 1) * CAP, :].rearrange("(b p) d -> p b d", p=128), [xdisp], [xe])
            return xe

        def transposes(xe):
            xeT = xeTr.next()
            for b in range(NBLK):
                for g8 in range(0, 16, 8):
                    pt = psTr.next()
                    for j in range(8):
                        c = g8 + j
                        TR(pt[:, j, :], xe[:, b, c * 128:(c + 1) * 128], ident_b, [xe, identb_t], [pt])
                    CP(evac_eng(), xeT[:, g8:g8 + 8, b * 128:(b + 1) * 128], pt[:], [pt], [xeT])
            return xeT

        W_cur = load_weights(0)
        xe_cur = load_xe(0)
        xeT_cur = transposes(xe_cur)
        for ex in range(NE_RUN):
            Wg, Wu, Wd = W_cur
            xeT = xeT_cur
            if ex + 1 < NE_RUN:
                W_cur = load_weights(ex + 1)
                xe_nxt = load_xe(ex + 1)
            hT = hTe.next()
            for mc in range(4):
                psg = pss4.next()
                for kc in range(16):
                    MM(psg[:, 0:CAP], Wg[:, kc, mc * 128:(mc + 1) * 128], xeT[:, kc, :], kc == 0, kc == 15, [Wg, xeT], [psg])
                psu = pss4.next()
                for kc in range(16):
                    MM(psu[:, 0:CAP], Wu[:, kc, mc * 128:(mc + 1) * 128], xeT[:, kc, :], kc == 0, kc == 15, [Wu, xeT], [psu])
                sg = sgt.next()
                tg = tgt.next()
                ACT(sg[:], psg[:, 0:CAP], AF.Sigmoid, [psg], [sg])
                TT("dve", tg[:], psg[:, 0:CAP], sg[:], ALU.mult, [psg, sg], [tg])
                TT("dve", hT[:, mc, :], psu[:, 0:CAP], tg[:], ALU.mult, [psu, tg], [hT])
            if ex + 1 < NE_RUN:
                xeT_cur = transposes(xe_nxt)
            for b in range(NBLK):
                ye = yer.next()
                for nb in range(4):
                    ps = pss4.next()
                    for kc in range(4):
                        MM(ps[:, :], hT[:, kc, b * 128:(b + 1) * 128], Wd[:, kc, nb * 512:(nb + 1) * 512], kc == 0, kc == 3,
                           [hT, Wd], [ps])
                    CP(evac_eng(), ye[:, nb * 512:(nb + 1) * 512], ps[:, :], [ps], [ye])
                DMA("sp", ydisp[ex * CAP + b * 128:ex * CAP + (b + 1) * 128, :], ye[:], [ye], [ydisp])
        P.barrier()

        A.off = m4b
        y1r = A.ring(2, [D], F32, "y1")
        y2r = A.ring(2, [D], F32, "y2")
        x1r = A.ring(2, [D], F32, "x1r")
        outr = A.ring(2, [D], F32, "outr")
        def loads4c(t):
            rows = slice(t * 128, (t + 1) * 128)
            y1 = y1r.next()
            y2 = y2r.next()
            for k, yk in enumerate((y1, y2)):
                P.dma("pool", (lambda e, t=t, k=k, yk=yk: e.indirect_dma_start(
                    out=yk[:], out_offset=None, in_=ydisp[:, :],
                    in_offset=bass.IndirectOffsetOnAxis(ap=destI[:, t, k:k + 1], axis=0))),
                    reads=[ydisp, destI], writes=[yk])
            xt = x1r.next()
            DMA("sp", xt[:], x1_s[rows, :], [x1_s], [xt])
            return y1, y2, xt
        nxt4 = loads4c(0)
        for t in range(NT):
            rows = slice(t * 128, (t + 1) * 128)
            y1, y2, xt = nxt4
            if t + 1 < NT:
                nxt4 = loads4c(t + 1)
            TS("dve", y1[:], y1[:], wts[:, t, 0:1], None, ALU.mult, None, [y1, wts], [y1])
            STT(y1[:], y2[:], wts[:, t, 1:2], y1[:], ALU.mult, ALU.add, [y2, wts, y1], [y1])
            TT("pool", y2[:], y1[:], Bgt2[:], ALU.mult, [y1, Bgt2], [y2])
            ot = outr.next()
            TT("dve", ot[:], y2[:], xt[:], ALU.add, [y2, xt], [ot])
            DMA("sp", out_d[rows, :], ot[:], [ot], [])
        if stop_after == 99:
            pass
        P.emit()
    return nc


def _rope_tables():
    t = np.arange(4096)
    row = (t // 64).astype(np.float32)
    col = (t % 64).astype(np.float32)
    inv = (1.0 / (np.float32(10000.0) ** (np.arange(16, dtype=np.float32) / np.float32(16)))).astype(np.float32)
    ar = (row[:, None] * inv).astype(np.float32)
    ac = (col[:, None] * inv).astype(np.float32)
    cos = np.concatenate([np.cos(ar), np.cos(ar), np.cos(ac), np.cos(ac)], axis=1).T.astype(np.float32)
    sin = np.concatenate([np.sin(ar), np.sin(ar), np.sin(ac), np.sin(ac)], axis=1).T.astype(np.float32)
    return np.ascontiguousarray(cos), np.ascontiguousarray(sin)


def _consts():
    j = np.arange(128)[:, None]
    i = np.arange(128)[None, :]
    c = np.zeros((8, 128, 128), np.float32)
    c[0] = np.eye(128)
    c[1] = (j > i)
    c[2] = (j <= i)
    c[3] = (j <= i)
    c[4] = (j < i)
    c[5] = (j >= i)
    c[6] = (j >= i)
    c[7] = 1.0
    R = np.zeros((64, 64), np.float32)
    for base in (0, 32):
        for m in range(16):
            R[base + m, base + m + 16] = -1.0
            R[base + 16 + m, base + m] = 1.0
    rt = np.ascontiguousarray(R.T)
    iota = np.tile(np.arange(64, dtype=np.float32)[None, :], (128, 1))
    return c, rt, iota


def _prep(inp, cores):
    f = lambda k: np.asarray(inp[k], dtype=np.float32)
    x, c, ctx, c_ctx = f("x"), f("c"), f("ctx"), f("c_ctx")
    w_in = f("w_in")[0]
    cos, sin = _rope_tables()
    cst, rt, iota = _consts()
    w_uq = f("w_uq")[0].reshape(512, 16, 192)
    w_ukv = f("w_ukv")[0].reshape(512, 16, 256)
    shared = {
        "w_mod": f("w_mod")[0],
        "bmod2": np.ascontiguousarray(np.stack([f("b_mod")[0]] * 2)),
        "norm1_g": f("norm1_g"), "norm2_g": f("norm2_g"),
        "w_in": w_in,
        "gla_g": f("gla_norm_g"),
        "qa_g": np.ascontiguousarray(f("q_a_norm_g")[0].reshape(4, 128).T),
        "kva_g": np.ascontiguousarray(f("kv_a_norm_g")[0].reshape(4, 128).T),
        "w_uq_n": np.ascontiguousarray(w_uq[:, :, :128].reshape(512, 2048)),
        "w_uq_r": np.ascontiguousarray(w_uq[:, :, 128:].reshape(512, 1024)),
        "w_ukv_k": np.ascontiguousarray(w_ukv[:, :, :128].reshape(512, 2048)),
        "w_ukv_v": np.ascontiguousarray(w_ukv[:, :, 128:].reshape(512, 2048)),
        "qn_g": np.ascontiguousarray(f("q_norm_g")[0].reshape(192, 1)),
        "kn_g": np.ascontiguousarray(f("k_norm_g")[0].reshape(192, 1)),
        "w_o_gla": f("w_o_gla")[0], "w_o_mla": f("w_o_mla")[0], "w_out": f("w_out")[0],
        "w_rt": np.ascontiguousarray(np.concatenate([f("w_router_group")[0], f("w_router_expert")[0]], axis=1)),
        "b_rt": np.ascontiguousarray(np.concatenate([f("b_router_group")[0], f("b_router_expert")[0]])[None, :]),
        "w_eg": f("w_exp_gate")[0], "w_eu": f("w_exp_up")[0], "w_ed": f("w_exp_down")[0],
        "consts": cst, "rt_c": rt, "iota_c": iota,
    }
    af = w_in[:, 6144:6160]
    ab = w_in[:, 6160:6176]
    wdf = np.concatenate([f("w_decay_f")[0], f("b_decay_f")], axis=0)
    wdb = np.concatenate([f("w_decay_b")[0], f("b_decay_b")], axis=0)
    maps, orders = [], []
    for core in cores:
        b, hf = core // 2, core % 2
        if hf == 1:
            loc = np.arange(2048, 4096)
            oth = np.arange(0, 2048)
            ctxA, ctxB = ctx[b], ctx[b][::-1]
            a1, a2, wd1, wd2 = af, ab, wdf, wdb
        else:
            loc = np.arange(2047, -1, -1)
            oth = np.arange(4095, 2047, -1)
            ctxA, ctxB = ctx[b][::-1], ctx[b]
            a1, a2, wd1, wd2 = ab, af, wdb, wdf
        m = dict(shared)
        m["x_loc"] = np.ascontiguousarray(x[b][loc])
        m["x_A"] = np.ascontiguousarray(np.concatenate([ctxA, x[b][oth]], axis=0))
        m["x_B"] = np.ascontiguousarray(ctxB)
        cc = np.stack([c[b], c_ctx], axis=1)
        m["cT"] = np.ascontiguousarray(cc.reshape(16, 128, 2).transpose(1, 0, 2))
        m["w_a12"] = np.ascontiguousarray(np.concatenate([a1, a2], axis=1))
        m["wd1"] = np.ascontiguousarray(wd1)
        m["wd2"] = np.ascontiguousarray(wd2)
        m["cosq"] = np.ascontiguousarray(cos[:, loc])
        m["sinq"] = np.ascontiguousarray(sin[:, loc])
        m["cosk"] = np.ascontiguousarray(np.concatenate([np.ones((64, 256), np.float32), cos[:, oth], cos[:, loc]], axis=1))
        m["sink"] = np.ascontiguousarray(np.concatenate([np.zeros((64, 256), np.float32), sin[:, oth], sin[:, loc]], axis=1))
        maps.append(m)
        orders.append((b, loc))
    return maps, orders


def kernel(**inputs):
    cores = list(range(8))
    maps, orders = _prep(inputs, cores)
    nc = build()
    res = run_bass_kernel_spmd(nc, maps, core_ids=cores)
    out = np.zeros((4, 4096, 2048), np.float32)
    for (b, loc), r in zip(orders, res.results):
        out[b, loc] = r["out"]
    return out
```

```python
import contextlib
import numpy as np
import concourse.bass as bass
import concourse.mybir as mybir
from concourse.bass_utils import run_bass_kernel_spmd

F32 = mybir.dt.float32
BF16 = mybir.dt.bfloat16
I32 = mybir.dt.int32
AF = mybir.ActivationFunctionType
ALU = mybir.AluOpType
AX = mybir.AxisListType

D = 2048
NLOC = 2048
NA = 2304
NB = 256
NKEY = NA + NLOC
EPS = 1e-6
NEXP = 64
CAP = 384
D_IN = 11360

ENGS = ("pe", "act", "dve", "pool", "sp")
NDSEM = 6
import os
POOL_ENG = os.environ.get("POOL_ENG", "pool")


class Buf:
    __slots__ = ("name", "writers", "readers", "excl")

    def __init__(self, name=""):
        self.name = name
        self.writers = {}
        self.readers = {}
        self.excl = False


class Op:
    __slots__ = ("eng", "fn", "deps", "is_dma", "dma_sem", "dma_val", "inc", "cnt")

    def __init__(self, eng, fn, is_dma):
        self.eng = eng
        self.fn = fn
        self.deps = []
        self.is_dma = is_dma
        self.dma_sem = None
        self.dma_val = None
        self.inc = False
        self.cnt = None


class Prog:
    def __init__(self, nc):
        self.nc = nc
        self.ops = {e: [] for e in ENGS}
        self.ndma = {e: 0 for e in ENGS}
        self.last_dma = {}
        self.pending = {e: None for e in ENGS}

    def _add(self, eng, fn, reads, writes, is_dma):
        op = Op(eng, fn, is_dma)
        deps = {}
        reads = [getattr(b, "buf", b) for b in reads]
        writes = [getattr(b, "buf", b) for b in writes]
        writes = writes + [b for b in reads if b.excl and b not in writes]
        reads = [b for b in reads if not b.excl]
        if self.pending[eng] is not None:
            for t in self.pending[eng]:
                deps[id(t)] = t
            self.pending[eng] = None
        for b in reads:
            b = getattr(b, "buf", b)
            for t in b.writers.values():
                deps[id(t)] = t
        for b in writes:
            b = getattr(b, "buf", b)
            for t in b.writers.values():
                deps[id(t)] = t
            for t in b.readers.values():
                deps[id(t)] = t
        if is_dma:
            k = self.ndma[eng]
            self.ndma[eng] += 1
            op.dma_sem = (eng, k % NDSEM)
            op.dma_val = 16 * (k // NDSEM + 1)
            self.last_dma[op.dma_sem] = op
            key = ("dma", id(op))
        else:
            key = eng
        op.deps = list(deps.values())
        for b in reads:
            b = getattr(b, "buf", b)
            b.readers[key] = op
        for b in writes:
            b = getattr(b, "buf", b)
            b.writers = {key: op}
            b.readers = {}
        self.ops[eng].append(op)
        return op

    def op(self, eng, fn, reads=(), writes=()):
        return self._add(eng, fn, reads, writes, False)

    def dma(self, eng, fn, reads=(), writes=()):
        return self._add(eng, fn, reads, writes, True)

    def barrier(self):
        toks = []
        for e in ENGS:
            for op in reversed(self.ops[e]):
                if not op.is_dma:
                    toks.append(op)
                    break
        toks += list(self.last_dma.values())
        for e in ENGS:
            self.pending[e] = list(toks) + (self.pending[e] or [])

    def emit(self):
        nc = self.nc
        for e in ENGS:
            for op in self.ops[e]:
                for d in op.deps:
                    if not d.is_dma and not (d.eng == e and e == "pe"):
                        d.inc = True
        for e in ENGS:
            c = 0
            for op in self.ops[e]:
                if not op.is_dma and op.inc:
                    c += 1
                    op.cnt = c
        with contextlib.ExitStack() as st:
            esem = {e: st.enter_context(nc.semaphore("s_" + e)) for e in ENGS if e != "sp"}
            dsem = {}
            for e in ENGS:
                if self.ndma[e] > 0:
                    for i in range(NDSEM):
                        dsem[(e, i)] = st.enter_context(nc.semaphore(f"d_{e}{i}"))
            block = st.enter_context(nc.Block())

            def run(e, eng):
                waited = {}

                def wait(sem, val, key):
                    if waited.get(key, 0) >= val:
                        return
                    waited[key] = val
                    eng.wait_ge(sem, val)

                for op in self.ops[e]:
                    for d in op.deps:
                        if d.is_dma:
                            wait(dsem[d.dma_sem], d.dma_val, d.dma_sem)
                        elif not (d.eng == e and e == "pe"):
                            wait(esem[d.eng], d.cnt, d.eng)
                    if op.is_dma:
                        if op.dma_val > 16:
                            wait(dsem[op.dma_sem], op.dma_val - 16, op.dma_sem)
                        op.fn(eng).then_inc(dsem[op.dma_sem], 16)
                    else:
                        ins = op.fn(eng)
                        if op.inc:
                            ins.then_inc(esem[e], 1)
                last = {}
                for op in self.ops[e]:
                    if op.is_dma:
                        last[op.dma_sem] = op.dma_val
                for k, v in last.items():
                    wait(dsem[k], v, k)

            if self.ops["sp"]:
                @block.sync
                def _(eng):
                    run("sp", eng)
            if self.ops["act"]:
                @block.scalar
                def _(eng):
                    run("act", eng)
            if self.ops["dve"]:
                @block.vector
                def _(eng):
                    run("dve", eng)
            if self.ops["pool"]:
                @block.gpsimd
                def _(eng):
                    run("pool", eng)
            if self.ops["pe"]:
                @block.tensor
                def _(eng):
                    run("pe", eng)


class Tl:
    def __init__(self, ap, name=""):
        self.ap = ap
        self.buf = Buf(name)

    def __getitem__(self, k):
        return self.ap[k]


class Ring:
    def __init__(self, tiles):
        self.t = tiles
        self.i = 0

    def next(self):
        t = self.t[self.i % len(self.t)]
        self.i += 1
        return t


_DSZ = {F32: 4, BF16: 2, I32: 4}


class Arena:
    def __init__(self, nc, st, nbytes):
        self.t = st.enter_context(nc.sbuf_tensor("arena", [128, nbytes // 4], F32))
        self.cap = nbytes
        self.off = 0

    def reset(self):
        self.off = 0

    def alloc(self, free, dt, name=""):
        free = tuple(free)
        n = int(np.prod(free))
        nb = (n * _DSZ[dt] + 31) // 32 * 32
        assert self.off + nb <= self.cap, (name, self.off, nb, self.cap)
        ap = self.t[:, self.off // 4:(self.off + nb) // 4]
        self.off += nb
        if dt != F32:
            ap = ap.bitcast(dt)
        ap = ap[:, 0:n]
        if len(free) == 2:
            ap = ap.rearrange("p (a b) -> p a b", a=free[0])
        elif len(free) == 3:
            ap = ap.rearrange("p (a b c) -> p a b c", a=free[0], b=free[1])
        return Tl(ap, name)

    def ring(self, k, free, dt, name=""):
        return Ring([self.alloc(free, dt, f"{name}{i}") for i in range(k)])


def build(dbg=(), stop_after=99, nexp=NEXP):
    nc = bass.Bass("TRN2", target_bir_lowering=False)
    dbg = set(dbg)

    def din(name, shape, dt=F32):
        return nc.dram_tensor(name, list(shape), dt, kind="ExternalInput").ap()

    def dscr(name, shape, dt):
        kind = "ExternalOutput" if name in dbg else "Internal"
        return Tl(nc.dram_tensor(name, list(shape), dt, kind=kind).ap(), name)

    x_loc = din("x_loc", [NLOC, D])
    x_A = din("x_A", [NA, D])
    x_B = din("x_B", [NB, D])
    cT = din("cT", [128, 16, 2])
    w_mod = din("w_mod", [D, 6 * D])
    bmod2 = din("bmod2", [2, 6 * D])
    norm1_g = din("norm1_g", [1, D])
    norm2_g = din("norm2_g", [1, D])
    w_in = din("w_in", [D, D_IN])
    w_a12 = din("w_a12", [D, 32])
    wd1 = din("wd1", [17, 1024])
    wd2 = din("wd2", [17, 1024])
    gla_g = din("gla_g", [1, 512])
    qa_g = din("qa_g", [128, 4])
    kva_g = din("kva_g", [128, 4])
    w_uq_n = din("w_uq_n", [512, 2048])
    w_uq_r = din("w_uq_r", [512, 1024])
    w_ukv_k = din("w_ukv_k", [512, 2048])
    w_ukv_v = din("w_ukv_v", [512, 2048])
    qn_g = din("qn_g", [192, 1])
    kn_g = din("kn_g", [192, 1])
    w_o_gla = din("w_o_gla", [D, D])
    w_o_mla = din("w_o_mla", [D, D])
    w_out = din("w_out", [D, D])
    w_rt = din("w_rt", [D, 72])
    b_rt = din("b_rt", [1, 72])
    w_eg = din("w_eg", [nexp, D, 512])
    w_eu = din("w_eu", [nexp, D, 512])
    w_ed = din("w_ed", [nexp, 512, D])
    cosq = din("cosq", [64, NLOC])
    sinq = din("sinq", [64, NLOC])
    cosk = din("cosk", [64, NKEY])
    sink = din("sink", [64, NKEY])
    consts = din("consts", [8, 128, 128])
    rt_c = din("rt_c", [64, 64])
    iota_c = din("iota_c", [128, 64])
    out_d = nc.dram_tensor("out", [NLOC, D], F32, kind="ExternalOutput").ap()

    mod_s = dscr("mod_s", [2, 6 * D], F32)
    qT_s = dscr("qT_s", [1024, NLOC], BF16)
    kT_s = dscr("kT_s", [1024, NLOC], BF16)
    k_s = dscr("k_s", [NLOC, 1024], BF16)
    v_s = dscr("v_s", [NLOC, 2048], BF16)
    r_s = dscr("r_s", [NLOC, 2048], BF16)
    aT_s = dscr("aT_s", [32, NLOC], F32)
    cqnT_s = dscr("cqnT_s", [512, NLOC], BF16)
    ckvnT_s = dscr("ckvnT_s", [512, NKEY], BF16)
    krT_s = dscr("krT_s", [64, NKEY], F32)
    gaT_s = dscr("gaT_s", [D, NLOC], F32)
    gbT_s = dscr("gbT_s", [D, NLOC], F32)
    kA_s = dscr("kA_s", [NA, 1024], BF16)
    vA_s = dscr("vA_s", [NA, 2048], BF16)
    aA_s = dscr("aA_s", [16, NA], F32)
    kB_s = dscr("kB_s", [NB, 1024], BF16)
    vB_s = dscr("vB_s", [NB, 2048], BF16)
    aB_s = dscr("aB_s", [16, NB], F32)
    of_s = dscr("of_s", [NLOC, 2048], F32)
    yglaT_s = dscr("yglaT_s", [D, NLOC], BF16)
    ymlaT_s = dscr("ymlaT_s", [D, NLOC], BF16)
    x1_s = dscr("x1_s", [NLOC, D], F32)
    h2_s = dscr("h2_s", [NLOC, D], BF16)
    xdisp = dscr("xdisp", [NEXP * CAP + 128, D], BF16)
    cnt_s = dscr("cnt_s", [1, 64], F32)
    ydisp = dscr("ydisp", [NEXP * CAP + 128, D], F32)

    P = Prog(nc)
    with contextlib.ExitStack() as st:
        A = Arena(nc, st, 200 * 1024)
        def sbt(name, shape, dt):
            return Tl(st.enter_context(nc.sbuf_tensor(name, list(shape), dt)), name)
        cst_f = sbt("cst_f", [128, 8, 128], F32)
        cst_b = sbt("cst_b", [128, 8, 128], BF16)
        psb = [Tl(st.enter_context(nc.psum_tensor(f"ps{i}", [128, 512], F32)), f"ps{i}") for i in range(5)]
        psT = [Tl(st.enter_context(nc.psum_tensor(f"psT{i}", [128, 8, 128], BF16)), f"psT{i}") for i in range(2)]
        psS = Tl(st.enter_context(nc.psum_tensor("psS", [128, 512], F32)), "psS")
        for _t in psb + psT + [psS]:
            _t.buf.excl = True
        psr = Ring(psb)
        psTr = Ring(psT[:int(os.environ.get('NPST', '2'))])

        IDENT, SU, UI, MASKF, SL, LI, MASKB, ONES = range(8)

        P.dma("sp", lambda e: e.dma_start(out=cst_f[:], in_=consts.rearrange("c p n -> p c n")), writes=[cst_f])
        P.op("dve", lambda e: e.tensor_copy(out=cst_b[:], in_=cst_f[:]), reads=[cst_f], writes=[cst_b])

        def ACT(out_ap, in_ap, func, reads, writes, **kw):
            P.op("act", lambda e: e.activation(out=out_ap, in_=in_ap, func=func, **kw), reads=reads, writes=writes)

        def CP(eng, out_ap, in_ap, reads, writes):
            if eng == "act":
                P.op("act", lambda e: e.copy(out=out_ap, in_=in_ap), reads=reads, writes=writes)
            else:
                P.op(eng, lambda e: e.tensor_copy(out=out_ap, in_=in_ap), reads=reads, writes=writes)

        def TT(eng, out_ap, in0, in1, op, reads, writes):
            P.op(eng, lambda e: e.tensor_tensor(out=out_ap, in0=in0, in1=in1, op=op), reads=reads, writes=writes)

        def TS(eng, out_ap, in0, s1, s2, op0, op1, reads, writes):
            if s2 is None:
                P.op(eng, lambda e: e.tensor_scalar(out=out_ap, in0=in0, scalar1=s1, scalar2=None, op0=op0),
                     reads=reads, writes=writes)
            else:
                P.op(eng, lambda e: e.tensor_scalar(out=out_ap, in0=in0, scalar1=s1, scalar2=s2, op0=op0, op1=op1),
                     reads=reads, writes=writes)

        def STT(out_ap, in0, scalar, in1, op0, op1, reads, writes):
            P.op("dve", lambda e: e.scalar_tensor_tensor(out=out_ap, in0=in0, scalar=scalar, in1=in1, op0=op0, op1=op1),
                 reads=reads, writes=writes)

        def MM(ps_ap, lhsT, rhs, start, stop, reads, writes):
            P.op("pe", lambda e: e.matmul(ps_ap, lhsT=lhsT, rhs=rhs, start=start, stop=stop), reads=reads, writes=writes)

        def TR(ps_ap, in_ap, ident, reads, writes):
            P.op("pe", lambda e: e.transpose(out=ps_ap, in_=in_ap, identity=ident), reads=reads, writes=writes)

        def DMA(eng, out_ap, in_ap, reads, writes):
            P.dma(eng, lambda e: e.dma_start(out=out_ap, in_=in_ap), reads=reads, writes=writes)

        def RED(eng, out_ap, in_ap, op, reads, writes):
            P.op(eng, lambda e: e.tensor_reduce(out=out_ap, in_=in_ap, axis=AX.X, op=op), reads=reads, writes=writes)

        flip = [0]

        EVAC = os.environ.get("EVAC", "both")

        def evac_eng():
            flip[0] ^= 1
            if EVAC != "both":
                return EVAC
            return "act" if flip[0] else "dve"

        def rstd_ops(sst, ssa, rst, rsa, n, extra=1.0):
            ACT(rsa, ssa, AF.Sqrt, [sst], [rst], scale=1.0 / n, bias=EPS)
            P.op("dve", lambda e: e.reciprocal(out=rsa, in_=rsa), reads=[rst], writes=[rst])
            if extra != 1.0:
                TS("dve", rsa, rsa, extra, None, ALU.mult, None, [rst], [rst])

        IDENT, SU, UI, MASKF, SL, LI, MASKB, ONES = range(8)
        DMA("sp", cst_f[:], consts.rearrange("c p n -> p c n"), [], [cst_f])
        CP("dve", cst_b[:], cst_f[:], [cst_f], [cst_b])
        identb_t = sbt("identb", [128, 128], BF16)
        CP("dve", identb_t[:], cst_f[:, IDENT, :], [cst_f], [identb_t])
        ident_b = identb_t[:] if os.environ.get("IDSEP", "1") == "1" else cst_b[:, IDENT, :]
        ident_f = cst_f[:, IDENT, :]

        A.reset()
        cTf = A.alloc([16, 2], F32, "cTf")
        cTe = A.alloc([16, 2], F32, "cTe")
        cTs = A.alloc([16, 2], BF16, "cTs")
        bm = A.alloc([6 * D], F32, "bm")
        mrow = A.alloc([6 * D], F32, "mrow")
        wtm = A.ring(3, [16, 512], BF16, "wtm")
        DMA("sp", cTf[:], cT, [], [cTf])
        DMA("sp", bm[0:2, :], bmod2, [], [bm])
        ACT(cTe[:], cTf[:], AF.Sigmoid, [cTf], [cTe])
        TT("dve", cTs[:], cTf[:], cTe[:], ALU.mult, [cTf, cTe], [cTs])
        for nb in range(24):
            wt = wtm.next()
            DMA("pool", wt[:], w_mod[:, nb * 512:(nb + 1) * 512].rearrange("(kc p) n -> p kc n", p=128), [], [wt])
            ps = psr.next()
            for kc in range(16):
                MM(ps[0:2, :], cTs[:, kc, :], wt[:, kc, :], kc == 0, kc == 15, [cTs, wt], [ps])
            TT("dve", mrow[0:2, nb * 512:(nb + 1) * 512], ps[0:2, :], bm[0:2, nb * 512:(nb + 1) * 512], ALU.add,
               [ps, bm], [mrow])
        DMA("sp", mod_s[:], mrow[0:2, :], [mrow], [mod_s])
        P.barrier()
        if stop_after == 0:
            P.emit()
            return nc

        def mod_tile(dst, tmp1, tmp2, row, chunk, gain_ap):
            DMA("sp", tmp1[:], mod_s[row, chunk * D:(chunk + 1) * D].partition_broadcast(128), [mod_s], [tmp1])
            DMA("sp", tmp2[:], gain_ap.partition_broadcast(128), [], [tmp2])
            STT(dst[:], tmp1[:], 1.0, tmp2[:], ALU.add, ALU.mult, [tmp1, tmp2], [dst])

        def shift_tile(dst, row, chunk):
            DMA("sp", dst[:], mod_s[row, chunk * D:(chunk + 1) * D].partition_broadcast(128), [mod_s], [dst])

        STEPS = int(os.environ.get("P1A_STEPS", "9"))

        def norm_mod_tile(xt, Bsc_, Bsh_, hb, junk, ss, rs, tmp, n=D):
            if STEPS >= 2:
                ACT(junk[:], xt[:], AF.Square, [xt], [junk, ss], accum_out=ss[:])
            if STEPS >= 3:
                rstd_ops(ss, ss[:], rs, rs[:], n)
            if STEPS >= 4:
                STT(tmp[:], xt[:], rs[:, 0:1], Bsc_[:], ALU.mult, ALU.mult, [xt, rs, Bsc_], [tmp])
            if STEPS >= 5:
                TT(POOL_ENG, hb[:], tmp[:], Bsh_[:], ALU.add, [tmp, Bsh_], [hb])

        dbg_t = sbt('dbg_t', [128, 128], BF16)

        def transpose_tile(src, dstT, col0, nch=16):
            for g in range(0, nch, 8):
                pt = psTr.next()
                for j in range(8):
                    c = g + j
                    TR(pt[:, j, :], src[:, c * 128:(c + 1) * 128], ident_b, [src, cst_b, identb_t], [pt])
                CP(evac_eng(), dstT[:, g:g + 8, col0:col0 + 128], pt[:], [pt], [dstT])

        A.reset()
        hT = A.alloc([16, NA], BF16, "hT")
        Bsc = A.alloc([D], F32, "Bsc")
        Bsh = A.alloc([D], F32, "Bsh")
        Bscc = A.alloc([D], F32, "Bscc")
        Bshc = A.alloc([D], F32, "Bshc")
        mark1 = A.off
        t1 = A.alloc([D], F32, "t1")
        t2 = A.alloc([D], F32, "t2")
        mod_tile(Bsc, t1, t2, 0, 1, norm1_g[0, :])
        shift_tile(Bsh, 0, 0)
        t1b = A.alloc([D], F32, "t1b")
        t2b = A.alloc([D], F32, "t2b")
        mod_tile(Bscc, t1b, t2b, 1, 1, norm1_g[0, :])
        shift_tile(Bshc, 1, 0)
        P.barrier()
        if stop_after == 10:
            P.emit()
            return nc
        A.off = mark1
        mark_u = A.off
        xtr = A.ring(2, [D], F32, "xt")
        tmpr = A.ring(1, [D], F32, "tmp")
        hbr = A.ring(2, [D], BF16, "hb")
        junk = A.alloc([D], BF16, "junk")
        ssr = A.ring(2, [1], F32, "ss")
        rsr = A.ring(2, [1], F32, "rs")
        A.off = mark_u
        wtr = A.ring(2, [16, 512], BF16, "wt")
        stT = A.ring(2, [NA], F32, "stT")
        stN = A.ring(2, [9, 512], BF16, "stN")
        raw4 = A.alloc([4, 512], F32, "raw4")
        sq4 = A.ring(2, [512], BF16, "sq")
        sgr = A.ring(2, [512], F32, "sg")
        rbc = A.alloc([512], F32, "rbc")
        gcol_q = A.alloc([4], F32, "gcq")
        gcol_kv = A.alloc([4], F32, "gckv")

        def p1a(x_d, ntok, nctx_tiles):
            for t in range(min(ntok // 128, int(os.environ.get('P1A_TILES', '99')))):
                xt = xtr.next()
                DMA("sp", xt[:], x_d[t * 128:(t + 1) * 128, :], [], [xt])
                hb = hbr.next()
                isc = t < nctx_tiles
                norm_mod_tile(xt, Bscc if isc else Bsc, Bshc if isc else Bsh, hb, junk, ssr.next(), rsr.next(), tmpr.next())
                if STEPS >= 6:
                    transpose_tile(hb, hT, t * 128)

        def load_w(src2d, ncols):
            wt = wtr.next()
            DMA("pool", wt[:, :, 0:ncols], src2d.rearrange("(kc p) n -> p kc n", p=128), [], [wt])
            return wt

        def mm_T(wt, m0, mc, g0, gn):
            ps = psr.next()
            for kc in range(16):
                MM(ps[0:mc, 0:gn], wt[:, kc, m0:m0 + mc], hT[:, kc, g0:g0 + gn], kc == 0, kc == 15, [wt, hT], [ps])
            return ps

        def mm_N(wt, ncols, t):
            ps = psr.next()
            for kc in range(16):
                MM(ps[:, 0:ncols], hT[:, kc, t * 128:(t + 1) * 128], wt[:, kc, 0:ncols], kc == 0, kc == 15, [wt, hT], [ps])
            return ps

        def groups(ntok):
            return [(g0, min(512, ntok - g0)) for g0 in range(0, ntok, 512)]

        def job_T(wsrc, ncols, ntok, dst, dst_row0, dst_col0, dt, func=None, scale=1.0):
            wt = load_w(wsrc, ncols)
            for m0 in range(0, ncols, 128):
                mc = min(128, ncols - m0)
                stg = stT.next()
                sap = stg[:] if dt == F32 else stg[:].bitcast(BF16)
                for (g0, gn) in groups(ntok):
                    ps = mm_T(wt, m0, mc, g0, gn)
                    if func is not None:
                        ACT(sap[0:mc, g0:g0 + gn], ps[0:mc, 0:gn], func, [ps], [stg], scale=scale)
                    elif scale != 1.0:
                        P_mul(sap[0:mc, g0:g0 + gn], ps[0:mc, 0:gn], scale, [ps], [stg])
                    else:
                        CP(evac_eng(), sap[0:mc, g0:g0 + gn], ps[0:mc, 0:gn], [ps], [stg])
                DMA("sp", dst[dst_row0 + m0:dst_row0 + m0 + mc, dst_col0:dst_col0 + ntok], sap[0:mc, 0:ntok], [stg], [dst])
            return wt

        def P_mul(out_ap, in_ap, scale, reads, writes):
            P.op("act", lambda e: e.mul(out=out_ap, in_=in_ap, mul=scale), reads=reads, writes=writes)

        def job_N(wsrc, ncols, ntok, dst, dst_col0, silu=False, wt=None):
            if wt is None:
                wt = load_w(wsrc, ncols)
            nt = ntok // 128
            for t0 in range(0, nt, 9):
                tn = min(9, nt - t0)
                stg = stN.next()
                for tt in range(tn):
                    t = t0 + tt
                    ps = mm_N(wt, ncols, t)
                    if silu:
                        sg = sgr.next()
                        ACT(sg[:, 0:ncols], ps[:, 0:ncols], AF.Sigmoid, [ps], [sg])
                        TT("dve", stg[:, tt, 0:ncols], ps[:, 0:ncols], sg[:, 0:ncols], ALU.mult, [ps, sg], [stg])
                    else:
                        CP(evac_eng(), stg[:, tt, 0:ncols], ps[:, 0:ncols], [ps], [stg])
                DMA("sp", dst[t0 * 128:(t0 + tn) * 128, dst_col0:dst_col0 + ncols].rearrange("(t p) c -> p t c", p=128),
                    stg[:, 0:tn, 0:ncols], [stg], [dst])

        def job_Tnorm(wsrc, ntok, gcol, dst, dst_col0):
            wt = load_w(wsrc, 512)
            stgs = [stT.next(), stT.next()]
            for (g0, gn) in groups(ntok):
                for m in range(4):
                    ps = mm_T(wt, m * 128, 128, g0, gn)
                    sq = sq4.next()
                    CP("dve", raw4[:, m, 0:gn], ps[:, 0:gn], [ps], [raw4])
                    ACT(sq[:, 0:gn], raw4[:, m, 0:gn], AF.Square, [raw4], [sq])
                    MM(psS[:, 0:gn], cst_b[:, ONES, :], sq[:, 0:gn], m == 0, m == 3, [sq, cst_b], [psS])
                rstd_ops(psS, psS[:, 0:gn], rbc, rbc[:, 0:gn], 512)
                for m in range(4):
                    stg = stgs[m // 2]
                    sap = stg[:].bitcast(BF16)
                    off = (m % 2) * NA
                    STT(sap[:, off + g0:off + g0 + gn], raw4[:, m, 0:gn], gcol[:, m:m + 1], rbc[:, 0:gn],
                        ALU.mult, ALU.mult, [raw4, gcol, rbc], [stg])
            for m in range(4):
                stg = stgs[m // 2]
                sap = stg[:].bitcast(BF16)
                off = (m % 2) * NA
                DMA("sp", dst[m * 128:(m + 1) * 128, dst_col0:dst_col0 + ntok], sap[:, off:off + ntok], [stg], [dst])

        O_Q, O_K, O_V, O_R, O_CQ, O_CKV, O_KR, O_GA, O_GB = 0, 1024, 2048, 4096, 6176, 6688, 7200, 7264, 9312

        def load_gcols():
            DMA("sp", gcol_q[:], qa_g, [], [gcol_q])
            DMA("sp", gcol_kv[:], kva_g, [], [gcol_kv])

        p1a(x_A, NA, 2)
        P.barrier()
        if stop_after == 11:
            P.emit()
            return nc
        load_gcols()
        for b in range(2):
            job_N(w_in[:, O_K + b * 512:O_K + (b + 1) * 512], 512, NA, kA_s, b * 512)
        for b in range(4):
            job_N(w_in[:, O_V + b * 512:O_V + (b + 1) * 512], 512, NA, vA_s, b * 512)
        if stop_after == 12:
            P.emit()
            return nc
        job_T(w_a12[:, 0:16], 16, NA, aA_s, 0, 0, F32)
        if stop_after == 13:
            P.emit()
            return nc
        job_Tnorm(w_in[:, O_CKV:O_CKV + 512], NA, gcol_kv, ckvnT_s, 0)
        if stop_after == 14:
            P.emit()
            return nc
        job_T(w_in[:, O_KR:O_KR + 64], 64, NA, krT_s, 0, 0, F32)
        if stop_after == 15:
            P.emit()
            return nc
        P.barrier()
        p1a(x_B, NB, 2)
        P.barrier()
        load_gcols()
        for b in range(2):
            job_N(w_in[:, O_K + b * 512:O_K + (b + 1) * 512], 512, NB, kB_s, b * 512)
        for b in range(4):
            job_N(w_in[:, O_V + b * 512:O_V + (b + 1) * 512], 512, NB, vB_s, b * 512)
        job_T(w_a12[:, 16:32], 16, NB, aB_s, 0, 0, F32)
        P.barrier()
        p1a(x_loc, NLOC, 0)
        P.barrier()
        load_gcols()
        for b in range(2):
            job_T(w_in[:, O_Q + b * 512:O_Q + (b + 1) * 512], 512, NLOC, qT_s, b * 512, 0, BF16, scale=1.0 / 16.0)
        for b in range(2):
            wtk = job_T(w_in[:, O_K + b * 512:O_K + (b + 1) * 512], 512, NLOC, kT_s, b * 512, 0, BF16)
            job_N(None, 512, NLOC, k_s, b * 512, wt=wtk)
        for b in range(4):
            job_N(w_in[:, O_V + b * 512:O_V + (b + 1) * 512], 512, NLOC, v_s, b * 512)
        for b in range(4):
            job_N(w_in[:, O_R + b * 512:O_R + (b + 1) * 512], 512, NLOC, r_s, b * 512, silu=True)
        job_T(w_a12, 32, NLOC, aT_s, 0, 0, F32)
        job_Tnorm(w_in[:, O_CQ:O_CQ + 512], NLOC, gcol_q, cqnT_s, 0)
        job_Tnorm(w_in[:, O_CKV:O_CKV + 512], NLOC, gcol_kv, ckvnT_s, NA)
        job_T(w_in[:, O_KR:O_KR + 64], 64, NLOC, krT_s, 0, NA, F32)
        for b in range(4):
            job_T(w_in[:, O_GA + b * 512:O_GA + (b + 1) * 512], 512, NLOC, gaT_s, b * 512, 0, F32, func=AF.Sigmoid)
        for b in range(4):
            job_T(w_in[:, O_GB + b * 512:O_GB + (b + 1) * 512], 512, NLOC, gbT_s, b * 512, 0, F32, func=AF.Sigmoid)
        P.barrier()
        if stop_after == 1:
            P.emit()
            return nc
        A.reset()
        NKT = NKEY // 128
        ckvnT = A.alloc([4, NKEY], BF16, "ckvnT")
        cqnT = A.alloc([4, NLOC], BF16, "cqnT")
        KrT = A.alloc([NKEY], BF16, "KrT")
        krss = A.alloc([NKT], F32, "krss")
        gcols = A.alloc([4], F32, "gcols")
        RTf = A.alloc([64], F32, "RTf")
        RTb = A.alloc([64], BF16, "RTb")
        wk = A.alloc([4, 256], BF16, "wk")
        wv = A.alloc([4, 256], BF16, "wv")
        wqn = A.alloc([4, 256], BF16, "wqn")
        wqr = A.alloc([4, 128], BF16, "wqr")
        KT = A.alloc([2, NKEY], BF16, "KT")
        Vt = A.alloc([NKT, 256], BF16, "V")
        kscale = A.alloc([2, NKT], F32, "kscale")
        QTn = A.alloc([2, NLOC], BF16, "QTn")
        QTr = A.alloc([2, NLOC], BF16, "QTr")
        rawn = A.alloc([512], F32, "rawn")
        rawr = A.alloc([512], F32, "rawr")
        sqn = A.alloc([512], BF16, "sqn")
        sqr = A.alloc([512], BF16, "sqr")
        rbc2 = A.alloc([512], F32, "rbc2")
        qrg = A.alloc([512], BF16, "qrg")
        tA = A.alloc([512], F32, "tA")
        tB = A.alloc([512], F32, "tB")
        cosc = A.alloc([512], F32, "cosc")
        sinc = A.alloc([512], F32, "sinc")
        krf = A.alloc([512], F32, "krf")
        tmpk = A.alloc([8], F32, "tmpk")
        pTr = A.ring(3, [512], BF16, "pT")
        recip = A.alloc([512], F32, "recip")
        racc = A.alloc([512], F32, "racc")
        rhi = A.alloc([512], BF16, "rhi")
        rlo = A.alloc([512], BF16, "rlo")
        ystg = A.ring(2, [NLOC], BF16, "ystg")
        pss4 = Ring(psb[0:4])
        po = psb[4]
        ones_b = cst_b[:, ONES, :]

        DMA("sp", ckvnT[:], ckvnT_s[:].rearrange("(rc p) n -> p rc n", p=128), [ckvnT_s], [ckvnT])
        DMA("sp", cqnT[:], cqnT_s[:].rearrange("(rc p) n -> p rc n", p=128), [cqnT_s], [cqnT])
        DMA("sp", gcols[:, 0:1], kn_g[0:128, :], [], [gcols])
        DMA("sp", gcols[0:64, 1:2], kn_g[128:192, :], [], [gcols])
        DMA("sp", gcols[:, 2:3], qn_g[0:128, :], [], [gcols])
        DMA("sp", gcols[0:64, 3:4], qn_g[128:192, :], [], [gcols])
        DMA("sp", RTf[0:64, :], rt_c, [], [RTf])
        CP("dve", RTb[0:64, :], RTf[0:64, :], [RTf], [RTb])

        def kchunks():
            return [(c0, min(512, NKEY - c0)) for c0 in range(0, NKEY, 512)]

        def rope_apply(dst_ap, dst_t, xg_bf, xg_t, cos_ap, sin_ap, tabs, n):
            ps2 = pss4.next()
            MM(ps2[0:64, 0:n], RTb[0:64, :], xg_bf, True, True, [RTb, xg_t], [ps2])
            TT("dve", tA[0:64, 0:n], xg_bf, cos_ap, ALU.mult, [xg_t] + tabs, [tA])
            TT("dve", tB[0:64, 0:n], ps2[0:64, 0:n], sin_ap, ALU.mult, [ps2] + tabs, [tB])
            TT("pool", dst_ap, tA[0:64, 0:n], tB[0:64, 0:n], ALU.add, [tA, tB], [dst_t])

        for (c0, cn) in kchunks():
            kt0, nt = c0 // 128, cn // 128
            DMA("sp", krf[0:64, 0:cn], krT_s[:, c0:c0 + cn], [krT_s], [krf])
            DMA("sp", cosc[0:64, 0:cn], cosk[:, c0:c0 + cn], [], [cosc])
            DMA("sp", sinc[0:64, 0:cn], sink[:, c0:c0 + cn], [], [sinc])
            ACT(sqr[0:64, 0:cn], krf[0:64, 0:cn], AF.Square, [krf], [sqr])
            for j in range(nt):
                MM(psS[:, j:j + 1], sqr[0:64, j * 128:(j + 1) * 128], ones_b[0:64, 0:1], True, True, [sqr, cst_b], [psS])
            CP("dve", krss[:, kt0:kt0 + nt], psS[:, 0:nt], [psS], [krss])
            TS("dve", qrg[0:64, 0:cn], krf[0:64, 0:cn], gcols[0:64, 1:2], None, ALU.mult, None, [krf, gcols], [qrg])
            rope_apply(KrT[0:64, c0:c0 + cn], KrT, qrg[0:64, 0:cn], qrg, cosc[0:64, 0:cn], sinc[0:64, 0:cn], [cosc, sinc], cn)

        zrow2 = A.alloc([D], BF16, "zrow2")
        zrowf = A.alloc([D], F32, "zrowf")
        P.op("pool", lambda e: e.memset(zrow2[:], 0.0), reads=[], writes=[zrow2])
        P.op("pool", lambda e: e.memset(zrowf[:], 0.0), reads=[], writes=[zrowf])
        SCL = 192.0 ** -0.5
        NGRP = int(os.environ.get("MLA_GROUPS", "8"))
        for g in range(NGRP):
            DMA("pool", wk[:], w_ukv_k[:, g * 256:(g + 1) * 256].rearrange("(rc p) n -> p rc n", p=128), [], [wk])
            DMA("pool", wv[:], w_ukv_v[:, g * 256:(g + 1) * 256].rearrange("(rc p) n -> p rc n", p=128), [], [wv])
            DMA("pool", wqn[:], w_uq_n[:, g * 256:(g + 1) * 256].rearrange("(rc p) n -> p rc n", p=128), [], [wqn])
            DMA("pool", wqr[:], w_uq_r[:, g * 128:(g + 1) * 128].rearrange("(rc p) n -> p rc n", p=128), [], [wqr])
            if g == 0:
                for e_ in range((NEXP * CAP) // 128 + 1):
                    DMA("pool", xdisp[e_ * 128:(e_ + 1) * 128, :], zrow2[:], [zrow2], [])
                DMA("pool", ydisp[NEXP * CAP:NEXP * CAP + 128, :], zrowf[:], [zrowf], [])
            for (c0, cn) in kchunks():
                kt0, nt = c0 // 128, cn // 128
                for hl in range(2):
                    ps = pss4.next()
                    for rc in range(4):
                        MM(ps[:, 0:cn], wk[:, rc, hl * 128:(hl + 1) * 128], ckvnT[:, rc, c0:c0 + cn], rc == 0, rc == 3,
                           [wk, ckvnT], [ps])
                    TS("dve", KT[:, hl, c0:c0 + cn], ps[:, 0:cn], gcols[:, 0:1], None, ALU.mult, None, [ps, gcols], [KT])
                    ACT(sqn[:, 0:cn], ps[:, 0:cn], AF.Square, [ps], [sqn])
                    for j in range(nt):
                        MM(psS[:, hl * 4 + j:hl * 4 + j + 1], sqn[:, j * 128:(j + 1) * 128], ones_b[:, 0:1], True, True,
                           [sqn, cst_b], [psS])
                for hl in range(2):
                    TT("dve", tmpk[:, 0:nt], psS[:, hl * 4:hl * 4 + nt], krss[:, kt0:kt0 + nt], ALU.add, [psS, krss], [tmpk])
                    rstd_ops(tmpk, tmpk[:, 0:nt], kscale, kscale[:, hl, kt0:kt0 + nt], 192, extra=SCL)
            for kt in range(NKT):
                ps = pss4.next()
                for rc in range(4):
                    MM(ps[:, 0:256], ckvnT[:, rc, kt * 128:(kt + 1) * 128], wv[:, rc, :], rc == 0, rc == 3, [wv, ckvnT], [ps])
                CP(evac_eng(), Vt[:, kt, :], ps[:, 0:256], [ps], [Vt])
            for qc in range(4):
                q0 = qc * 512
                DMA("sp", cosc[0:64, :], cosq[:, q0:q0 + 512], [], [cosc])
                DMA("sp", sinc[0:64, :], sinq[:, q0:q0 + 512], [], [sinc])
                for hl in range(2):
                    psn = pss4.next()
                    for rc in range(4):
                        MM(psn[:, :], wqn[:, rc, hl * 128:(hl + 1) * 128], cqnT[:, rc, q0:q0 + 512], rc == 0, rc == 3,
                           [wqn, cqnT], [psn])
                    psq = pss4.next()
                    for rc in range(4):
                        MM(psq[0:64, :], wqr[:, rc, hl * 64:(hl + 1) * 64], cqnT[:, rc, q0:q0 + 512], rc == 0, rc == 3,
                           [wqr, cqnT], [psq])
                    CP("dve", rawn[:], psn[:, :], [psn], [rawn])
                    CP("act", rawr[0:64, :], psq[0:64, :], [psq], [rawr])
                    ACT(sqn[:], rawn[:], AF.Square, [rawn], [sqn])
                    ACT(sqr[0:64, :], rawr[0:64, :], AF.Square, [rawr], [sqr])
                    MM(psS[:, :], ones_b, sqn[:], True, False, [sqn, cst_b], [psS])
                    MM(psS[:, :], cst_b[0:64, ONES, :], sqr[0:64, :], False, True, [sqr, cst_b], [psS])
                    rstd_ops(psS, psS[:, :], rbc2, rbc2[:], 192)
                    STT(QTn[:, hl, q0:q0 + 512], rawn[:], gcols[:, 2:3], rbc2[:], ALU.mult, ALU.mult, [rawn, gcols, rbc2], [QTn])
                    STT(qrg[0:64, :], rawr[0:64, :], gcols[0:64, 3:4], rbc2[0:64, :], ALU.mult, ALU.mult,
                        [rawr, gcols, rbc2], [qrg])
                    rope_apply(QTr[0:64, hl, q0:q0 + 512], QTr, qrg[0:64, :], qrg, cosc[0:64, :], sinc[0:64, :], [cosc, sinc], 512)
            for hl in range(0 if os.environ.get('MLA_NOATT') else 2):
                h = 2 * g + hl
                stg = ystg.next()
                for qc in range(4):
                    q0 = qc * 512
                    def qk(kt, hl=hl, q0=q0):
                        pss = pss4.next()
                        MM(pss[:, :], KT[:, hl, kt * 128:(kt + 1) * 128], QTn[:, hl, q0:q0 + 512], True, False, [KT, QTn], [pss])
                        MM(pss[:, :], KrT[0:64, kt * 128:(kt + 1) * 128], QTr[0:64, hl, q0:q0 + 512], False, True,
                           [KrT, QTr], [pss])
                        return pss
                    LA = 2
                    pend = [qk(i) for i in range(LA)]
                    for kt in range(NKT):
                        pss = pend.pop(0)
                        if kt + LA < NKT:
                            pend.append(qk(kt + LA))
                        pT = pTr.next()
                        ACT(pT[:], pss[:, :], AF.Exp, [pss, kscale], [pT], scale=kscale[:, hl, kt:kt + 1])
                        MM(po[:, :], Vt[:, kt, hl * 128:(hl + 1) * 128], pT[:], kt == 0, kt == NKT - 1, [Vt, pT], [po])
                        MM(psS[:, :], ones_b, pT[:], kt == 0, kt == NKT - 1, [pT, cst_b], [psS])
                    P.op("dve", lambda e: e.reciprocal(out=recip[:], in_=psS[:, :]), reads=[psS], writes=[recip])
                    TT("dve", stg[:, q0:q0 + 512], po[:, :], recip[:], ALU.mult, [po, recip], [stg])
                DMA("sp", ymlaT_s[h * 128:(h + 1) * 128, :], stg[:], [stg], [ymlaT_s])
        P.barrier()
        if stop_after == 2:
            P.emit()
            return nc
        A.reset()
        S = A.alloc([4, 2, 512], F32, "S")
        Sb = A.alloc([4, 2, 512], BF16, "Sb")
        wdf = A.alloc([1024], F32, "wdf")
        wdh = A.alloc([1024], BF16, "wdh")
        wdl = A.alloc([1024], BF16, "wdl")
        wdt = A.alloc([1024], F32, "wdt")
        glg = A.alloc([512], F32, "glg")
        aTr = A.ring(2, [128], F32, "aT")
        ahr = A.ring(2, [128], BF16, "ah")
        alr = A.ring(2, [128], BF16, "al")
        att_ = A.alloc([128], F32, "att_")
        gex = A.alloc([1024], F32, "gex")
        gtm = A.alloc([1024], F32, "gtm")
        ghi = A.alloc([1024], BF16, "ghi")
        glo = A.alloc([1024], BF16, "glo")
        gt2 = A.alloc([1024], F32, "gt2")
        qTr = A.ring(2, [8, 128], BF16, "qTt")
        kTr = A.ring(2, [8, 128], BF16, "kTt")
        ktr = A.ring(2, [1024], BF16, "kt")
        vtr = A.ring(2, [2048], BF16, "vt")
        ekt = A.alloc([256], F32, "ekt")
        ektr = A.ring(2, [256], F32, "ektr")
        kend = A.ring(2, [256], BF16, "kend")
        eb = A.ring(2, [256], F32, "eb")
        enb = A.ring(2, [256], F32, "enb")
        qdec = A.ring(2, [2, 128], BF16, "qdec")
        kinv = A.ring(2, [2, 128], BF16, "kinv")
        attm = A.ring(2, [128], BF16, "attm")
        de2 = A.ring(2, [2], F32, "de2")
        ofst = A.ring(2, [2048], F32, "ofst")
        rtl = A.ring(2, [2048], BF16, "rt")
        ssg = A.ring(2, [4], F32, "ssg")
        rsg = A.ring(2, [4], F32, "rsg")
        junkg = A.alloc([512], BF16, "junkg")
        ytmp = A.alloc([512], F32, "ytmp")
        ybf = A.ring(2, [2048], BF16, "ybf")
        yTs = A.ring(2, [16, 128], BF16, "yTs")

        DMA("sp", glg[:], gla_g[0, :].partition_broadcast(128), [], [glg])

        def MEMSET(eng, ap, val, writes):
            P.op(eng, lambda e: e.memset(ap, val), reads=[], writes=writes)

        def hilo(eng, src_ap, hi_ap, lo_ap, tmp_ap, src_t, hi_t, lo_t, tmp_t):
            CP(eng, hi_ap, src_ap, [src_t], [hi_t])
            TT(eng, tmp_ap, src_ap, hi_ap, ALU.subtract, [src_t, hi_t], [tmp_t])
            CP(eng, lo_ap, tmp_ap, [tmp_t], [lo_t])

        def load_wd(wd_d):
            DMA("sp", wdf[0:17, :], wd_d, [], [wdf])
            hilo("dve", wdf[0:17, :], wdh[0:17, :], wdl[0:17, :], wdt[0:17, :], wdf, wdh, wdl, wdt)

        def load_a(a_src_ap, a_src_t):
            aT = aTr.next()
            MEMSET("pool", aT[0:32, :], 1.0, [aT])
            DMA("sp", aT[0:16, :], a_src_ap, [a_src_t], [aT])
            return aT

        def gates(aT):
            ah = ahr.next()
            al = alr.next()
            hilo("pool", aT[0:32, :], ah[0:32, :], al[0:32, :], att_[0:32, :], aT, ah, al, att_)
            for half in range(2):
                ps = pss4.next()
                cs = slice(half * 512, (half + 1) * 512)
                MM(ps[:, :], ah[0:17, :], wdh[0:17, cs], True, False, [ah, wdh], [ps])
                MM(ps[:, :], ah[0:17, :], wdl[0:17, cs], False, False, [ah, wdl], [ps])
                MM(ps[:, :], al[0:17, :], wdh[0:17, cs], False, True, [al, wdh], [ps])
                ACT(gex[:, cs], ps[:, :], AF.Exp, [ps], [gex], scale=-1.0)
            ACT(gtm[:], gex[:], AF.Ln, [gex], [gtm], bias=1.0)
            TS("dve", gtm[:], gtm[:], -1.0 / 16.0, None, ALU.mult, None, [gtm], [gtm])
            hilo("dve", gtm[:], ghi[:], glo[:], gt2[:], gtm, ghi, glo, gt2)

        def mm_hl(ps_ap, ps_t, lhs_fn, rhs_fn):
            for i, gx in enumerate((ghi, glo)):
                MM(ps_ap, lhs_fn(gx), rhs_fn(gx), i == 0, i == 1, [gx, cst_b], [ps_t])

        def kend_for(h, kt_tile, EM):
            ps = pss4.next()
            mm_hl(ps[:, 0:256], ps, lambda gx: cst_b[:, EM, :], lambda gx: gx[:, h * 256:(h + 1) * 256])
            ACT(ekt[:], ps[:, 0:256], AF.Exp, [ps], [ekt])
            ke = kend.next()
            TT("dve", ke[:], kt_tile[:, h * 256:(h + 1) * 256], ekt[:], ALU.mult, [kt_tile, ekt], [ke])
            return ke

        def state_update(h, ke, vt, de_ap, de_t):
            for dkc in range(2):
                ps = pss4.next()
                MM(ps[:, :], ke[:, dkc * 128:(dkc + 1) * 128], vt[:, h * 512:(h + 1) * 512], True, True, [ke, vt], [ps])
                STT(S[:, h, dkc, :], S[:, h, dkc, :], de_ap(dkc), ps[:, :], ALU.mult, ALU.add, [S, de_t, ps], [S])
            CP("act", Sb[:, h, :, :], S[:, h, :, :], [S], [Sb])

        def state_pass(k_d, v_d, a_d, ntok):
            def loads(t):
                kt_tile = ktr.next()
                vt = vtr.next()
                DMA("sp", kt_tile[:], k_d[t * 128:(t + 1) * 128, :], [k_d], [kt_tile])
                DMA("sp", vt[:], v_d[t * 128:(t + 1) * 128, :], [v_d], [vt])
                aT = load_a(a_d[0:16, t * 128:(t + 1) * 128], a_d)
                return kt_tile, vt, aT
            nt_ = ntok // 128
            nxt = loads(0)
            for t in range(nt_):
                kt_tile, vt, aT = nxt
                if t + 1 < nt_:
                    nxt = loads(t + 1)
                gates(aT)
                for hp_ in range(0, 4, 2):
                    pair = (hp_, hp_ + 1)
                    psE, psD, ek, kes, d2s = {}, {}, {}, {}, {}
                    for h in pair:
                        psE[h] = pss4.next()
                        mm_hl(psE[h][:, 0:256], psE[h], lambda gx: cst_b[:, SU, :], lambda gx, h=h: gx[:, h * 256:(h + 1) * 256])
                    for h in pair:
                        psD[h] = pss4.next()
                        for dkc in range(2):
                            c0 = h * 256 + dkc * 128
                            mm_hl(psD[h][:, dkc:dkc + 1], psD[h], lambda gx, c0=c0: gx[:, c0:c0 + 128],
                                  lambda gx: cst_b[:, ONES, 0:1])
                    for h in pair:
                        ek[h] = ektr.next()
                        ACT(ek[h][:], psE[h][:, 0:256], AF.Exp, [psE[h]], [ek[h]])
                        d2s[h] = de2.next()
                        ACT(d2s[h][:], psD[h][:, 0:2], AF.Exp, [psD[h]], [d2s[h]])
                    for h in pair:
                        kes[h] = kend.next()
                        TT("dve", kes[h][:], kt_tile[:, h * 256:(h + 1) * 256], ek[h][:], ALU.mult, [kt_tile, ek[h]], [kes[h]])
                    for h in pair:
                        state_update(h, kes[h], vt, lambda dkc, d2=d2s[h]: d2[:, dkc:dkc + 1], d2s[h])

        def local_pass(direction):
            EM, CM = (SU, UI) if direction == 0 else (SL, LI)
            decol = 127 if direction == 0 else 0
            a_row0 = 0 if direction == 0 else 16
            tiles = range(NLOC // 128) if direction == 0 else range(NLOC // 128 - 1, -1, -1)
            def loads(t):
                ts_ = slice(t * 128, (t + 1) * 128)
                qTt = qTr.next()
                kTt = kTr.next()
                kt_tile = ktr.next()
                vt = vtr.next()
                DMA("sp", qTt[:], qT_s[:, ts_].rearrange("(c p) t -> p c t", p=128), [qT_s], [qTt])
                DMA("sp", kTt[:], kT_s[:, ts_].rearrange("(c p) t -> p c t", p=128), [kT_s], [kTt])
                DMA("sp", kt_tile[:], k_s[ts_, :], [k_s], [kt_tile])
                DMA("sp", vt[:], v_s[ts_, :], [v_s], [vt])
                aT = load_a(aT_s[a_row0:a_row0 + 16, ts_], aT_s)
                of = ofst.next()
                rt = None
                if direction == 1:
                    DMA("sp", of[:], of_s[ts_, :], [of_s], [of])
                    rt = rtl.next()
                    DMA("sp", rt[:], r_s[ts_, :], [r_s], [rt])
                return qTt, kTt, kt_tile, vt, aT, of, rt
            tiles = list(tiles)
            nxt = loads(tiles[0])
            for ti, t in enumerate(tiles):
                ts_ = slice(t * 128, (t + 1) * 128)
                qTt, kTt, kt_tile, vt, aT, of, rt = nxt
                if ti + 1 < len(tiles):
                    nxt = loads(tiles[ti + 1])
                gates(aT)
                if direction == 1:
                    ss = ssg.next()
                    rs = rsg.next()
                for hp_ in range(0, 4, 2):
                    pair = (hp_, hp_ + 1)
                    psE, psB = {}, {}
                    for h in pair:
                        psE[h] = pss4.next()
                        mm_hl(psE[h][:, 0:256], psE[h], lambda gx: cst_b[:, EM, :], lambda gx, h=h: gx[:, h * 256:(h + 1) * 256])
                    for h in pair:
                        psB[h] = pss4.next()
                        for dkc in range(2):
                            c0 = h * 256 + dkc * 128
                            mm_hl(psB[h][:, dkc * 128:(dkc + 1) * 128], psB[h], lambda gx, c0=c0: gx[:, c0:c0 + 128],
                                  lambda gx: cst_b[:, CM, :])
                    ek, e1s, e2s = {}, {}, {}
                    for h in pair:
                        ek[h] = ektr.next()
                        ACT(ek[h][:], psE[h][:, 0:256], AF.Exp, [psE[h]], [ek[h]])
                    for h in pair:
                        e1s[h] = eb.next()
                        e2s[h] = enb.next()
                        ACT(e1s[h][:], psB[h][:, 0:256], AF.Exp, [psB[h]], [e1s[h]])
                        ACT(e2s[h][:], psB[h][:, 0:256], AF.Exp, [psB[h]], [e2s[h]], scale=-1.0)
                    kes, qds, kis = {}, {}, {}
                    for h in pair:
                        kes[h] = kend.next()
                        TT("dve", kes[h][:], kt_tile[:, h * 256:(h + 1) * 256], ek[h][:], ALU.mult, [kt_tile, ek[h]], [kes[h]])
                        qds[h] = qdec.next()
                        kis[h] = kinv.next()
                        TT("dve", qds[h][:], qTt[:, 2 * h:2 * h + 2, :], e1s[h][:].rearrange("p (a b) -> p a b", a=2), ALU.mult,
                           [qTt, e1s[h]], [qds[h]])
                        TT("pool", kis[h][:], kTt[:, 2 * h:2 * h + 2, :], e2s[h][:].rearrange("p (a b) -> p a b", a=2), ALU.mult,
                           [kTt, e2s[h]], [kis[h]])
                    ams = {}
                    psA = {}
                    for h in pair:
                        psA[h] = pss4.next()
                        for dkc in range(2):
                            MM(psA[h][:, 0:128], kis[h][:, dkc, :], qds[h][:, dkc, :], dkc == 0, dkc == 1, [kis[h], qds[h]], [psA[h]])
                    for h in pair:
                        ams[h] = attm.next()
                        TT("dve", ams[h][:], psA[h][:, 0:128], cst_f[:, CM, :], ALU.mult, [psA[h], cst_f], [ams[h]])
                    for h in pair:
                        pO = po if h % 2 == 0 else psS
                        MM(pO[:, :], ams[h][:], vt[:, h * 512:(h + 1) * 512], True, False, [ams[h], vt], [pO])
                        for dkc in range(2):
                            MM(pO[:, :], qds[h][:, dkc, :], Sb[:, h, dkc, :], False, dkc == 1, [qds[h], Sb], [pO])
                    for h in pair:
                        pO = po if h % 2 == 0 else psS
                        if direction == 0:
                            CP("act", of[:, h * 512:(h + 1) * 512], pO[:, :], [pO], [of])
                        else:
                            TT("dve", of[:, h * 512:(h + 1) * 512], pO[:, :], of[:, h * 512:(h + 1) * 512], ALU.add, [pO, of], [of])
                            ACT(junkg[:], of[:, h * 512:(h + 1) * 512], AF.Square, [of], [junkg, ss], accum_out=ss[:, h:h + 1])
                    for h in pair:
                        state_update(h, kes[h], vt, lambda dkc, e1=e1s[h]: e1[:, dkc * 128 + decol:dkc * 128 + decol + 1], e1s[h])
                if direction == 0:
                    DMA("sp", of_s[ts_, :], of[:], [of], [of_s])
                else:
                    rstd_ops(ss, ss[:], rs, rs[:], 512)
                    yb = ybf.next()
                    for h in range(4):
                        hs = slice(h * 512, (h + 1) * 512)
                        STT(ytmp[:], of[:, hs], rs[:, h:h + 1], glg[:], ALU.mult, ALU.mult, [of, rs, glg], [ytmp])
                        TT("pool", yb[:, hs], ytmp[:], rt[:, hs], ALU.mult, [ytmp, rt], [yb])
                    yT = yTs.next()
                    transpose_tile(yb, yT, 0)
                    DMA("sp", yglaT_s[:, ts_].rearrange("(c p) t -> p c t", p=128), yT[:], [yT], [yglaT_s])

        def zero_state():
            MEMSET("dve", S[:], 0.0, [S])
            MEMSET("pool", Sb[:], 0.0, [Sb])

        load_wd(wd1)
        zero_state()
        state_pass(kA_s, vA_s, aA_s, NA)
        local_pass(0)
        load_wd(wd2)
        zero_state()
        state_pass(kB_s, vB_s, aB_s, NB)
        local_pass(1)
        P.barrier()
        if stop_after == 3:
            P.emit()
            return nc
        A.reset()
        Bgt1 = A.alloc([D], F32, "Bgt1")
        shift_tile(Bgt1, 0, 2)
        GT = 1024
        ygT = A.alloc([16, GT], BF16, "ygT")
        ymT = A.alloc([16, GT], BF16, "ymT")
        yT3 = A.alloc([16, GT], BF16, "yT3")
        w3r = A.ring(2, [16, 512], BF16, "w3")
        gar = A.ring(2, [4, 512], F32, "ga")
        gbr = A.ring(2, [4, 512], F32, "gb")
        t3a = A.ring(2, [512], F32, "t3a")
        t3b = A.ring(2, [512], F32, "t3b")
        x3r = A.ring(4, [512], F32, "x3")
        x3o = A.ring(2, [512], F32, "x3o")

        def load_w3(src2d):
            wt = w3r.next()
            DMA("pool", wt[:], src2d.rearrange("(kc p) n -> p kc n", p=128), [], [wt])
            return wt

        for g in range(NLOC // GT):
            gs = slice(g * GT, (g + 1) * GT)
            DMA("sp", ygT[:], yglaT_s[:, gs].rearrange("(c p) t -> p c t", p=128), [yglaT_s], [ygT])
            DMA("sp", ymT[:], ymlaT_s[:, gs].rearrange("(c p) t -> p c t", p=128), [ymlaT_s], [ymT])
            for mb in range(4):
                ms = slice(mb * 512, (mb + 1) * 512)
                Wg = load_w3(w_o_gla[:, ms])
                Wm = load_w3(w_o_mla[:, ms])
                for hf in range(GT // 512):
                    hs = slice(hf * 512, (hf + 1) * 512)
                    ts_ = slice(g * GT + hf * 512, g * GT + (hf + 1) * 512)
                    ga = gar.next()
                    gb = gbr.next()
                    DMA("sp", ga[:], gaT_s[ms, ts_].rearrange("(m p) t -> p m t", p=128), [gaT_s], [ga])
                    DMA("sp", gb[:], gbT_s[ms, ts_].rearrange("(m p) t -> p m t", p=128), [gbT_s], [gb])
                    for m in range(4):
                        ps1 = pss4.next()
                        for kc in range(16):
                            MM(ps1[:, :], Wg[:, kc, m * 128:(m + 1) * 128], ygT[:, kc, hs], kc == 0, kc == 15, [Wg, ygT], [ps1])
                        ps2 = pss4.next()
                        for kc in range(16):
                            MM(ps2[:, :], Wm[:, kc, m * 128:(m + 1) * 128], ymT[:, kc, hs], kc == 0, kc == 15, [Wm, ymT], [ps2])
                        ta = t3a.next()
                        tb = t3b.next()
                        TT("dve", ta[:], ps1[:, :], ga[:, m, :], ALU.mult, [ps1, ga], [ta])
                        TT("dve", tb[:], ps2[:, :], gb[:, m, :], ALU.mult, [ps2, gb], [tb])
                        TT("pool", yT3[:, mb * 4 + m, hs], ta[:], tb[:], ALU.add, [ta, tb], [yT3])
            for nb in range(4):
                ns = slice(nb * 512, (nb + 1) * 512)
                Wo = load_w3(w_out[:, ns])
                for t4 in range(0, GT // 128, 4):
                    xts = []
                    for tt in range(t4, t4 + 4):
                        rows = slice(g * GT + tt * 128, g * GT + (tt + 1) * 128)
                        xt = x3r.next()
                        DMA("sp", xt[:], x_loc[rows, ns], [], [xt])
                        xts.append(xt)
                    for tt in range(t4, t4 + 4):
                        rows = slice(g * GT + tt * 128, g * GT + (tt + 1) * 128)
                        xt = xts[tt - t4]
                        ps = pss4.next()
                        for kc in range(16):
                            MM(ps[:, :], yT3[:, kc, tt * 128:(tt + 1) * 128], Wo[:, kc, :], kc == 0, kc == 15, [yT3, Wo], [ps])
                        ta = t3a.next()
                        TT("dve", ta[:], ps[:, :], Bgt1[:, ns], ALU.mult, [ps, Bgt1], [ta])
                        xo = x3o.next()
                        TT("pool", xo[:], ta[:], xt[:], ALU.add, [ta, xt], [xo])
                        DMA("sp", x1_s[rows, ns], xo[:], [xo], [x1_s])
        P.barrier()
        if stop_after == 4:
            P.emit()
            return nc

        A.reset()
        NT = NLOC // 128
        Bgt2 = A.alloc([D], F32, "Bgt2")
        shift_tile(Bgt2, 0, 5)
        destI = A.alloc([NT, 2], I32, "destI")
        wts = A.alloc([NT, 2], F32, "wts")
        carry = A.alloc([64], F32, "carry")
        iot = A.alloc([64], F32, "iot")
        brt = A.alloc([72], F32, "brt")
        wrf = A.alloc([16, 72], F32, "wrf")
        wrh = A.alloc([16, 72], BF16, "wrh")
        wrl = A.alloc([16, 72], BF16, "wrl")
        wrt = A.alloc([16, 72], F32, "wrt")
        m4b = A.off
        Bsc2 = A.alloc([D], F32, "Bsc2")
        Bsh2 = A.alloc([D], F32, "Bsh2")
        m4 = A.off
        u1 = A.alloc([D], F32, "u1")
        u2 = A.alloc([D], F32, "u2")
        mod_tile(Bsc2, u1, u2, 0, 4, norm2_g[0, :])
        shift_tile(Bsh2, 0, 3)
        P.barrier()
        A.off = m4
        x1t = A.ring(2, [D], F32, "x1t")
        tmp4 = A.alloc([D], F32, "tmp4")
        h2f = A.alloc([D], F32, "h2f")
        h2h = A.ring(2, [D], BF16, "h2h")
        h2l = A.alloc([D], BF16, "h2l")
        h2p = A.ring(2, [D], BF16, "h2p")
        junk4 = A.alloc([D], BF16, "junk4")
        h2hT = A.alloc([16, 128], BF16, "h2hT")
        h2lT = A.alloc([16, 128], BF16, "h2lT")
        zrow = A.alloc([D], BF16, "zrow")
        sm = A.alloc([64], F32, "sm")
        lg = A.alloc([72], F32, "lg")
        ohG = A.alloc([8], F32, "ohG")
        eg8 = A.alloc([8], F32, "eg8")
        msk = A.alloc([8, 8], F32, "msk")
        le = A.alloc([8], F32, "le")
        le2 = A.alloc([8], F32, "le2")
        oh1 = A.alloc([8], F32, "oh1")
        oh2 = A.alloc([8], F32, "oh2")
        o64a = A.alloc([8, 8], F32, "o64a")
        o64b = A.alloc([8, 8], F32, "o64b")
        Cb = A.alloc([64], BF16, "Cb")
        Cf = A.alloc([64], F32, "Cf")
        pex = A.alloc([64], F32, "pex")
        t64 = A.alloc([64], F32, "t64")
        dstf = A.alloc([2], F32, "dstf")

        DMA("sp", iot[:], iota_c, [], [iot])
        DMA("sp", brt[:], b_rt[0, :].partition_broadcast(128), [], [brt])
        DMA("sp", wrf[:], w_rt.rearrange("(kc p) n -> p kc n", p=128), [], [wrf])
        hilo("dve", wrf[:], wrh[:], wrl[:], wrt[:], wrf, wrh, wrl, wrt)
        MEMSET("dve", carry[:], 0.0, [carry])

        def col(i):
            return sm[:, i:i + 1]

        def TSs(out_ap, in0, s1, op0, reads, writes, s2=None, op1=None):
            TS("dve", out_ap, in0, s1, s2, op0, op1, reads, writes)

        for t in range(NT):
            rows = slice(t * 128, (t + 1) * 128)
            xt = x1t.next()
            DMA("sp", xt[:], x1_s[rows, :], [x1_s], [xt])
            ss = ssr_4 = None
            ACT(junk4[:], xt[:], AF.Square, [xt], [junk4, sm], accum_out=col(0))
            rstd_ops(sm, col(0), sm, col(1), D)
            STT(tmp4[:], xt[:], col(1), Bsc2[:], ALU.mult, ALU.mult, [xt, sm, Bsc2], [tmp4])
            TT("pool", h2f[:], tmp4[:], Bsh2[:], ALU.add, [tmp4, Bsh2], [h2f])
            hh = h2h.next()
            CP("act", hh[:], h2f[:], [h2f], [hh])
            TT("dve", tmp4[:], h2f[:], hh[:], ALU.subtract, [h2f, hh], [tmp4])
            CP("pool", h2l[:], tmp4[:], [tmp4], [h2l])
            hp = h2p.next()
            CP("pool", hp[:].rearrange("s (kc p) -> s kc p", kc=16), hh[:].rearrange("s (p kc) -> s kc p", kc=16), [hh], [hp])
            transpose_tile(hh, h2hT, 0)
            transpose_tile(h2l, h2lT, 0)
            psl = pss4.next()
            for kc in range(16):
                MM(psl[:, 0:72], h2hT[:, kc, :], wrh[:, kc, :], kc == 0, False, [h2hT, wrh], [psl])
                MM(psl[:, 0:72], h2hT[:, kc, :], wrl[:, kc, :], False, False, [h2hT, wrl], [psl])
                MM(psl[:, 0:72], h2lT[:, kc, :], wrh[:, kc, :], False, kc == 15, [h2lT, wrh], [psl])
            TT("dve", lg[:], psl[:, 0:72], brt[:], ALU.add, [psl, brt], [lg])
            RED("dve", col(2), lg[:, 0:8], ALU.max, [lg], [sm])
            TSs(col(3), col(2), -1.0, ALU.mult, [sm], [sm])
            ACT(eg8[:], lg[:, 0:8], AF.Exp, [lg, sm], [eg8, sm], bias=col(3), accum_out=col(4))
            P.op("dve", lambda e: e.reciprocal(out=col(5), in_=col(4)), reads=[sm], writes=[sm])
            TSs(ohG[:], lg[:, 0:8], col(2), ALU.is_equal, [lg, sm], [ohG])
            lgE = lg[:, 8:72].rearrange("p (g e) -> p g e", g=8)
            TT("dve", msk[:], lgE, ohG[:].unsqueeze(2).to_broadcast([128, 8, 8]), ALU.mult, [lg, ohG], [msk])
            RED("dve", le[:], msk[:].rearrange("p g e -> p e g"), ALU.add, [msk], [le])
            RED("dve", col(6), le[:], ALU.max, [le], [sm])
            TSs(oh1[:], le[:], col(6), ALU.is_equal, [le, sm], [oh1])
            STT(le2[:], oh1[:], -1.0e30, le[:], ALU.mult, ALU.add, [oh1, le], [le2])
            RED("dve", col(7), le2[:], ALU.max, [le2], [sm])
            TSs(oh2[:], le2[:], col(7), ALU.is_equal, [le2, sm], [oh2])
            TSs(col(8), col(6), -1.0, ALU.mult, [sm], [sm])
            ACT(col(9), col(7), AF.Exp, [sm], [sm], bias=col(8))
            TSs(col(10), col(9), 1.0, ALU.add, [sm], [sm])
            P.op("dve", lambda e: e.reciprocal(out=col(10), in_=col(10)), reads=[sm], writes=[sm])
            TT("dve", wts[:, t, 0:1], col(5), col(10), ALU.mult, [sm], [wts])
            TT("dve", wts[:, t, 1:2], wts[:, t, 0:1], col(9), ALU.mult, [sm, wts], [wts])
            gB = ohG[:].unsqueeze(2).to_broadcast([128, 8, 8])
            TT("dve", o64a[:], oh1[:].unsqueeze(1).to_broadcast([128, 8, 8]), gB, ALU.mult, [oh1, ohG], [o64a])
            TT("dve", o64b[:], oh2[:].unsqueeze(1).to_broadcast([128, 8, 8]), gB, ALU.mult, [oh2, ohG], [o64b])
            a64 = o64a[:].rearrange("p g e -> p (g e)")
            b64 = o64b[:].rearrange("p g e -> p (g e)")
            TT("dve", Cf[:], a64, b64, ALU.add, [o64a, o64b], [Cf])
            CP("dve", Cb[:], Cf[:], [Cf], [Cb])
            psx = pss4.next()
            MM(psx[:, 0:64], cst_b[:, SL, :], Cb[:], True, True, [cst_b, Cb], [psx])
            TT("dve", pex[:], psx[:, 0:64], carry[:], ALU.add, [psx, carry], [pex])
            pst = pss4.next()
            MM(pst[:, 0:64], cst_b[:, ONES, :], Cb[:], True, True, [cst_b, Cb], [pst])
            TT("dve", carry[:], carry[:], pst[:, 0:64], ALU.add, [carry, pst], [carry])
            for k, o64 in enumerate((a64, b64)):
                src_t = o64a if k == 0 else o64b
                TT("dve", t64[:], o64, pex[:], ALU.mult, [src_t, pex], [t64])
                RED("dve", col(12 + k), t64[:], ALU.add, [t64], [sm])
                TT("dve", t64[:], o64, iot[:], ALU.mult, [src_t, iot], [t64])
                RED("dve", col(14 + k), t64[:], ALU.add, [t64], [sm])
                TSs(col(16 + k), col(12 + k), float(CAP), ALU.is_ge, [sm], [sm], s2=1.0e6, op1=ALU.mult)
                STT(dstf[:, k:k + 1], col(14 + k), float(CAP), col(12 + k), ALU.mult, ALU.add, [sm], [dstf])
                TT("dve", dstf[:, k:k + 1], dstf[:, k:k + 1], col(16 + k), ALU.add, [dstf, sm], [dstf])
                TSs(dstf[:, k:k + 1], dstf[:, k:k + 1], float(NEXP * CAP), ALU.min, [dstf], [dstf])
            CP("dve", destI[:, t, :], dstf[:], [dstf], [destI])
            for k in range(2):
                P.dma("pool", (lambda e, t=t, k=k, hh=hp: e.indirect_dma_start(
                    out=xdisp[:, :], out_offset=bass.IndirectOffsetOnAxis(ap=destI[:, t, k:k + 1], axis=0),
                    in_=hh[:], in_offset=None)),
                    reads=[hp, destI], writes=[xdisp])
        DMA("sp", cnt_s[:], carry[0:1, :], [carry], [cnt_s])
        P.barrier()
        if stop_after == 5:
            P.emit()
            return nc

        A.off = m4b
        NBLK = CAP // 128
        NE_RUN = int(os.environ.get("NE_RUN", str(NEXP)))
        MODE4 = os.environ.get("MODE4", "")
        wgr = A.ring(2, [16, 512], BF16, "wg")
        wur = A.ring(2, [16, 512], BF16, "wu")
        wdr = A.ring(2, [4, 2048], BF16, "wd")
        xer = A.ring(2, [NBLK, D], BF16, "xe")
        xeTr = A.ring(2, [16, CAP], BF16, "xeT")
        hTe = A.ring(2, [4, CAP], BF16, "hTe")
        sgt = A.ring(2, [CAP], F32, "sgt")
        tgt = A.ring(2, [CAP], F32, "tgt")
        yer = A.ring(2, [D], F32, "ye")
        def load_weights(ex):
            Wg = wgr.next()
            Wu = wur.next()
            Wd = wdr.next()
            if not (MODE4 == "cmp" and ex >= 2):
                DMA("pool", Wg[:], w_eg[ex].rearrange("(p kc) n -> p kc n", kc=16), [], [Wg])
                DMA("pool", Wu[:], w_eu[ex].rearrange("(p kc) n -> p kc n", kc=16), [], [Wu])
                DMA("pool", Wd[:], w_ed[ex].rearrange("(kc p) n -> p kc n", p=128), [], [Wd])
            return Wg, Wu, Wd

        def load_xe(ex):
            xe = xer.next()
            DMA("sp", xe[:], xdisp[ex * CAP:(ex + 1) * CAP, :].rearrange("(b p) d -> p b d", p=128), [xdisp], [xe])
            return xe

        def transposes(xe):
            xeT = xeTr.next()
            for b in range(NBLK):
                for g8 in range(0, 16, 8):
                    pt = psTr.next()
                    for j in range(8):
                        c = g8 + j
                        TR(pt[:, j, :], xe[:, b, c * 128:(c + 1) * 128], ident_b, [xe, identb_t], [pt])
                    CP(evac_eng(), xeT[:, g8:g8 + 8, b * 128:(b + 1) * 128], pt[:], [pt], [xeT])
            return xeT

        W_cur = load_weights(0)
        xe_cur = load_xe(0)
        xeT_cur = transposes(xe_cur)
        for ex in range(NE_RUN):
            Wg, Wu, Wd = W_cur
            xeT = xeT_cur
            if ex + 1 < NE_RUN:
                W_cur = load_weights(ex + 1)
                xe_nxt = load_xe(ex + 1)
            hT = hTe.next()
            for mc in range(4):
                psg = pss4.next()
                for kc in range(16):
                    MM(psg[:, 0:CAP], Wg[:, kc, mc * 128:(mc + 1) * 128], xeT[:, kc, :], kc == 0, kc == 15, [Wg, xeT], [psg])
                psu = pss4.next()
                for kc in range(16):
                    MM(psu[:, 0:CAP], Wu[:, kc, mc * 128:(mc + 1) * 128], xeT[:, kc, :], kc == 0, kc == 15, [Wu, xeT], [psu])
                sg = sgt.next()
                tg = tgt.next()
                ACT(sg[:], psg[:, 0:CAP], AF.Sigmoid, [psg], [sg])
                TT("dve", tg[:], psg[:, 0:CAP], sg[:], ALU.mult, [psg, sg], [tg])
                TT("dve", hT[:, mc, :], psu[:, 0:CAP], tg[:], ALU.mult, [psu, tg], [hT])
            if ex + 1 < NE_RUN:
                xeT_cur = transposes(xe_nxt)
            for b in range(NBLK):
                ye = yer.next()
                for nb in range(4):
                    ps = pss4.next()
                    for kc in range(4):
                        MM(ps[:, :], hT[:, kc, b * 128:(b + 1) * 128], Wd[:, kc, nb * 512:(nb + 1) * 512], kc == 0, kc == 3,
                           [hT, Wd], [ps])
                    CP(evac_eng(), ye[:, nb * 512:(nb + 1) * 512], ps[:, :], [ps], [ye])
                DMA("sp", ydisp[ex * CAP + b * 128:ex * CAP + (b + 1) * 128, :], ye[:], [ye], [ydisp])
        P.barrier()

        A.off = m4b
        y1r = A.ring(2, [D], F32, "y1")
        y2r = A.ring(2, [D], F32, "y2")
        x1r = A.ring(2, [D], F32, "x1r")
        outr = A.ring(2, [D], F32, "outr")
        def loads4c(t):
            rows = slice(t * 128, (t + 1) * 128)
            y1 = y1r.next()
            y2 = y2r.next()
            for k, yk in enumerate((y1, y2)):
                P.dma("pool", (lambda e, t=t, k=k, yk=yk: e.indirect_dma_start(
                    out=yk[:], out_offset=None, in_=ydisp[:, :],
                    in_offset=bass.IndirectOffsetOnAxis(ap=destI[:, t, k:k + 1], axis=0))),
                    reads=[ydisp, destI], writes=[yk])
            xt = x1r.next()
            DMA("sp", xt[:], x1_s[rows, :], [x1_s], [xt])
            return y1, y2, xt
        nxt4 = loads4c(0)
        for t in range(NT):
            rows = slice(t * 128, (t + 1) * 128)
            y1, y2, xt = nxt4
            if t + 1 < NT:
                nxt4 = loads4c(t + 1)
            TS("dve", y1[:], y1[:], wts[:, t, 0:1], None, ALU.mult, None, [y1, wts], [y1])
            STT(y1[:], y2[:], wts[:, t, 1:2], y1[:], ALU.mult, ALU.add, [y2, wts, y1], [y1])
            TT("pool", y2[:], y1[:], Bgt2[:], ALU.mult, [y1, Bgt2], [y2])
            ot = outr.next()
            TT("dve", ot[:], y2[:], xt[:], ALU.add, [y2, xt], [ot])
            DMA("sp", out_d[rows, :], ot[:], [ot], [])
        if stop_after == 99:
            pass
        P.emit()
    return nc


def _rope_tables():
    t = np.arange(4096)
    row = (t // 64).astype(np.float32)
    col = (t % 64).astype(np.float32)
    inv = (1.0 / (np.float32(10000.0) ** (np.arange(16, dtype=np.float32) / np.float32(16)))).astype(np.float32)
    ar = (row[:, None] * inv).astype(np.float32)
    ac = (col[:, None] * inv).astype(np.float32)
    cos = np.concatenate([np.cos(ar), np.cos(ar), np.cos(ac), np.cos(ac)], axis=1).T.astype(np.float32)
    sin = np.concatenate([np.sin(ar), np.sin(ar), np.sin(ac), np.sin(ac)], axis=1).T.astype(np.float32)
    return np.ascontiguousarray(cos), np.ascontiguousarray(sin)


def _consts():
    j = np.arange(128)[:, None]
    i = np.arange(128)[None, :]
    c = np.zeros((8, 128, 128), np.float32)
    c[0] = np.eye(128)
    c[1] = (j > i)
    c[2] = (j <= i)
    c[3] = (j <= i)
    c[4] = (j < i)
    c[5] = (j >= i)
    c[6] = (j >= i)
    c[7] = 1.0
    R = np.zeros((64, 64), np.float32)
    for base in (0, 32):
        for m in range(16):
            R[base + m, base + m + 16] = -1.0
            R[base + 16 + m, base + m] = 1.0
    rt = np.ascontiguousarray(R.T)
    iota = np.tile(np.arange(64, dtype=np.float32)[None, :], (128, 1))
    return c, rt, iota


def _prep(inp, cores):
    f = lambda k: np.asarray(inp[k], dtype=np.float32)
    x, c, ctx, c_ctx = f("x"), f("c"), f("ctx"), f("c_ctx")
    w_in = f("w_in")[0]
    cos, sin = _rope_tables()
    cst, rt, iota = _consts()
    w_uq = f("w_uq")[0].reshape(512, 16, 192)
    w_ukv = f("w_ukv")[0].reshape(512, 16, 256)
    shared = {
        "w_mod": f("w_mod")[0],
        "bmod2": np.ascontiguousarray(np.stack([f("b_mod")[0]] * 2)),
        "norm1_g": f("norm1_g"), "norm2_g": f("norm2_g"),
        "w_in": w_in,
        "gla_g": f("gla_norm_g"),
        "qa_g": np.ascontiguousarray(f("q_a_norm_g")[0].reshape(4, 128).T),
        "kva_g": np.ascontiguousarray(f("kv_a_norm_g")[0].reshape(4, 128).T),
        "w_uq_n": np.ascontiguousarray(w_uq[:, :, :128].reshape(512, 2048)),
        "w_uq_r": np.ascontiguousarray(w_uq[:, :, 128:].reshape(512, 1024)),
        "w_ukv_k": np.ascontiguousarray(w_ukv[:, :, :128].reshape(512, 2048)),
        "w_ukv_v": np.ascontiguousarray(w_ukv[:, :, 128:].reshape(512, 2048)),
        "qn_g": np.ascontiguousarray(f("q_norm_g")[0].reshape(192, 1)),
        "kn_g": np.ascontiguousarray(f("k_norm_g")[0].reshape(192, 1)),
        "w_o_gla": f("w_o_gla")[0], "w_o_mla": f("w_o_mla")[0], "w_out": f("w_out")[0],
        "w_rt": np.ascontiguousarray(np.concatenate([f("w_router_group")[0], f("w_router_expert")[0]], axis=1)),
        "b_rt": np.ascontiguousarray(np.concatenate([f("b_router_group")[0], f("b_router_expert")[0]])[None, :]),
        "w_eg": f("w_exp_gate")[0], "w_eu": f("w_exp_up")[0], "w_ed": f("w_exp_down")[0],
        "consts": cst, "rt_c": rt, "iota_c": iota,
    }
    af = w_in[:, 6144:6160]
    ab = w_in[:, 6160:6176]
    wdf = np.concatenate([f("w_decay_f")[0], f("b_decay_f")], axis=0)
    wdb = np.concatenate([f("w_decay_b")[0], f("b_decay_b")], axis=0)
    maps, orders = [], []
    for core in cores:
        b, hf = core // 2, core % 2
        if hf == 1:
            loc = np.arange(2048, 4096)
            oth = np.arange(0, 2048)
            ctxA, ctxB = ctx[b], ctx[b][::-1]
            a1, a2, wd1, wd2 = af, ab, wdf, wdb
        else:
            loc = np.arange(2047, -1, -1)
            oth = np.arange(4095, 2047, -1)
            ctxA, ctxB = ctx[b][::-1], ctx[b]
            a1, a2, wd1, wd2 = ab, af, wdb, wdf
        m = dict(shared)
        m["x_loc"] = np.ascontiguousarray(x[b][loc])
        m["x_A"] = np.ascontiguousarray(np.concatenate([ctxA, x[b][oth]], axis=0))
        m["x_B"] = np.ascontiguousarray(ctxB)
        cc = np.stack([c[b], c_ctx], axis=1)
        m["cT"] = np.ascontiguousarray(cc.reshape(16, 128, 2).transpose(1, 0, 2))
        m["w_a12"] = np.ascontiguousarray(np.concatenate([a1, a2], axis=1))
        m["wd1"] = np.ascontiguousarray(wd1)
        m["wd2"] = np.ascontiguousarray(wd2)
        m["cosq"] = np.ascontiguousarray(cos[:, loc])
        m["sinq"] = np.ascontiguousarray(sin[:, loc])
        m["cosk"] = np.ascontiguousarray(np.concatenate([np.ones((64, 256), np.float32), cos[:, oth], cos[:, loc]], axis=1))
        m["sink"] = np.ascontiguousarray(np.concatenate([np.zeros((64, 256), np.float32), sin[:, oth], sin[:, loc]], axis=1))
        maps.append(m)
        orders.append((b, loc))
    return maps, orders


def kernel(**inputs):
    cores = list(range(8))
    maps, orders = _prep(inputs, cores)
    nc = build()
    res = run_bass_kernel_spmd(nc, maps, core_ids=cores)
    out = np.zeros((4, 4096, 2048), np.float32)
    for (b, loc), r in zip(orders, res.results):
        out[b, loc] = r["out"]
    return out
```
